# Optimizing a Trainium2 kernel written in Bass

```python
import math
import jax
import jax.numpy as jnp
from jax import lax
import numpy as np

D_MODEL = 1024
BATCH = 16
SEQ = 4096
DEPTH = 1

D_MIX = D_MODEL
SSD_WIDTH = D_MIX // 2
SSD_HEADDIM = 64
SSD_HEADS = SSD_WIDTH // SSD_HEADDIM
SSD_NGROUPS = 2
SSD_HPG = SSD_HEADS // SSD_NGROUPS
SSD_DSTATE = 128
SSD_CONV = 5
SSD_CHUNK = 128
SSD_CONV_CH = SSD_WIDTH + 2 * SSD_NGROUPS * SSD_DSTATE
DA_WIDTH = D_MIX - SSD_WIDTH
DA_HEADDIM = 64
DA_VDIM = 2 * DA_HEADDIM
DA_HEADS = DA_WIDTH // DA_VDIM
Q_BLOCK = 128
COL_Z = SSD_WIDTH
COL_XBC = SSD_CONV_CH
COL_DT = 2 * SSD_HEADS
COL_Q = DA_HEADS * 2 * DA_HEADDIM
COL_K = DA_HEADS * 2 * DA_HEADDIM
COL_V = DA_HEADS * DA_VDIM
D_IN_PROJ = COL_Z + COL_XBC + COL_DT + COL_Q + COL_K + COL_V
N_EXPERT_GROUPS = 4
EXPERTS_PER_GROUP = 8
N_EXPERTS = N_EXPERT_GROUPS * EXPERTS_PER_GROUP
TOP_K = 2
D_EXPERT = 512
MOE_BLOCK = 128
EPS = 1e-6

kernel_name = 'hymba_ssd_diffattn_hmoe_encoder'


def rmsnorm(x, w):
    xf = x.astype(jnp.float32)
    y = xf * lax.rsqrt(jnp.mean(xf * xf, axis=-1, keepdims=True) + EPS)
    return (y * w.astype(jnp.float32)).astype(x.dtype)


def centred_depthwise_conv(u, w, b):
    pad = (SSD_CONV - 1) // 2
    out = lax.conv_general_dilated(
        u, w[:, None, :].astype(u.dtype), window_strides=(1,), padding=[(pad, pad)],
        dimension_numbers=('NWC', 'WIO', 'NWC'), feature_group_count=u.shape[-1])
    return out + b.astype(u.dtype)


def ssd_scan(x, dt, a, bm, cm):
    bsz, L, G, R, P = x.shape
    N = bm.shape[-1]
    Q = SSD_CHUNK
    nc = L // Q
    xdt = (x * dt[..., None]).reshape(bsz, nc, Q, G, R, P)
    a_dt = (dt * a).reshape(bsz, nc, Q, G, R).transpose(0, 3, 4, 1, 2)
    bm = bm.reshape(bsz, nc, Q, G, N)
    cm = cm.reshape(bsz, nc, Q, G, N)
    a_cs = jnp.cumsum(a_dt, axis=-1)
    lower = jnp.tril(jnp.ones((Q, Q), dtype=bool))
    seg = a_cs[..., :, None] - a_cs[..., None, :]
    decay_in = jnp.exp(jnp.where(lower, seg, -jnp.inf))
    cb = jnp.einsum('bclgn,bcsgn->bcgls', cm, bm)
    y_diag = jnp.einsum('bcgls,bgrcls,bcsgrp->bclgrp', cb, decay_in, xdt)
    decay_to_end = jnp.exp(a_cs[..., -1:] - a_cs)
    chunk_states = jnp.einsum('bcsgn,bgrcs,bcsgrp->bcgrpn', bm, decay_to_end, xdt)
    chunk_decay = jnp.exp(a_cs[..., -1])

    def step(h, inp):
        s, d = inp
        return d[..., None, None] * h + s, h

    h0 = jnp.zeros((bsz, G, R, P, N), dtype=xdt.dtype)
    _, h_prev = lax.scan(step, h0, (jnp.moveaxis(chunk_states, 1, 0),
                                    jnp.moveaxis(chunk_decay, -1, 0)))
    h_prev = jnp.moveaxis(h_prev, 0, 1)
    y_off = jnp.einsum('bclgn,bgrcl,bcgrpn->bclgrp', cm, jnp.exp(a_cs), h_prev)
    return (y_diag + y_off).reshape(bsz, L, G, R, P)


def ssd_mixer(z, xbc, dt_raw, conv_w, conv_b, dt_bias_f, dt_bias_b, a_log_f, a_log_b,
              d_skip, norm_w):
    bsz, L = z.shape[:2]
    G, R, P, N, H = SSD_NGROUPS, SSD_HPG, SSD_HEADDIM, SSD_DSTATE, SSD_HEADS
    xbc = jax.nn.silu(centred_depthwise_conv(xbc, conv_w, conv_b)).astype(jnp.float32)
    xs = xbc[..., :SSD_WIDTH].reshape(bsz, L, G, R, P)
    bm = xbc[..., SSD_WIDTH:SSD_WIDTH + G * N].reshape(bsz, L, G, N)
    cm = xbc[..., SSD_WIDTH + G * N:].reshape(bsz, L, G, N)
    dt_raw = dt_raw.astype(jnp.float32)
    dt_f = jax.nn.softplus(dt_raw[..., :H] + dt_bias_f).reshape(bsz, L, G, R)
    dt_b = jax.nn.softplus(dt_raw[..., H:] + dt_bias_b).reshape(bsz, L, G, R)
    a_f = -jnp.exp(a_log_f.astype(jnp.float32)).reshape(G, R)
    a_b = -jnp.exp(a_log_b.astype(jnp.float32)).reshape(G, R)
    flip = lambda t: jnp.flip(t, axis=1)
    y_f = ssd_scan(xs, dt_f, a_f, bm, cm)
    y_b = flip(ssd_scan(flip(xs), flip(dt_b), a_b, flip(bm), flip(cm)))
    y = y_f + y_b + d_skip.astype(jnp.float32).reshape(G, R)[:, :, None] * xs
    y = y.reshape(bsz, L, SSD_WIDTH)
    gated = y * jax.nn.silu(z.astype(jnp.float32))
    return rmsnorm(gated, norm_w).astype(z.dtype)


def diff_attention(q, k, v, lq1, lk1, lq2, lk2, subln_w, lambda_init):
    bsz, L = q.shape[:2]
    H, D, V = DA_HEADS, DA_HEADDIM, DA_VDIM
    out_dtype = q.dtype
    q = q.reshape(bsz, L, H, 2, D).transpose(0, 2, 3, 1, 4).astype(jnp.float32) * (D ** -0.5)
    k = k.reshape(bsz, L, H, 2, D).transpose(0, 2, 3, 1, 4).astype(jnp.float32)
    v = v.reshape(bsz, L, H, V).transpose(0, 2, 1, 3).astype(jnp.float32)
    lam = (jnp.exp(jnp.sum(lq1.astype(jnp.float32) * lk1.astype(jnp.float32)))
           - jnp.exp(jnp.sum(lq2.astype(jnp.float32) * lk2.astype(jnp.float32)))
           + lambda_init)
    slopes = jnp.power(2.0, -8.0 * jnp.arange(1, H + 1, dtype=jnp.float32) / H)
    nq = L // Q_BLOCK
    q_blocks = jnp.moveaxis(q.reshape(bsz, H, 2, nq, Q_BLOCK, D), 3, 0)
    k_pos = jnp.arange(L, dtype=jnp.float32)

    def attend(args):
        qb, blk = args
        q_pos = (blk * Q_BLOCK).astype(jnp.float32) + jnp.arange(Q_BLOCK, dtype=jnp.float32)
        dist = jnp.abs(q_pos[:, None] - k_pos[None, :])
        s = jnp.einsum('bhcqd,bhckd->bhcqk', qb, k) - slopes[:, None, None, None] * dist
        p = jax.nn.softmax(s, axis=-1)
        w = p[:, :, 0] - lam * p[:, :, 1]
        return jnp.einsum('bhqk,bhkv->bhqv', w, v)

    o = lax.map(attend, (q_blocks, jnp.arange(nq, dtype=jnp.int32)))
    o = o.transpose(1, 0, 3, 2, 4).reshape(bsz, L, H, V)
    o = rmsnorm(o, subln_w) * (1.0 - lambda_init)
    return o.reshape(bsz, L, H * V).astype(out_dtype)


def hier_moe(u, w_rg, b_rg, w_re, b_re, w_gate, w_up, w_down):
    bsz, L, d = u.shape
    T = bsz * L
    xt = u.reshape(T, d)
    g_logits = jnp.einsum('td,dg->tg', xt, w_rg).astype(jnp.float32) + b_rg
    p_group = jax.nn.softmax(g_logits, axis=-1)
    g_idx = jnp.argmax(g_logits, axis=-1).astype(jnp.int32)
    p_g = jnp.take_along_axis(p_group, g_idx[:, None], axis=1)
    e_logits = (jnp.einsum('td,de->te', xt, w_re).astype(jnp.float32) + b_re)
    e_logits = e_logits.reshape(T, N_EXPERT_GROUPS, EXPERTS_PER_GROUP)
    sel = jnp.take_along_axis(e_logits, g_idx[:, None, None], axis=1)[:, 0]
    top_p, top_i = lax.top_k(jax.nn.softmax(sel, axis=-1), TOP_K)
    top_p = top_p / jnp.sum(top_p, axis=-1, keepdims=True)
    gates = (p_g * top_p).reshape(-1)
    eid = (g_idx[:, None] * EXPERTS_PER_GROUP + top_i).reshape(-1).astype(jnp.int32)
    tok = jnp.repeat(jnp.arange(T, dtype=jnp.int32), TOP_K)
    n_assign = T * TOP_K
    order = jnp.argsort(eid)
    e_s, tok_s, gate_s = eid[order], tok[order], gates[order]
    counts = jnp.bincount(eid, length=N_EXPERTS).astype(jnp.int32)
    starts = jnp.cumsum(counts) - counts
    padded = (counts + MOE_BLOCK - 1) // MOE_BLOCK * MOE_BLOCK
    p_ends = jnp.cumsum(padded)
    p_starts = p_ends - padded
    slot = p_starts[e_s] + jnp.arange(n_assign, dtype=jnp.int32) - starts[e_s]
    n_blocks = (n_assign + MOE_BLOCK - 1) // MOE_BLOCK + N_EXPERTS
    n_slots = n_blocks * MOE_BLOCK
    slot_tok = jnp.zeros((n_slots,), jnp.int32).at[slot].set(tok_s)
    slot_gate = jnp.zeros((n_slots,), jnp.float32).at[slot].set(gate_s)
    block_start = jnp.arange(n_blocks, dtype=jnp.int32) * MOE_BLOCK
    block_e = jnp.minimum(jnp.searchsorted(p_ends, block_start, side='right'),
                          N_EXPERTS - 1).astype(jnp.int32)

    def run_block(args):
        toks, g, e = args
        xb = xt[toks]
        hdn = jax.nn.silu(xb @ w_gate[e]) * (xb @ w_up[e])
        return (hdn @ w_down[e]) * g[:, None].astype(xb.dtype)

    y = lax.map(run_block, (slot_tok.reshape(n_blocks, MOE_BLOCK),
                            slot_gate.reshape(n_blocks, MOE_BLOCK), block_e))
    y = jax.ops.segment_sum(y.reshape(n_slots, d), slot_tok, num_segments=T)
    return y.reshape(bsz, L, d).astype(u.dtype)


def setup_inputs(seed: int = 0) -> dict:
    key = jax.random.key(seed)
    ks = jax.random.split(key, 32)
    f32 = jnp.float32

    def nrm(k, shape, scale):
        return jax.random.normal(k, shape, f32) * scale

    def gain(k, shape):
        return 1.0 + 0.02 * jax.random.normal(k, shape, f32)

    def dt_bias(k):
        u = jax.random.uniform(k, (DEPTH, SSD_HEADS), f32)
        dt = jnp.exp(u * (math.log(0.1) - math.log(0.001)) + math.log(0.001))
        return dt + jnp.log(-jnp.expm1(-dt))

    def a_log(k):
        return jnp.log(jax.random.uniform(k, (DEPTH, SSD_HEADS), f32, minval=1.0, maxval=16.0))

    return {
        'x': jax.random.normal(ks[0], (BATCH, SEQ, D_MODEL), f32),
        'norm_mix_w': gain(ks[1], (DEPTH, D_MODEL)),
        'w_in': nrm(ks[2], (DEPTH, D_MODEL, D_IN_PROJ), D_MODEL ** -0.5),
        'conv_w': nrm(ks[3], (DEPTH, SSD_CONV, SSD_CONV_CH), SSD_CONV ** -0.5),
        'conv_b': nrm(ks[4], (DEPTH, SSD_CONV_CH), 0.02),
        'dt_bias_fwd': dt_bias(ks[5]),
        'dt_bias_bwd': dt_bias(ks[6]),
        'a_log_fwd': a_log(ks[7]),
        'a_log_bwd': a_log(ks[8]),
        'ssd_d': 1.0 + 0.1 * jax.random.normal(ks[9], (DEPTH, SSD_HEADS), f32),
        'ssd_norm_w': gain(ks[10], (DEPTH, SSD_WIDTH)),
        'lambda_q1': nrm(ks[11], (DEPTH, DA_HEADDIM), 0.1),
        'lambda_k1': nrm(ks[12], (DEPTH, DA_HEADDIM), 0.1),
        'lambda_q2': nrm(ks[13], (DEPTH, DA_HEADDIM), 0.1),
        'lambda_k2': nrm(ks[14], (DEPTH, DA_HEADDIM), 0.1),
        'subln_w': gain(ks[15], (DEPTH, DA_VDIM)),
        'w_out': nrm(ks[16], (DEPTH, D_MIX, D_MODEL), D_MIX ** -0.5),
        'norm_ffn_w': gain(ks[17], (DEPTH, D_MODEL)),
        'w_router_group': nrm(ks[18], (DEPTH, D_MODEL, N_EXPERT_GROUPS), D_MODEL ** -0.5),
        'b_router_group': nrm(ks[19], (DEPTH, N_EXPERT_GROUPS), 0.01),
        'w_router_exp': nrm(ks[20], (DEPTH, D_MODEL, N_EXPERTS), D_MODEL ** -0.5),
        'b_router_exp': nrm(ks[21], (DEPTH, N_EXPERTS), 0.01),
        'w_exp_gate': nrm(ks[22], (DEPTH, N_EXPERTS, D_MODEL, D_EXPERT), D_MODEL ** -0.5),
        'w_exp_up': nrm(ks[23], (DEPTH, N_EXPERTS, D_MODEL, D_EXPERT), D_MODEL ** -0.5),
        'w_exp_down': nrm(ks[24], (DEPTH, N_EXPERTS, D_EXPERT, D_MODEL), D_EXPERT ** -0.5),
        'norm_final_w': gain(ks[25], (D_MODEL,)),
    }


def reference(x, norm_mix_w, w_in, conv_w, conv_b, dt_bias_fwd, dt_bias_bwd, a_log_fwd,
              a_log_bwd, ssd_d, ssd_norm_w, lambda_q1, lambda_k1, lambda_q2, lambda_k2,
              subln_w, w_out, norm_ffn_w, w_router_group, b_router_group, w_router_exp,
              b_router_exp, w_exp_gate, w_exp_up, w_exp_down, norm_final_w):
    splits = [int(s) for s in np.cumsum([COL_Z, COL_XBC, COL_DT, COL_Q, COL_K])]
    h = x
    for l in range(DEPTH):
        lambda_init = 0.8 - 0.6 * math.exp(-0.3 * l)
        u = rmsnorm(h, norm_mix_w[l])
        proj = jnp.einsum('bsd,de->bse', u, w_in[l])
        z, xbc, dt_raw, q, k, v = jnp.split(proj, splits, axis=-1)
        y_ssd = ssd_mixer(z, xbc, dt_raw, conv_w[l], conv_b[l], dt_bias_fwd[l], dt_bias_bwd[l],
                          a_log_fwd[l], a_log_bwd[l], ssd_d[l], ssd_norm_w[l])
        y_da = diff_attention(q, k, v, lambda_q1[l], lambda_k1[l], lambda_q2[l], lambda_k2[l],
                              subln_w[l], lambda_init)
        mix = jnp.concatenate([y_ssd.astype(h.dtype), y_da.astype(h.dtype)], axis=-1)
        h = h + jnp.einsum('bse,ed->bsd', mix, w_out[l])
        h = h + hier_moe(rmsnorm(h, norm_ffn_w[l]), w_router_group[l], b_router_group[l],
                         w_router_exp[l], b_router_exp[l], w_exp_gate[l], w_exp_up[l],
                         w_exp_down[l])
    return rmsnorm(h, norm_final_w)
```

```python
import math
from contextlib import ExitStack
import numpy as np
import ml_dtypes
import concourse.bass as bass
import concourse.mybir as mybir
from concourse.bass_utils import run_bass_kernel_spmd

F32 = mybir.dt.float32
BF16 = mybir.dt.bfloat16
I32 = mybir.dt.int32
ALU = mybir.AluOpType
AF = mybir.ActivationFunctionType
AX = mybir.AxisListType

D = 1024
NE = 32
DE = 512
EPS = 1e-6
LAMBDA_INIT = 0.2
NEGBIG = 30000.0
STRICT_SYNC = True


class R:
    __slots__ = ("w", "rd")

    def __init__(self):
        self.w = None
        self.rd = {}


class Prog:
    ENG = ("pe", "act", "dve", "pool", "sp")

    def __init__(self, nc, n_dma_sems=14):
        self.nc = nc
        self.ops = []
        self.n_dma_sems = n_dma_sems

    def op(self, eng, emit, reads=(), writes=(), dma=False):
        reads = [r for r in reads if r is not None]
        writes = [w for w in writes if w is not None]
        self.ops.append(dict(eng=eng, emit=emit, reads=tuple(reads), writes=tuple(writes),
                             dma=dma, deps=set(), marked=False, barrier=False))

    def barrier(self):
        for e in self.ENG:
            self.ops.append(dict(eng=e, emit=None, reads=(), writes=(), dma=False, deps=set(),
                                 marked=False, barrier=True))

    def _analyze(self):
        ops = self.ops
        last_on_eng = {e: None for e in self.ENG}
        dmas = []
        for i, o in enumerate(ops):
            e = o["eng"]
            if o["barrier"]:
                for e2 in self.ENG:
                    if e2 != e and last_on_eng[e2] is not None:
                        o["deps"].add(last_on_eng[e2])
                o["deps"].update(dmas)
                if e == self.ENG[-1]:
                    dmas = []
                continue
            deps = set()
            for r in o["reads"]:
                if r.w is not None:
                    deps.add(("raw", r.w))
            for w in o["writes"]:
                if w.w is not None:
                    deps.add(("waw", w.w))
                for j in w.rd.values():
                    deps.add(("war", j))
            for kind, j in deps:
                if j == i:
                    continue
                oj = ops[j]
                if oj["eng"] == e and not oj["dma"] and not o["dma"]:
                    if e == "pe" or (kind != "raw" and not STRICT_SYNC):
                        continue
                o["deps"].add(j)
            for r in o["reads"]:
                r.rd[("dma", i) if o["dma"] else e] = i
            for w in o["writes"]:
                w.w = i
                w.rd = {}
            if o["dma"]:
                dmas.append(i)
            else:
                last_on_eng[e] = i
        for o in ops:
            for j in o["deps"]:
                ops[j]["marked"] = True

    def emit(self, stack):
        nc = self.nc
        self._analyze()
        ops = self.ops
        sems = {e: stack.enter_context(nc.semaphore("s_" + e)) for e in self.ENG}
        dq = ("sp", "pool", "act")
        dsem = {e: [stack.enter_context(nc.semaphore("d_%s_%d" % (e, k)))
                    for k in range(self.n_dma_sems)] for e in dq}
        cnt = {e: 0 for e in self.ENG}
        dcount = {e: 0 for e in dq}
        dval = {e: [0] * self.n_dma_sems for e in dq}
        prev_on_sem = {}
        for i, o in enumerate(ops):
            if o["barrier"]:
                continue
            e = o["eng"]
            if o["dma"]:
                k = dcount[e] % self.n_dma_sems
                dcount[e] += 1
                o["prev_same_sem"] = prev_on_sem.get((e, k))
                dval[e][k] += 16
                o["done"] = (dsem[e][k], dval[e][k], ("d", e, k))
                prev_on_sem[(e, k)] = i
            elif o["marked"]:
                cnt[e] += 1
                o["done"] = (sems[e], cnt[e], ("c", e))
        final_dma = [(dsem[e][k], dval[e][k]) for e in dq for k in range(self.n_dma_sems)
                     if dval[e][k] > 0]
        blk = stack.enter_context(nc.Block())
        for e in self.ENG:
            my = [(i, o) for i, o in enumerate(ops) if o["eng"] == e]

            def body(engine, my=my, e=e):
                seen = {}
                for i, o in my:
                    deps = set(o["deps"])
                    if o["dma"] and o.get("prev_same_sem") is not None:
                        deps.add(o["prev_same_sem"])
                    need = {}
                    for j in deps:
                        s, v, key = ops[j]["done"]
                        if seen.get(key, 0) >= v:
                            continue
                        if key not in need or need[key][1] < v:
                            need[key] = (s, v)
                    for key, (s, v) in need.items():
                        engine.wait_ge(s, v)
                        seen[key] = v
                    if o["barrier"]:
                        continue
                    ins = o["emit"]()
                    if "done" in o:
                        ins.then_inc(o["done"][0], 16 if o["dma"] else 1)
                if e == "sp":
                    for s, v in final_dma:
                        engine.wait_ge(s, v)

            getattr(blk, {"pe": "tensor", "act": "scalar", "dve": "vector",
                          "pool": "gpsimd", "sp": "sync"}[e])(body)


CF_IDENT, CF_ONES, CF_TF, CF_TB, CF_UF, CF_UB, CF_MF, CF_MB = range(8)
CF_D2 = 8
CF_MISC = 8 + 16
NCF = (8 + 16) * 128 + 16


def make_consts(L, BS):
    t = np.arange(128)
    a = t[:, None]
    b = t[None, :]
    cf = np.zeros((128, NCF), np.float32)

    def put(k, m):
        cf[:, k * 128:(k + 1) * 128] = m
    put(CF_IDENT, (a == b))
    put(CF_ONES, np.ones((128, 128)))
    put(CF_TF, (a <= b))
    put(CF_TB, (a >= b))
    put(CF_UF, (a > b))
    put(CF_UB, (a < b))
    put(CF_MF, (b >= a))
    put(CF_MB, (b <= a))
    ii = np.arange(512)[None, :]
    for dk in range(4):
        d2 = 2.0 * np.maximum(a + 128 * dk - ii, 0)
        cf[:, (CF_D2 + 4 * dk) * 128:(CF_D2 + 4 * dk + 4) * 128] = d2
    m0 = CF_MISC * 128
    cf[:, m0 + 0] = t * BS
    cf[:, m0 + 1] = t
    cb = np.zeros((128, 4 * 128), np.float32)
    cb[:, 384:512] = ((a < 64) == (b < 64))
    cb[:, 0:128] = (a == b)
    cb[:, 128:256] = 1.0
    cb[:, 256:384] = (a < b)
    cb = cb.astype(ml_dtypes.bfloat16)
    slopes = [2.0 ** (-8.0 * (h + 1) / 4) for h in range(4)]
    pos = np.arange(L)
    ip = pos % 512
    jp = pos % 128
    qrows = np.zeros((4, 3, L), np.float32)
    krA = np.zeros((4, 3, L), np.float32)
    for h in range(4):
        s8 = 8.0 * slopes[h]
        qrows[h, 0] = -s8 * (ip % 256)
        qrows[h, 1] = -s8 * 256 * (ip // 256)
        qrows[h, 2] = 1.0
        krA[h, 0] = 1.0
        krA[h, 1] = 1.0
        krA[h, 2] = s8 * jp
    krB = -krA
    nkt, nqb = L // 128, L // 512
    btab = np.zeros((4, nkt * nqb), np.float32)
    for h in range(4):
        for kt in range(nkt):
            for qb in range(nqb):
                if kt >= 4 * qb + 4:
                    v = -(128 * kt - 512 * qb)
                else:
                    v = -(512 * qb - 128 * kt)
                btab[h, kt * nqb + qb] = slopes[h] * v
    return dict(cf=cf, cb=cb, qrows=qrows.astype(ml_dtypes.bfloat16),
                krA=krA.astype(ml_dtypes.bfloat16), krB=krB.astype(ml_dtypes.bfloat16), btab=btab), slopes


def build(NSEQ, L, BS, dbg=False):
    nc = bass.Bass("TRN2", target_bir_lowering=False)
    T = NSEQ * L
    NT = T // 128
    NC = L // 128
    NQB = L // 512
    NB = (2 * T) // BS + NE
    NSUB = BS // 128
    NSLOT = NB * BS
    assert NB <= 128

    def din(name, shape, dt=F32):
        return nc.dram_tensor(name, list(shape), dt, kind="ExternalInput").ap()

    def dscr(name, shape, dt):
        return nc.dram_tensor(name, list(shape), dt, kind="Internal").ap()

    x = din("x", [NSEQ, L, D])
    w_in = din("w_in", [D, 3088])
    w_out = din("w_out", [D, D])
    wr = din("wr", [D, 36])
    br = din("br", [1, 36])
    wg = din("wg", [NE * 128 * 2, 2048])
    wu = din("wu", [NE * 128 * 2, 2048])
    wd = din("wd", [NE * 512, 1024])
    nmw = din("nmw", [1, D])
    nfw = din("nfw", [1, D])
    nlw = din("nlw", [1, D])
    convw = din("convw", [128, 40])
    convb = din("convb", [128, 8])
    dtb = din("dtb", [1, 16])
    alog = din("alog", [1, 16])
    dsk = din("dsk", [1, 8])
    snw = din("snw", [1, 512])
    lam = din("lam", [4, 64])
    sub_w = din("sub_w", [1, 128])
    cf_d = din("cf", [128, NCF])
    cb_d = din("cb", [128, 512], BF16)
    qrows_d = din("qrows", [4, 3, L], BF16)
    krA_d = din("krA", [4, 3, L], BF16)
    krB_d = din("krB", [4, 3, L], BF16)
    btab_d = din("btab", [4, NC * NQB])
    out = nc.dram_tensor("out", [NSEQ, L, D], F32, kind="ExternalOutput").ap()

    nmax_d = dscr("nmax_d", [128, 16], F32)
    xbc_d = dscr("xbc_d", [D, L + 4], BF16)
    qT_d = dscr("qT_d", [512, L], BF16)
    kT_d = dscr("kT_d", [512, L], BF16)
    v_d = dscr("v_d", [L, 512], BF16)
    z_d = dscr("z_d", [L, 512], F32)
    dt_d = dscr("dt_d", [L, 16], F32)
    mix_d = dscr("mix_d", [L, D], BF16)
    h_d = dscr("h_d", [T, D], F32)
    xn_d = dscr("xn_d", [T, D], BF16)
    xs_d = dscr("xs_d", [NSLOT, D], BF16)
    y_d = dscr("y_d", [NSLOT, D], BF16)
    dbg_d = {}
    if dbg:
        for nm, shp in (("dbg_mix", [NSEQ * L, D]), ("dbg_h", [T, D]), ("dbg_route", [128, NT * 8])):
            dbg_d[nm] = nc.dram_tensor(nm, shp, F32, kind="ExternalOutput").ap()

    P = Prog(nc)
    top = ExitStack()
    with top:
        uid = [0]

        def SB(st, name, shape, dt=F32):
            uid[0] += 1
            return st.enter_context(nc.sbuf_tensor("sb_%s_%d" % (name, uid[0]), list(shape), dt))

        def PS(st, name, shape, dt=F32):
            uid[0] += 1
            return st.enter_context(nc.psum_tensor("ps_%s_%d" % (name, uid[0]), list(shape), dt))

        class MB:
            def __init__(self, st, name, shape, dt=F32, n=2, psum=False):
                mk = PS if psum else SB
                self.t = [mk(st, "%s%d" % (name, i), shape, dt) for i in range(n)]
                self.r = [R() for _ in range(n)]
                self.n = n
                self.k = -1

            def nxt(self):
                self.k += 1
                i = self.k % self.n
                return self.t[i], self.r[i]

        rr = {"ev": 0}

        def evac(out_ap, in_ap, reads, writes, eng=None):
            if eng is None:
                eng = ("act", "dve")[rr["ev"] % 2]
                rr["ev"] += 1
            if eng == "act":
                P.op("act", lambda: nc.scalar.copy(out=out_ap, in_=in_ap), reads=reads, writes=writes)
            else:
                P.op("dve", lambda: nc.vector.tensor_copy(out=out_ap, in_=in_ap), reads=reads, writes=writes)

        def dma(eng, out_ap, in_ap, reads=(), writes=()):
            q = {"sp": nc.sync, "pool": nc.gpsimd, "act": nc.scalar}[eng]
            P.op(eng, lambda: q.dma_start(out=out_ap, in_=in_ap), reads=reads, writes=writes, dma=True)

        cf = SB(top, "cf", [128, NCF]); r_c = R()
        cb = SB(top, "cb", [128, 512], BF16)
        dma("sp", cf[:], cf_d, writes=[r_c])
        dma("sp", cb[:], cb_d, writes=[r_c])

        def CF(k, n=1):
            return cf[:, k * 128:(k + n) * 128]
        ident_f = CF(CF_IDENT)
        ones_f = CF(CF_ONES)
        ident_b = cb[:, 0:128]
        ones_b = cb[:, 128:256]
        tstrict_b = cb[:, 256:384]
        bones_c = cb[:, 384:512]
        eps_t = SB(top, "eps_t", [128, 1])
        P.op("pool", lambda: nc.gpsimd.memset(eps_t[:], EPS), writes=[r_c])
        neghalf = SB(top, "neghalf", [128, 1])
        P.op("pool", lambda: nc.gpsimd.memset(neghalf[:], -0.5), writes=[r_c])
        poshalf = SB(top, "poshalf", [128, 1])
        P.op("pool", lambda: nc.gpsimd.memset(poshalf[:], 0.5), writes=[r_c])
        zero_bf = SB(top, "zero_bf", [128, 2048], BF16)
        if dbg:
            pass
        P.op("pool", lambda: nc.gpsimd.memset(zero_bf[:], 0.0), writes=[r_c])
        xs_v = xs_d.rearrange("(a p) d -> p a d", p=128)
        r_xs = None
        na = NSLOT // 128
        r_xbc = None
        dma("sp", xbc_d[:, 0:2].rearrange("(a p) d -> p a d", p=128), zero_bf[:, 0:16].rearrange("p (a d) -> p a d", d=2),
            reads=[r_c], writes=[r_xbc])
        dma("sp", xbc_d[:, L + 2:L + 4].rearrange("(a p) d -> p a d", p=128), zero_bf[:, 0:16].rearrange("p (a d) -> p a d", d=2),
            reads=[r_c], writes=[r_xbc])


        oh1_all = SB(top, "oh1_all", [128, NT, NE], BF16)
        oh2_all = SB(top, "oh2_all", [128, NT, NE], BF16)
        rank_all = SB(top, "rank_all", [128, NT, NE])
        g_all = SB(top, "g_all", [128, NT, 2])
        carry = SB(top, "carry", [128, NE])
        slot_i = SB(top, "slot_i", [128, NT, 2], I32)
        idx_g = SB(top, "idx_g", [128, NB, 2], I32)
        idx_d = SB(top, "idx_d", [128, NB, 4], I32)
        r_route = R()
        P.op("pool", lambda: nc.gpsimd.memset(carry[:], 0.0), writes=[r_route])

        r_q = r_k = r_v = r_z = r_dt = r_mix = None
        r_h = r_xn = r_y = None

        def rms_rstd(st_name, src_ap, ss, rstd, sq_scr, r_src, r_s, n):
            P.op("act", lambda: nc.scalar.activation(out=sq_scr, in_=src_ap, func=AF.Square, accum_out=ss),
                 reads=[r_src], writes=[r_s])
            P.op("act", lambda: nc.scalar.activation(out=rstd, in_=ss, func=AF.Ln, bias=eps_t[:, 0:1],
                                                     scale=1.0 / n), reads=[r_s, r_c], writes=[r_s])
            P.op("act", lambda: nc.scalar.activation(out=rstd, in_=rstd, func=AF.Exp, scale=-0.5),
                 reads=[r_s], writes=[r_s])

        def seq_body(s):
            P.barrier()
            with ExitStack() as st:
                win = SB(st, "win", [128, 8, 3088], BF16); r_win = R()
                nmw_t = SB(st, "nmw_t", [128, D])
                dma("sp", nmw_t[:], nmw.partition_broadcast(128), writes=[r_win])
                wv = w_in.rearrange("(kc p) f -> p kc f", p=128)
                for c0 in range(0, 3088, 512):
                    c1 = min(3088, c0 + 512)
                    dma("pool", win[:, :, c0:c1], wv[:, :, c0:c1], writes=[r_win])
                xt = MB(st, "xt", [128, D], F32, 4)
                sq = SB(st, "sq", [128, D]); r_sq = R()
                stat = MB(st, "stat", [128, 2], F32, 4)
                ub = MB(st, "ub", [128, D], BF16, 4)
                uT = MB(st, "uT", [128, 8, 512], BF16, 2)
                pT = MB(st, "pT", [128, 8, 128], BF16, 2, psum=True)
                pA = MB(st, "pA", [128, 512], F32, 3, psum=True)
                pB = MB(st, "pB", [128, 512], F32, 2, psum=True)
                zs = MB(st, "zs", [128, 512], F32, 2)
                vs = MB(st, "vs", [128, 512], BF16, 2)
                ds = MB(st, "ds", [128, 16], F32, 2)
                fs = MB(st, "fs", [128, 512], BF16, 3)
                sqA = MB(st, "sqA", [128, 512], BF16, 5)
                pNA = MB(st, "pNA", [128, 512], F32, 1, psum=True)
                nmax = SB(st, "nmax", [128, 16]); nmt = SB(st, "nmt", [128, 1]); r_nm = R()
                P.op("pool", lambda: nc.gpsimd.memset(nmax[:], 0.0), writes=[r_nm])
                def prepA1(tb):
                    ubs = []
                    for j in range(4):
                        t0 = tb * 512 + j * 128
                        xtt, r_xt = xt.nxt()
                        dma("sp", xtt[:], x[s, t0:t0 + 128, :], writes=[r_xt])
                        stt, r_st = stat.nxt()
                        rms_rstd("a", xtt[:], stt[:, 0:1], stt[:, 1:2], sq[:], r_xt, r_st, D)
                        ubt, r_ub = ub.nxt()
                        P.op("dve", lambda ubt=ubt, xtt=xtt, stt=stt: nc.vector.scalar_tensor_tensor(
                            out=ubt[:], in0=xtt[:], scalar=stt[:, 1:2], in1=nmw_t[:], op0=ALU.mult, op1=ALU.mult),
                            reads=[r_xt, r_st, r_win], writes=[r_ub])
                        ubs.append((ubt, r_ub))
                    return ubs

                def prepA2(ubs):
                    uTt, r_uT = uT.nxt()
                    for j in range(4):
                        ubt, r_ub = ubs[j]
                        pTt, r_pT = pT.nxt()
                        for k in range(8):
                            P.op("pe", lambda k=k, pTt=pTt, ubt=ubt: nc.tensor.transpose(
                                out=pTt[:, k, :], in_=ubt[:, k * 128:(k + 1) * 128], identity=ident_b),
                                reads=[r_ub, r_c], writes=[r_pT])
                        evac(uTt[:, :, j * 128:(j + 1) * 128], pTt[:], [r_pT], [r_uT])
                    return uTt, r_uT

                pend_bm = []

                def emit_bm(sqb, r_sqb, cc):
                    pn, r_pn = pNA.nxt()
                    P.op("pe", lambda: nc.tensor.matmul(pn[:], lhsT=bones_c, rhs=sqb[:], start=True, stop=True),
                         reads=[r_sqb, r_c], writes=[r_pn])
                    P.op("dve", lambda: nc.vector.reduce_max(out=nmt[:, 0:1], in_=pn[:], axis=AX.X),
                         reads=[r_pn], writes=[r_nm])
                    P.op("dve", lambda: nc.vector.tensor_tensor(out=nmax[:, cc - 8:cc - 7], in0=nmax[:, cc - 8:cc - 7],
                                                                in1=nmt[:, 0:1], op=ALU.max), reads=[r_nm], writes=[r_nm])

                pendA = prepA2(prepA1(0))
                NTB = L // 512
                for tb in range(NTB):
                    uTt, r_uT = pendA
                    ubs_n = prepA1(tb + 1) if tb + 1 < NTB else None
                    for j in range(4):
                        t0 = tb * 512 + j * 128
                        for (c0, n, kind) in ((0, 512, "z"), (1536, 16, "dt"), (2576, 512, "v")):
                            pt, r_p = pA.nxt()
                            for k in range(8):
                                P.op("pe", lambda k=k, pt=pt, c0=c0, n=n, j=j, uTt=uTt: nc.tensor.matmul(
                                    pt[:, 0:n], lhsT=uTt[:, k, j * 128:(j + 1) * 128], rhs=win[:, k, c0:c0 + n],
                                    start=(k == 0), stop=(k == 7)), reads=[r_uT, r_win], writes=[r_p])
                            if kind == "z":
                                o, r_o = zs.nxt()
                                evac(o[:], pt[:], [r_p], [r_o])
                                dma("sp", z_d[t0:t0 + 128, :], o[:], reads=[r_o], writes=[r_z])
                            elif kind == "v":
                                o, r_o = vs.nxt()
                                evac(o[:], pt[:], [r_p], [r_o])
                                dma("sp", v_d[t0:t0 + 128, :], o[:], reads=[r_o], writes=[r_v])
                            else:
                                o, r_o = ds.nxt()
                                evac(o[:], pt[:, 0:16], [r_p], [r_o])
                                dma("sp", dt_d[t0:t0 + 128, :], o[:], reads=[r_o], writes=[r_dt])
                    for cc in range(16):
                        c0 = 512 + cc * 128 if cc < 8 else (1552 + (cc - 8) * 128)
                        pt, r_p = pB.nxt()
                        for k in range(8):
                            P.op("pe", lambda k=k, pt=pt, c0=c0, uTt=uTt: nc.tensor.matmul(
                                pt[:], lhsT=win[:, k, c0:c0 + 128], rhs=uTt[:, k, :],
                                start=(k == 0), stop=(k == 7)), reads=[r_uT, r_win], writes=[r_p])
                        if len(pend_bm) > 2:
                            emit_bm(*pend_bm.pop(0))
                        o, r_o = fs.nxt()
                        evac(o[:], pt[:], [r_p], [r_o])
                        if cc >= 8:
                            sqb, r_sqb = sqA.nxt()
                            P.op("pool", lambda sqb=sqb, o=o: nc.gpsimd.tensor_tensor(out=sqb[:], in0=o[:], in1=o[:], op=ALU.mult),
                                 reads=[r_o], writes=[r_sqb])
                            pend_bm.append((sqb, r_sqb, cc))
                        if cc < 8:
                            dma("sp", xbc_d[cc * 128:(cc + 1) * 128, 2 + tb * 512:2 + (tb + 1) * 512], o[:],
                                reads=[r_o], writes=[r_xbc])
                        elif cc < 12:
                            dma("sp", qT_d[(cc - 8) * 128:(cc - 7) * 128, tb * 512:(tb + 1) * 512], o[:],
                                reads=[r_o], writes=[r_q])
                        else:
                            dma("sp", kT_d[(cc - 12) * 128:(cc - 11) * 128, tb * 512:(tb + 1) * 512], o[:],
                                reads=[r_o], writes=[r_k])
                    if ubs_n is not None:
                        pendA = prepA2(ubs_n)
                while pend_bm:
                    emit_bm(*pend_bm.pop(0))
                dma("sp", nmax_d, nmax[:], reads=[r_nm])

            P.barrier()
            with ExitStack() as st:
                xs_tm = SB(st, "xs_tm", [128, NC, 512], BF16)
                B_tm = SB(st, "B_tm", [128, NC, 256], BF16)
                BT = SB(st, "BT", [128, 2, L], BF16)
                CT = SB(st, "CT", [128, 2, L], BF16)
                r_prep = R()
                cw = SB(st, "cw", [128, 8, 5]); cbias = SB(st, "cbias", [128, 8, 1]); r_cw = R()
                dma("sp", cw[:], convw.rearrange("p (cc k) -> p cc k", k=5), writes=[r_cw])
                dma("sp", cbias[:], convb.rearrange("p (cc k) -> p cc k", k=1), writes=[r_cw])
                dtr = SB(st, "dtr", [128, NC, 16]); r_dtr = R()
                dtt = SB(st, "dtt", [128, NC, 16])
                adt = SB(st, "adt", [128, NC, 16])
                cs = SB(st, "cs", [128, NC, 16])
                ecs = SB(st, "ecs", [128, NC, 16])
                dte = SB(st, "dte", [128, NC, 16])
                cd = SB(st, "cd", [128, NC, 16])
                tmp1 = SB(st, "tmp1", [128, NC, 16]); tmp2 = SB(st, "tmp2", [128, NC, 16])
                dtb_t = SB(st, "dtb_t", [128, 16]); a_t = SB(st, "a_t", [128, 16]); dsk_t = SB(st, "dsk_t", [128, 8])
                snw_t = SB(st, "snw_t", [128, 512])
                r_dtp = R()
                dma("sp", dtr[:], dt_d.rearrange("(c p) h -> p c h", p=128), reads=[r_dt], writes=[r_dtr])
                dma("sp", dtb_t[:], dtb.partition_broadcast(128), writes=[r_dtp])
                dma("sp", a_t[:], alog.partition_broadcast(128), writes=[r_dtp])
                dma("sp", dsk_t[:], dsk.partition_broadcast(128), writes=[r_dtp])
                dma("sp", snw_t[:], snw.partition_broadcast(128), writes=[r_dtp])
                bc16 = lambda t: t[:].unsqueeze(1).to_broadcast([128, NC, 16])
                P.op("act", lambda: nc.scalar.activation(out=a_t[:], in_=a_t[:], func=AF.Exp), reads=[r_dtp], writes=[r_dtp])
                P.op("dve", lambda: nc.vector.tensor_scalar(out=a_t[:], in0=a_t[:], scalar1=-1.0, scalar2=None, op0=ALU.mult),
                     reads=[r_dtp], writes=[r_dtp])
                P.op("dve", lambda: nc.vector.tensor_tensor(out=dtr[:], in0=dtr[:], in1=bc16(dtb_t), op=ALU.add),
                     reads=[r_dtr, r_dtp], writes=[r_dtr])
                P.op("dve", lambda: nc.vector.tensor_scalar(out=tmp1[:], in0=dtr[:], scalar1=30.0, scalar2=None, op0=ALU.min),
                     reads=[r_dtr], writes=[r_dtp])
                P.op("act", lambda: nc.scalar.activation(out=tmp1[:], in_=tmp1[:], func=AF.Exp),
                     reads=[r_dtp], writes=[r_dtp])
                P.op("dve", lambda: nc.vector.tensor_scalar(out=tmp1[:], in0=tmp1[:], scalar1=1.0, scalar2=None, op0=ALU.add),
                     reads=[r_dtp], writes=[r_dtp])
                P.op("act", lambda: nc.scalar.activation(out=tmp1[:], in_=tmp1[:], func=AF.Ln),
                     reads=[r_dtp], writes=[r_dtp])
                P.op("dve", lambda: nc.vector.tensor_tensor(out=dtt[:], in0=tmp1[:], in1=dtr[:], op=ALU.max),
                     reads=[r_dtp, r_dtr], writes=[r_dtp])
                P.op("dve", lambda: nc.vector.tensor_tensor(out=adt[:], in0=dtt[:], in1=bc16(a_t), op=ALU.mult),
                     reads=[r_dtp], writes=[r_dtp])
                stc = ExitStack()
                pC = MB(stc, "pC", [128, 512], F32, 2, psum=True)
                adt_f = adt[:].rearrange("p c h -> p (c h)")
                ncol = NC * 16
                for (lh, dst, lo, hi) in ((CF(CF_TF), cs, 0, 8), (CF(CF_TB), cs, 8, 16), (ones_f, tmp1, 0, 16)):
                    for c0 in range(0, ncol, 512):
                        c1 = min(ncol, c0 + 512)
                        pt, r_p = pC.nxt()
                        P.op("pe", lambda pt=pt, lh=lh, c0=c0, c1=c1: nc.tensor.matmul(
                            pt[:, 0:c1 - c0], lhsT=lh, rhs=adt_f[:, c0:c1], start=True, stop=True),
                            reads=[r_dtp, r_c], writes=[r_p])
                        ca, cb_ = c0 // 16, c1 // 16
                        evac(dst[:, ca:cb_, lo:hi], pt[:, 0:c1 - c0].rearrange("p (c h) -> p c h", h=16)[:, :, lo:hi],
                             [r_p], [r_dtp], eng="dve")
                P.op("act", lambda: nc.scalar.activation(out=ecs[:], in_=cs[:], func=AF.Exp), reads=[r_dtp], writes=[r_dtp])
                P.op("dve", lambda: nc.vector.tensor_tensor(out=tmp2[:], in0=tmp1[:], in1=cs[:], op=ALU.subtract),
                     reads=[r_dtp], writes=[r_dtp])
                P.op("act", lambda: nc.scalar.activation(out=dte[:], in_=tmp2[:], func=AF.Exp), reads=[r_dtp], writes=[r_dtp])
                P.op("act", lambda: nc.scalar.activation(out=cd[:], in_=tmp1[:], func=AF.Exp), reads=[r_dtp], writes=[r_dtp])

                with ExitStack() as st2:
                    raw = MB(st2, "raw", [128, L + 4], BF16, 2)
                    xsT = MB(st2, "xsT", [128, L], BF16, 2)
                    pTr = MB(st2, "pTr", [128, 4, 128], BF16, 2, psum=True)
                    pcv = MB(st2, "pcv", [128, 512], F32, 2, psum=True)
                    dgw = SB(st2, "dgw", [128, 8, 5, 128], BF16); r_dg = R()
                    for cc in range(8):
                        for k in range(5):
                            P.op("dve", lambda cc=cc, k=k: nc.vector.tensor_scalar(
                                out=dgw[:, cc, k, :], in0=ident_b, scalar1=cw[:, cc, k:k + 1], scalar2=None, op0=ALU.mult),
                                reads=[r_cw, r_c], writes=[r_dg])
                    for cc in range(8):
                        rw, r_rw = raw.nxt()
                        dma("sp", rw[:], xbc_d[cc * 128:(cc + 1) * 128, :], reads=[r_xbc], writes=[r_rw])
                        if cc < 4:
                            dst, r_d = xsT.nxt()
                            dstap = dst[:]
                        elif cc < 6:
                            dstap, r_d = BT[:, cc - 4, :], r_prep
                        else:
                            dstap, r_d = CT[:, cc - 6, :], r_prep
                        for tbk in range(L // 512):
                            o0 = tbk * 512
                            pc, r_pc = pcv.nxt()
                            for k in range(5):
                                P.op("pe", lambda pc=pc, rw=rw, cc=cc, k=k, o0=o0: nc.tensor.matmul(
                                    pc[:], lhsT=dgw[:, cc, k, :], rhs=rw[:, o0 + k:o0 + k + 512], start=(k == 0), stop=(k == 4)),
                                    reads=[r_rw, r_dg], writes=[r_pc])
                            P.op("act", lambda pc=pc, dstap=dstap, o0=o0, cc=cc: nc.scalar.activation(
                                out=dstap[:, o0:o0 + 512], in_=pc[:], func=AF.Silu, bias=cbias[:, cc, 0:1]),
                                reads=[r_pc, r_cw], writes=[r_d])
                        if cc < 6:
                            for c0 in range(0, NC, 4):
                                pt, r_p = pTr.nxt()
                                for i in range(4):
                                    P.op("pe", lambda pt=pt, i=i, c0=c0, dstap=dstap: nc.tensor.transpose(
                                        out=pt[:, i, :], in_=dstap[:, (c0 + i) * 128:(c0 + i + 1) * 128],
                                        identity=ident_b), reads=[r_d, r_c], writes=[r_p])
                                if cc < 4:
                                    evac(xs_tm[:, c0:c0 + 4, cc * 128:(cc + 1) * 128], pt[:], [r_p], [r_prep])
                                else:
                                    evac(B_tm[:, c0:c0 + 4, (cc - 4) * 128:(cc - 3) * 128], pt[:], [r_p], [r_prep])

                stc.close()
                P.barrier()
                with ExitStack() as st2:
                    Hb_all = SB(st2, "Hb_all", [128, NC, 2, 256], BF16); r_Hb = R()
                    Hf = [[SB(st2, "Hf%d%d" % (d_, g), [128, 256]) for g in range(2)] for d_ in range(2)]
                    r_H = [[R(), R()], [R(), R()]]
                    Hbf = MB(st2, "Hbf", [128, 256], BF16, 2)
                    Hbf_g = [None, None]
                    xdt = MB(st2, "xdt", [128, 512], BF16, 3)
                    xdte = MB(st2, "xdte", [128, 512], BF16, 3)
                    RH = MB(st2, "RH", [128, 4, 128], F32, 2)
                    Eb = MB(st2, "Eb", [128, 4, 128], BF16, 2)
                    MT = MB(st2, "MT", [128, 4, 128], BF16, 2)
                    cbm = MB(st2, "cbm", [128, 128], BF16, 4)
                    yacc = MB(st2, "yacc", [128, 512], F32, 2)
                    ytmp = MB(st2, "ytmp", [128, 256], F32, 2)
                    zt = MB(st2, "zt", [128, 512], F32, 3)
                    gsc = SB(st2, "gsc", [128, 512]); r_gsc = R()
                    gst = MB(st2, "gst", [128, 2], F32, 2)
                    yo = MB(st2, "yo", [128, 512], BF16, 2)
                    pS = MB(st2, "pS", [128, 256], F32, 2, psum=True)
                    pCB = MB(st2, "pCB", [128, 128], F32, 1, psum=True)
                    pSeg = MB(st2, "pSeg", [128, 512], F32, 2, psum=True)
                    pY = MB(st2, "pY", [128, 256], F32, 2, psum=True)
                    pYo = MB(st2, "pYo", [128, 256], F32, 1, psum=True)
                    for d_ in range(2):
                        for g in range(2):
                            P.op("pool", lambda d_=d_, g=g: nc.gpsimd.memset(Hf[d_][g][:], 0.0), writes=[r_H[d_][g]])

                    def xdt_ops(c, d_):
                        a, r_a = xdt.nxt()
                        b, r_b = xdte.nxt()
                        h0 = 8 * d_
                        P.op("pool", lambda: nc.gpsimd.tensor_tensor(
                            out=a[:].rearrange("p (h e) -> p h e", h=8),
                            in0=xs_tm[:, c, :].rearrange("p (h e) -> p h e", h=8),
                            in1=dtt[:, c, h0:h0 + 8].unsqueeze(2).to_broadcast([128, 8, 64]), op=ALU.mult),
                            reads=[r_prep, r_dtp], writes=[r_a])
                        P.op("pool", lambda: nc.gpsimd.tensor_tensor(
                            out=b[:].rearrange("p (h e) -> p h e", h=8),
                            in0=a[:].rearrange("p (h e) -> p h e", h=8),
                            in1=dte[:, c, h0:h0 + 8].unsqueeze(2).to_broadcast([128, 8, 64]), op=ALU.mult),
                            reads=[r_a, r_dtp], writes=[r_b])
                        return (a, r_a), (b, r_b)

                    def state_update(c, d_, g, xe, r_xe):
                        pt, r_p = pS.nxt()
                        P.op("pe", lambda: nc.tensor.matmul(pt[:], lhsT=B_tm[:, c, g * 128:(g + 1) * 128],
                                                            rhs=xe[:, g * 256:(g + 1) * 256], start=True, stop=True),
                             reads=[r_prep, r_xe], writes=[r_p])
                        h0 = 8 * d_ + 4 * g
                        H = Hf[d_][g]
                        P.op("dve", lambda: nc.vector.tensor_tensor(
                            out=H[:].rearrange("p (h e) -> p h e", h=4), in0=H[:].rearrange("p (h e) -> p h e", h=4),
                            in1=cd[:, c, h0:h0 + 4].unsqueeze(2).to_broadcast([128, 4, 64]), op=ALU.mult),
                            reads=[r_H[d_][g], r_dtp], writes=[r_H[d_][g]])
                        P.op("dve", lambda: nc.vector.tensor_tensor(out=H[:], in0=H[:], in1=pt[:], op=ALU.add),
                             reads=[r_H[d_][g], r_p], writes=[r_H[d_][g]])

                    for c in range(NC - 1, -1, -1):
                        for g in range(2):
                            P.op("act", lambda c=c, g=g: nc.scalar.copy(out=Hb_all[:, c, g, :], in_=Hf[1][g][:]),
                                 reads=[r_H[1][g]], writes=[r_Hb])
                        if c > 0:
                            (_, _), (xe, r_xe) = xdt_ops(c, 1)
                            for g in range(2):
                                state_update(c, 1, g, xe, r_xe)

                    zq = []

                    def load_z(c):
                        z, r_zt = zt.nxt()
                        dma("sp", z[:], z_d[c * 128:(c + 1) * 128, :], reads=[r_z], writes=[r_zt])
                        zq.append((z, r_zt))
                    load_z(0)
                    for c in range(NC):
                        if c + 1 < NC:
                            load_z(c + 1)
                        ya, r_ya = yacc.nxt()
                        P.op("dve", lambda ya=ya, c=c: nc.vector.tensor_tensor(
                            out=ya[:].rearrange("p (h e) -> p h e", h=8),
                            in0=xs_tm[:, c, :].rearrange("p (h e) -> p h e", h=8),
                            in1=dsk_t[:].unsqueeze(2).to_broadcast([128, 8, 64]), op=ALU.mult),
                            reads=[r_prep, r_dtp], writes=[r_ya])
                        xd = [None, None]
                        for d_ in range(2):
                            xd[d_] = xdt_ops(c, d_)
                        def run_combos(c, ya, r_ya, xd):
                            pre = {}

                            def preG(g):
                                pcb, r_pcb = pCB.nxt()
                                P.op("pe", lambda: nc.tensor.matmul(
                                    pcb[:], lhsT=BT[:, g, c * 128:(c + 1) * 128], rhs=CT[:, g, c * 128:(c + 1) * 128],
                                    start=True, stop=True), reads=[r_prep], writes=[r_pcb])
                                cms = []
                                for d_ in range(2):
                                    cm, r_cm = cbm.nxt()
                                    P.op("dve", lambda cm=cm, d_=d_: nc.vector.tensor_tensor(
                                        out=cm[:], in0=pcb[:], in1=CF(CF_MF + d_), op=ALU.mult),
                                        reads=[r_pcb, r_c], writes=[r_cm])
                                    cms.append((cm, r_cm))
                                hb, r_hb = Hbf.nxt()
                                P.op("act", lambda: nc.scalar.copy(out=hb[:], in_=Hf[0][g][:]),
                                     reads=[r_H[0][g]], writes=[r_hb])
                                pre[g] = (cms, hb, r_hb)

                            cst = {}

                            def stX(g, d_):
                                h0 = 8 * d_ + 4 * g
                                rh, r_rh = RH.nxt()
                                P.op("pool", lambda: nc.gpsimd.tensor_tensor(
                                    out=rh[:], in0=CF(CF_TF + d_).unsqueeze(1).to_broadcast([128, 4, 128]),
                                    in1=adt[:, c, h0:h0 + 4].unsqueeze(2).to_broadcast([128, 4, 128]), op=ALU.mult),
                                    reads=[r_c, r_dtp], writes=[r_rh])
                                cst[(g, d_)] = dict(rh=(rh, r_rh))

                            def stY(g, d_):
                                rh, r_rh = cst[(g, d_)]["rh"]
                                psg, r_psg = pSeg.nxt()
                                P.op("pe", lambda: nc.tensor.matmul(
                                    psg[:], lhsT=CF(CF_UF + d_), rhs=rh[:].rearrange("p h l -> p (h l)"),
                                    start=True, stop=True), reads=[r_rh, r_c], writes=[r_psg])
                                eb, r_eb = Eb.nxt()
                                P.op("act", lambda: nc.scalar.activation(
                                    out=eb[:].rearrange("p h l -> p (h l)"), in_=psg[:], func=AF.Exp),
                                    reads=[r_psg], writes=[r_eb])
                                mt, r_mt = MT.nxt()
                                cm, r_cm = pre[g][0][d_]
                                P.op("dve", lambda: nc.vector.tensor_tensor(
                                    out=mt[:], in0=eb[:], in1=cm[:].unsqueeze(1).to_broadcast([128, 4, 128]),
                                    op=ALU.mult), reads=[r_eb, r_cm], writes=[r_mt])
                                cst[(g, d_)]["mt"] = (mt, r_mt)

                            def stZ(g, d_):
                                h0 = 8 * d_ + 4 * g
                                mt, r_mt = cst[(g, d_)]["mt"]
                                (xa, r_xa), (xe, r_xe) = xd[d_]
                                _, hb, r_hb = pre[g]
                                py, r_py = pY.nxt()
                                for r_ in range(4):
                                    hh = 4 * g + r_
                                    P.op("pe", lambda r_=r_, hh=hh: nc.tensor.matmul(
                                        py[:, r_ * 64:(r_ + 1) * 64], lhsT=mt[:, r_, :], rhs=xa[:, hh * 64:(hh + 1) * 64],
                                        start=True, stop=True), reads=[r_mt, r_xa], writes=[r_py])
                                pyo, r_pyo = pYo.nxt()
                                if d_ == 0:
                                    rhs_h, r_rhs = hb[:], r_hb
                                else:
                                    rhs_h, r_rhs = Hb_all[:, c, g, :], r_Hb
                                P.op("pe", lambda: nc.tensor.matmul(
                                    pyo[:], lhsT=CT[:, g, c * 128:(c + 1) * 128], rhs=rhs_h, start=True, stop=True),
                                    reads=[r_prep, r_rhs], writes=[r_pyo])
                                yt, r_yt = ytmp.nxt()
                                P.op("dve", lambda: nc.vector.tensor_tensor(
                                    out=yt[:].rearrange("p (h e) -> p h e", h=4),
                                    in0=pyo[:].rearrange("p (h e) -> p h e", h=4),
                                    in1=ecs[:, c, h0:h0 + 4].unsqueeze(2).to_broadcast([128, 4, 64]), op=ALU.mult),
                                    reads=[r_pyo, r_dtp], writes=[r_yt])
                                yg = ya[:, g * 256:(g + 1) * 256]
                                P.op("dve", lambda: nc.vector.tensor_tensor(out=yg, in0=yg, in1=yt[:], op=ALU.add),
                                     reads=[r_yt, r_ya], writes=[r_ya])
                                P.op("dve", lambda: nc.vector.tensor_tensor(out=yg, in0=yg, in1=py[:], op=ALU.add),
                                     reads=[r_py, r_ya], writes=[r_ya])

                            preG(0)
                            preG(1)
                            K4 = [(0, 0), (0, 1), (1, 0), (1, 1)]
                            stX(*K4[0]); stX(*K4[1]); stY(*K4[0]); stX(*K4[2]); stY(*K4[1]); stZ(*K4[0])
                            stX(*K4[3]); stY(*K4[2]); stZ(*K4[1]); stY(*K4[3]); stZ(*K4[2]); stZ(*K4[3])
                            if c < NC - 1:
                                for g in range(2):
                                    state_update(c, 0, g, xd[0][1][0], xd[0][1][1])

                        run_combos(c, ya, r_ya, xd)
                        z, r_zt = zq.pop(0)
                        P.op("act", lambda z=z: nc.scalar.activation(out=z[:], in_=z[:], func=AF.Silu), reads=[r_zt], writes=[r_zt])
                        P.op("dve", lambda ya=ya, z=z: nc.vector.tensor_tensor(out=ya[:], in0=ya[:], in1=z[:], op=ALU.mult),
                             reads=[r_zt, r_ya], writes=[r_ya])
                        gs, r_gs = gst.nxt()
                        rms_rstd("g", ya[:], gs[:, 0:1], gs[:, 1:2], gsc[:], r_ya, r_gs, 512)
                        yob, r_yo = yo.nxt()
                        P.op("dve", lambda yob=yob, ya=ya, gs=gs: nc.vector.scalar_tensor_tensor(
                            out=yob[:], in0=ya[:], scalar=gs[:, 1:2], in1=snw_t[:], op0=ALU.mult, op1=ALU.mult),
                            reads=[r_ya, r_gs, r_dtp], writes=[r_yo])
                        dma("sp", mix_d[c * 128:(c + 1) * 128, 0:512], yob[:], reads=[r_yo], writes=[r_mix])

            P.barrier()
            with ExitStack() as st:
                QA = [MB(st, "QA%d" % c_, [67, L], BF16, 2) for c_ in range(2)]
                KAa = [MB(st, "KAa%d" % c_, [67, L], BF16, 2) for c_ in range(2)]
                KAb = [MB(st, "KAb%d" % c_, [67, L], BF16, 2) for c_ in range(2)]
                VA = MB(st, "VA", [128, NC, 129], BF16, 2)
                btab_t = MB(st, "btab_t", [128, NC * NQB], F32, 2)
                bias_t = [MB(st, "bias_t%d" % c_, [128, NC * NQB], F32, 2) for c_ in range(2)]
                lam_t = SB(st, "lam_t", [128, 4, 64]); r_lam = R()
                lam_s = SB(st, "lam_s", [128, 4])
                subw_t = SB(st, "subw_t", [128, 128])
                nrmb = MB(st, "nrmb", [128, 2, 16], F32, 2)
                nrm = SB(st, "nrm", [128, 8]); r_nrm = R()
                Mbc = MB(st, "Mbc", [128, 2], F32, 2)
                mdiag = SB(st, "mdiag", [128, 2])
                PT = [MB(st, "PT%d" % c_, [128, 512], BF16, 3) for c_ in range(2)]
                osq2 = MB(st, "osq2", [128, 128], F32, 2)
                Sfix = MB(st, "Sfix", [128, 512], F32, 2)
                pSc = [MB(st, "pSc%d" % c_, [128, 512], F32, 2, psum=True) for c_ in range(2)]
                pO = MB(st, "pO", [128, 2, 256], F32, 4, psum=True)
                osb = MB(st, "osb", [128, 2, 129], F32, 5)
                ot = MB(st, "ot", [128, 128], F32, 2)
                ost = MB(st, "ost", [128, 4], F32, 8)
                osq = SB(st, "osq", [128, 128]); r_osq = R()
                ob = MB(st, "ob", [128, 128], BF16, 5)
                zf_list = list(range(0, na, 2)) if s == 0 else []
                zf_per = -(-len(zf_list) // (4 * NQB)) if zf_list else 0
                dma("sp", lam_t[:], lam.rearrange("a d -> (a d)").partition_broadcast(128), writes=[r_lam])
                dma("sp", subw_t[:], sub_w.partition_broadcast(128), writes=[r_lam])
                P.op("dve", lambda: nc.vector.tensor_tensor(out=lam_t[:, 0:2, :], in0=lam_t[:, 0:2, :], in1=lam_t[:, 2:4, :], op=ALU.mult),
                     reads=[r_lam], writes=[r_lam])
                P.op("dve", lambda: nc.vector.reduce_sum(out=lam_s[:, 0:2], in_=lam_t[:, 0:2, :], axis=AX.X), reads=[r_lam], writes=[r_lam])
                P.op("act", lambda: nc.scalar.activation(out=lam_s[:, 0:2], in_=lam_s[:, 0:2], func=AF.Exp), reads=[r_lam], writes=[r_lam])
                P.op("dve", lambda: nc.vector.tensor_tensor(out=lam_s[:, 2:3], in0=lam_s[:, 1:2], in1=lam_s[:, 0:1], op=ALU.subtract),
                     reads=[r_lam], writes=[r_lam])
                P.op("dve", lambda: nc.vector.tensor_scalar(out=lam_s[:, 2:3], in0=lam_s[:, 2:3], scalar1=-LAMBDA_INIT, scalar2=None, op0=ALU.add),
                     reads=[r_lam], writes=[r_lam])
                P.op("dve", lambda: nc.vector.tensor_scalar(out=subw_t[:], in0=subw_t[:], scalar1=1.0 - LAMBDA_INIT, scalar2=None, op0=ALU.mult),
                     reads=[r_lam], writes=[r_lam])
                pN = pSc[0]
                hctx = {}

                def setup_loads(h):
                    qa, ka, kb = [], [], []
                    for c_ in range(2):
                        t_, r_ = QA[c_].nxt(); qa.append((t_, r_))
                        row0 = h * 128 + c_ * 64
                        dma("sp", t_[0:64, :], qT_d[row0:row0 + 64, :], reads=[r_q], writes=[r_])
                        dma("sp", t_[64:67, :], qrows_d[h], writes=[r_])
                        t_, r_ = KAa[c_].nxt(); ka.append((t_, r_))
                        dma("sp", t_[0:64, :], kT_d[row0:row0 + 64, :], reads=[r_k], writes=[r_])
                        dma("sp", t_[64:67, :], krA_d[h], writes=[r_])
                        t_, r_ = KAb[c_].nxt(); kb.append((t_, r_))
                        dma("sp", t_[0:64, :], kT_d[row0:row0 + 64, :], reads=[r_k], writes=[r_])
                        dma("sp", t_[64:67, :], krB_d[h], writes=[r_])
                    va, r_va = VA.nxt()
                    dma("sp", va[:, :, 0:128], v_d[:, h * 128:(h + 1) * 128].rearrange("(c p) e -> p c e", p=128),
                        reads=[r_v], writes=[r_va])
                    P.op("pool", lambda va=va: nc.gpsimd.memset(va[:, :, 128:129], 1.0), writes=[r_va])
                    bt, r_bt = btab_t.nxt()
                    dma("sp", bt[:], btab_d[h:h + 1, :].partition_broadcast(128), writes=[r_bt])
                    hpre[h] = dict(qa=qa, ka=ka, kb=kb, va=(va, r_va), bt=(bt, r_bt))

                def setup_final(h):
                    pr = hpre.pop(h)
                    qa, ka, kb, bt, r_bt = pr["qa"], pr["ka"], pr["kb"], pr["bt"][0], pr["bt"][1]
                    nr, r_nr = nrmb.nxt()
                    dma("sp", nr[:, 0, :], nmax_d[0:1, :].partition_broadcast(128), writes=[r_nr])
                    dma("sp", nr[:, 1, :], nmax_d[64:65, :].partition_broadcast(128), writes=[r_nr])
                    mb, r_mb = Mbc.nxt()
                    P.op("dve", lambda: nc.vector.tensor_tensor(out=mb[:], in0=nr[:, :, h], in1=nr[:, :, 4 + h], op=ALU.mult),
                         reads=[r_nr], writes=[r_mb])
                    P.op("pool", lambda: nc.gpsimd.tensor_tensor(out=mb[:], in0=mb[:], in1=poshalf[:, 0:1].to_broadcast([128, 2]), op=ALU.pow),
                         reads=[r_mb, r_c], writes=[r_mb])
                    P.op("dve", lambda: nc.vector.tensor_scalar(out=mb[:], in0=mb[:], scalar1=-0.125 * 1.02, scalar2=None, op0=ALU.mult),
                         reads=[r_mb], writes=[r_mb])
                    bi = []
                    for c_ in range(2):
                        b_, r_b = bias_t[c_].nxt()
                        P.op("dve", lambda b_=b_, c_=c_: nc.vector.tensor_scalar(
                            out=b_[:], in0=bt[:], scalar1=mb[:, c_:c_ + 1], scalar2=None, op0=ALU.add),
                            reads=[r_bt, r_mb], writes=[r_b])
                        bi.append((b_, r_b))
                    hctx[h] = dict(qa=qa, ka=ka, kb=kb, va=pr["va"], bi=bi, s8=8.0 * (2.0 ** (-8.0 * (h + 1) / 4)))

                hpre = {}
                def kt_order(qb):
                    dg = [kt for kt in range(NC) if 4 * qb <= kt < 4 * qb + 4]
                    return [kt for kt in range(NC) if kt not in dg] + dg
                steps = []
                for h in range(4):
                    for qb in range(NQB):
                        od = kt_order(qb)
                        for pos, kt in enumerate(od):
                            for c_ in range(2):
                                steps.append(dict(h=h, qb=qb, kt=kt, c=c_, first=(pos == 0), last=(pos == NC - 1)))
                qctx = {}

                def emit_qk(sp_):
                    h, qb, kt, c_ = sp_["h"], sp_["qb"], sp_["kt"], sp_["c"]
                    cx = hctx[h]
                    caseB = kt >= 4 * qb + 4
                    diag = (4 * qb <= kt < 4 * qb + 4)
                    kop, r_kop = (cx["kb"] if caseB else cx["ka"])[c_]
                    qop, r_qop = cx["qa"][c_]
                    ps_, r_ps = pSc[c_].nxt()
                    P.op("pe", lambda: nc.tensor.matmul(
                        ps_[:], lhsT=kop[:, kt * 128:(kt + 1) * 128], rhs=qop[:, qb * 512:(qb + 1) * 512],
                        start=True, stop=True), reads=[r_kop, r_qop], writes=[r_ps])
                    src_ap, r_src = ps_[:], r_ps
                    if diag:
                        sf, r_sf = Sfix.nxt()
                        dk = kt - 4 * qb
                        s8 = cx["s8"]
                        P.op("dve", lambda: nc.vector.scalar_tensor_tensor(
                            out=sf[:], in0=CF(CF_D2 + 4 * dk, 4), scalar=-s8, in1=ps_[:], op0=ALU.mult, op1=ALU.add),
                            reads=[r_ps, r_c], writes=[r_sf])
                        src_ap, r_src = sf[:], r_sf
                    sp_["src"] = (src_ap, r_src)

                def emit_exp(sp_):
                    h, qb, kt, c_ = sp_["h"], sp_["qb"], sp_["kt"], sp_["c"]
                    src_ap, r_src = sp_["src"]
                    pt_, r_pt = PT[c_].nxt()
                    b_, r_b = hctx[h]["bi"][c_]
                    col = kt * NQB + qb
                    P.op("act", lambda: nc.scalar.activation(
                        out=pt_[:], in_=src_ap, func=AF.Exp, bias=b_[:, col:col + 1], scale=0.125),
                        reads=[r_src, r_b], writes=[r_pt])
                    sp_["pt"] = (pt_, r_pt)

                def emit_pv(sp_):
                    h, qb, kt, c_ = sp_["h"], sp_["qb"], sp_["kt"], sp_["c"]
                    if sp_["first"] and c_ == 0:
                        qctx[(h, qb)] = [pO.nxt() for _ in range(4)]
                    po = qctx[(h, qb)]
                    pt_, r_pt = sp_["pt"]
                    va, r_va = hctx[h]["va"]
                    for sub in range(4):
                        P.op("pe", lambda sub=sub: nc.tensor.matmul(
                            po[sub][0][:, c_, 0:129], lhsT=pt_[:, sub * 128:(sub + 1) * 128], rhs=va[:, kt, :],
                            start=(sp_["first"] and c_ == 0), stop=(sp_["last"] and c_ == 1), skip_group_check=True),
                            reads=[r_pt, r_va], writes=[po[sub][1]])

                def emit_epilogue(h, qb):
                    po = qctx.pop((h, qb))
                    for _ in range(zf_per):
                        if zf_list:
                            a0 = zf_list.pop(0)
                            dma("sp", xs_v[:, a0:a0 + 2, :], zero_bf[:].rearrange("p (a d) -> p a d", a=2), reads=[r_c])
                    os_l = []
                    for sub in range(4):
                        pot, r_po = po[sub]
                        o_, r_o = osb.nxt()
                        evac(o_[:], pot[:, :, 0:129], [r_po], [r_o], eng="dve")
                        os_l.append((o_, r_o))
                    for sub in range(4):
                        o_, r_o = os_l[sub]
                        t0 = qb * 512 + sub * 128
                        os_, r_os = ost.nxt()
                        P.op("dve", lambda o_=o_, os_=os_: nc.vector.reciprocal(out=os_[:, 0:2], in_=o_[:, :, 128]),
                             reads=[r_o], writes=[r_os])
                        P.op("dve", lambda os_=os_: nc.vector.tensor_tensor(out=os_[:, 1:2], in0=os_[:, 1:2], in1=lam_s[:, 2:3], op=ALU.mult),
                             reads=[r_os, r_lam], writes=[r_os])
                        oo, r_oo = ot.nxt()
                        P.op("dve", lambda oo=oo, o_=o_, os_=os_: nc.vector.tensor_scalar(
                            out=oo[:], in0=o_[:, 0, 0:128], scalar1=os_[:, 0:1], scalar2=None, op0=ALU.mult),
                            reads=[r_o, r_os], writes=[r_oo])
                        P.op("dve", lambda oo=oo, o_=o_, os_=os_: nc.vector.scalar_tensor_tensor(
                            out=oo[:], in0=o_[:, 1, 0:128], scalar=os_[:, 1:2], in1=oo[:], op0=ALU.mult, op1=ALU.add),
                            reads=[r_o, r_os, r_oo], writes=[r_oo])
                        oq, r_oq = osq2.nxt()
                        P.op("dve", lambda oo=oo, oq=oq: nc.vector.tensor_tensor(out=oq[:], in0=oo[:], in1=oo[:], op=ALU.mult),
                             reads=[r_oo], writes=[r_oq])
                        P.op("dve", lambda oq=oq, os_=os_: nc.vector.reduce_sum(out=os_[:, 2:3], in_=oq[:], axis=AX.X),
                             reads=[r_oq], writes=[r_os])
                        P.op("dve", lambda os_=os_: nc.vector.tensor_scalar(out=os_[:, 3:4], in0=os_[:, 2:3], scalar1=1.0 / 128, scalar2=EPS,
                                                                         op0=ALU.mult, op1=ALU.add), reads=[r_os], writes=[r_os])
                        P.op("pool", lambda os_=os_: nc.gpsimd.tensor_tensor(out=os_[:, 3:4], in0=os_[:, 3:4], in1=neghalf[:, 0:1], op=ALU.pow),
                             reads=[r_os, r_c], writes=[r_os])
                        ob_, r_ob = ob.nxt()
                        P.op("dve", lambda ob_=ob_, oo=oo, os_=os_: nc.vector.scalar_tensor_tensor(
                            out=ob_[:], in0=oo[:], scalar=os_[:, 3:4], in1=subw_t[:], op0=ALU.mult, op1=ALU.mult),
                            reads=[r_oo, r_os, r_lam], writes=[r_ob])
                        dma("pool", mix_d[t0:t0 + 128, 512 + h * 128:512 + (h + 1) * 128], ob_[:], reads=[r_ob], writes=[r_mix])

                setup_loads(0)
                setup_final(0)
                AHEAD, LAG = 2, 2
                nst = len(steps)
                per_head = NQB * NC * 2
                pend_parts = []
                for i in range(min(AHEAD, nst)):
                    emit_qk(steps[i])
                for i in range(nst + LAG):
                    if i < nst:
                        sp_ = steps[i]
                        hh = sp_["h"]
                        rel_i = i - hh * per_head
                        if hh + 1 < 4:
                            if rel_i == 2:
                                setup_loads(hh + 1)
                            if rel_i == per_head // 2:
                                setup_final(hh + 1)
                        emit_exp(sp_)
                        if i + AHEAD < nst:
                            emit_qk(steps[i + AHEAD])
                    j = i - LAG
                    if j >= 0:
                        sj = steps[j]
                        emit_pv(sj)
                        if sj["last"] and sj["c"] == 1:
                            emit_epilogue(sj["h"], sj["qb"])

            P.barrier()
            with ExitStack() as st:
                wo = SB(st, "wo", [128, 8, D], BF16); r_wo = R()
                wov = w_out.rearrange("(kc p) f -> p kc f", p=128)
                for c0 in range(0, D, 512):
                    dma("pool", wo[:, :, c0:c0 + 512], wov[:, :, c0:c0 + 512], writes=[r_wo])
                wr_t = SB(st, "wr_t", [128, 8, 36]); br_t = SB(st, "br_t", [128, 36])
                nfw_t = SB(st, "nfw_t", [128, D])
                dma("sp", nfw_t[:], nfw.partition_broadcast(128), writes=[r_wo])
                dma("sp", wr_t[:], wr.rearrange("(kc p) f -> p kc f", p=128), writes=[r_wo])
                dma("sp", br_t[:], br.partition_broadcast(128), writes=[r_wo])
                mx = MB(st, "mx", [128, D], BF16, 4)
                mxT = MB(st, "mxT", [128, 8, 128], BF16, 2)
                xt = MB(st, "xt2", [128, D], F32, 4)
                ht = MB(st, "ht", [128, D], F32, 2)
                sq = SB(st, "sq2", [128, D]); r_sq = R()
                stat = MB(st, "stat2", [128, 2], F32, 2)
                xn = MB(st, "xn", [128, D], F32, 3)
                xnb = MB(st, "xnb", [128, D], BF16, 2)
                xnT = MB(st, "xnT", [128, 8, 128], F32, 3)
                pT = MB(st, "pT2", [128, 8, 128], BF16, 1, psum=True)
                pH = MB(st, "pH", [128, 512], F32, 2, psum=True)
                pTf = MB(st, "pTf", [128, 4, 128], F32, 2, psum=True)
                pL = MB(st, "pL", [128, 64], F32, 2, psum=True)
                lg_all = SB(st, "lg_all", [128, NC, 36])
                r_lg = R()

                ldq = []

                def loadD(c):
                    t0 = c * 128
                    m_, r_m = mx.nxt()
                    dma("sp", m_[:], mix_d[t0:t0 + 128, :], reads=[r_mix], writes=[r_m])
                    x_, r_x = xt.nxt()
                    dma("sp", x_[:], x[s, t0:t0 + 128, :], writes=[r_x])
                    ldq.append((m_, r_m, x_, r_x))

                def stageA1(c):
                    ti = s * NC + c
                    if c + 2 < NC:
                        loadD(c + 2)
                    m_, r_m, x_, r_x = ldq.pop(0)
                    if dbg:
                        dma("pool", dbg_d["dbg_mix"][ti * 128:(ti + 1) * 128, :], m_[:], reads=[r_m])
                    pt, r_p = pT.nxt()
                    for k in range(8):
                        P.op("pe", lambda k=k, pt=pt, m_=m_: nc.tensor.transpose(
                            out=pt[:, k, :], in_=m_[:, k * 128:(k + 1) * 128], identity=ident_b), reads=[r_m, r_c], writes=[r_p])
                    mt_, r_mt = mxT.nxt()
                    evac(mt_[:], pt[:], [r_p], [r_mt])
                    return mt_, r_mt, x_, r_x

                def stageA2(c, mt_, r_mt, x_, r_x):
                    ti = s * NC + c
                    h_, r_ht = ht.nxt()
                    for half in range(2):
                        ph, r_ph = pH.nxt()
                        for k in range(8):
                            P.op("pe", lambda k=k, ph=ph, half=half: nc.tensor.matmul(
                                ph[:], lhsT=mt_[:, k, :], rhs=wo[:, k, half * 512:(half + 1) * 512], start=(k == 0), stop=(k == 7)),
                                reads=[r_mt, r_wo], writes=[r_ph])
                        P.op("dve", lambda h_=h_, ph=ph, half=half: nc.vector.tensor_tensor(
                            out=h_[:, half * 512:(half + 1) * 512], in0=x_[:, half * 512:(half + 1) * 512], in1=ph[:], op=ALU.add),
                            reads=[r_x, r_ph], writes=[r_ht])
                    dma("sp", h_d[ti * 128:(ti + 1) * 128, :], h_[:], reads=[r_ht], writes=[r_h])
                    if dbg:
                        dma("sp", dbg_d["dbg_h"][ti * 128:(ti + 1) * 128, :], h_[:], reads=[r_ht])
                    st_, r_st = stat.nxt()
                    rms_rstd("f", h_[:], st_[:, 0:1], st_[:, 1:2], sq[:], r_ht, r_st, D)
                    xn_, r_xn_ = xn.nxt()
                    P.op("dve", lambda: nc.vector.scalar_tensor_tensor(
                        out=xn_[:], in0=h_[:], scalar=st_[:, 1:2], in1=nfw_t[:], op0=ALU.mult, op1=ALU.mult),
                        reads=[r_ht, r_st, r_wo], writes=[r_xn_])
                    xb_, r_xb = xnb.nxt()
                    P.op("act", lambda: nc.scalar.copy(out=xb_[:], in_=xn_[:]), reads=[r_xn_], writes=[r_xb])
                    dma("sp", xn_d[ti * 128:(ti + 1) * 128, :], xb_[:], reads=[r_xb], writes=[r_xn])
                    return xn_, r_xn_

                def stageB1(c, xn_, r_xn_):
                    xT_, r_xT = xnT.nxt()
                    for k0 in range(0, 8, 4):
                        ptf, r_ptf = pTf.nxt()
                        for k in range(4):
                            P.op("pe", lambda k=k, k0=k0, ptf=ptf: nc.tensor.transpose(
                                out=ptf[:, k, :], in_=xn_[:, (k0 + k) * 128:(k0 + k + 1) * 128], identity=ident_f),
                                reads=[r_xn_, r_c], writes=[r_ptf])
                        evac(xT_[:, k0:k0 + 4, :], ptf[:], [r_ptf], [r_xT])
                    return xT_, r_xT

                def stageB2(c, xT_, r_xT):
                    pl, r_pl = pL.nxt()
                    for k in range(8):
                        P.op("pe", lambda k=k: nc.tensor.matmul(
                            pl[:, 0:36], lhsT=xT_[:, k, :], rhs=wr_t[:, k, :], start=(k == 0), stop=(k == 7)),
                            reads=[r_xT, r_wo], writes=[r_pl])
                    P.op("dve", lambda: nc.vector.tensor_tensor(out=lg_all[:, c, :], in0=pl[:, 0:36], in1=br_t[:], op=ALU.add),
                         reads=[r_pl, r_wo], writes=[r_lg])

                loadD(0)
                if NC > 1:
                    loadD(1)
                pend = stageA2(0, *stageA1(0))
                for c in range(NC):
                    a1 = stageA1(c + 1) if c + 1 < NC else None
                    b1 = stageB1(c, *pend)
                    nxt_ = stageA2(c + 1, *a1) if a1 is not None else None
                    stageB2(c, *b1)
                    pend = nxt_

                def route_batch(T0, lg_all, st, r_lg):
                    V = nc.vector
                    RW = [r_lg, r_route]

                    def dv(f):
                        P.op("dve", f, reads=RW, writes=RW)
                    q8 = SB(st, "q8", [128, 8, NC])
                    g4 = SB(st, "g4", [128, NC, 4])
                    me = SB(st, "me", [128, NC, NE])
                    lgG = lg_all[:, :, 0:4]
                    lgE = lg_all[:, :, 4:36]
                    b4 = lambda ap: ap.unsqueeze(2).to_broadcast([128, NC, 4])
                    b32 = lambda ap: ap.unsqueeze(2).to_broadcast([128, NC, NE])
                    o1 = oh1_all[:, T0:T0 + NC, :]
                    o2 = oh2_all[:, T0:T0 + NC, :]
                    dv(lambda: V.reduce_max(out=q8[:, 0, :], in_=lgG, axis=AX.X))
                    dv(lambda: V.tensor_tensor(out=g4[:], in0=lgG, in1=b4(q8[:, 0, :]), op=ALU.subtract))
                    P.op("act", lambda: nc.scalar.activation(out=g4[:], in_=g4[:], func=AF.Exp), reads=RW, writes=RW)
                    dv(lambda: V.reduce_sum(out=q8[:, 1, :], in_=g4[:], axis=AX.X))
                    dv(lambda: V.reciprocal(out=q8[:, 2, :], in_=q8[:, 1, :]))
                    dv(lambda: V.tensor_tensor(out=g4[:], in0=lgG, in1=b4(q8[:, 0, :]), op=ALU.is_equal))
                    dv(lambda: V.tensor_scalar(out=g4[:], in0=g4[:], scalar1=-1.0, scalar2=NEGBIG, op0=ALU.add, op1=ALU.mult))
                    for g in range(4):
                        dv(lambda g=g: V.tensor_tensor(out=me[:, :, g * 8:(g + 1) * 8], in0=lgE[:, :, g * 8:(g + 1) * 8],
                                                       in1=g4[:, :, g].unsqueeze(2).to_broadcast([128, NC, 8]), op=ALU.add))
                    dv(lambda: V.reduce_max(out=q8[:, 3, :], in_=me[:], axis=AX.X))
                    dv(lambda: V.tensor_tensor(out=o1, in0=me[:], in1=b32(q8[:, 3, :]), op=ALU.is_equal))
                    dv(lambda: V.scalar_tensor_tensor(out=me[:], in0=o1, scalar=-NEGBIG, in1=me[:], op0=ALU.mult, op1=ALU.add))
                    dv(lambda: V.reduce_max(out=q8[:, 4, :], in_=me[:], axis=AX.X))
                    dv(lambda: V.tensor_tensor(out=o2, in0=me[:], in1=b32(q8[:, 4, :]), op=ALU.is_equal))
                    dv(lambda: V.tensor_tensor(out=q8[:, 5, :], in0=q8[:, 4, :], in1=q8[:, 3, :], op=ALU.subtract))
                    P.op("act", lambda: nc.scalar.activation(out=q8[:, 5, :], in_=q8[:, 5, :], func=AF.Exp), reads=RW, writes=RW)
                    dv(lambda: V.tensor_scalar(out=q8[:, 6, :], in0=q8[:, 5, :], scalar1=1.0, scalar2=None, op0=ALU.add))
                    dv(lambda: V.reciprocal(out=q8[:, 6, :], in_=q8[:, 6, :]))
                    dv(lambda: V.tensor_tensor(out=g_all[:, T0:T0 + NC, 0], in0=q8[:, 6, :], in1=q8[:, 2, :], op=ALU.mult))
                    dv(lambda: V.tensor_tensor(out=g_all[:, T0:T0 + NC, 1], in0=g_all[:, T0:T0 + NC, 0], in1=q8[:, 5, :], op=ALU.mult))
                    aoh_all = SB(st, "aoh_all", [128, NC, NE], BF16)
                    dv(lambda: V.tensor_tensor(out=aoh_all[:], in0=o1, in1=o2, op=ALU.add))
                    for c in range(NC):
                        ti = T0 + c
                        pl2, r_pl2 = pL.nxt()
                        P.op("pe", lambda pl2=pl2, c=c: nc.tensor.matmul(pl2[:, 0:32], lhsT=tstrict_b, rhs=aoh_all[:, c, :], start=True, stop=True),
                             reads=RW + [r_c], writes=[r_pl2])
                        P.op("pe", lambda pl2=pl2, c=c: nc.tensor.matmul(pl2[:, 32:64], lhsT=ones_b, rhs=aoh_all[:, c, :], start=True, stop=True),
                             reads=RW + [r_c], writes=[r_pl2])
                        P.op("dve", lambda pl2=pl2, ti=ti: V.tensor_tensor(out=rank_all[:, ti, :], in0=pl2[:, 0:32], in1=carry[:], op=ALU.add),
                             reads=[r_pl2, r_route], writes=[r_route])
                        P.op("dve", lambda pl2=pl2: V.tensor_tensor(out=carry[:], in0=carry[:], in1=pl2[:, 32:64], op=ALU.add),
                             reads=[r_pl2, r_route], writes=[r_route])


                route_batch(s * NC, lg_all, st, r_lg)

        for s_ in range(NSEQ):
            seq_body(s_)

        P.barrier()
        with ExitStack() as st:
            V = nc.vector
            pe_ = SB(st, "pe_", [128, NE]); ps_a = SB(st, "ps_a", [128, NE]); ps_b = SB(st, "ps_b", [128, NE])
            tmpb = SB(st, "tmpb", [128, NT, NE])
            slotf = SB(st, "slotf", [128, NT, 2])
            bef = SB(st, "bef", [128, 4]); bdiag = SB(st, "bdiag", [128, 128])
            bebc = SB(st, "bebc", [128, 128])
            idxf = SB(st, "idxf", [128, NB, 4])
            pbc = MB(st, "pbc", [128, 128], F32, 1, psum=True)
            RWR = [r_route]

            def dv(f):
                P.op("dve", f, reads=RWR + [r_c], writes=RWR)
            dv(lambda: V.tensor_scalar(out=pe_[:], in0=carry[:], scalar1=0.0, scalar2=None, op0=ALU.is_gt))
            for m_ in range(1, (T + BS - 1) // BS):
                dv(lambda m_=m_: V.scalar_tensor_tensor(out=pe_[:], in0=carry[:], scalar=float(m_ * BS), in1=pe_[:],
                                                        op0=ALU.is_gt, op1=ALU.add))
            dv(lambda: V.tensor_scalar(out=pe_[:], in0=pe_[:], scalar1=float(BS), scalar2=None, op0=ALU.mult))
            src, dst = pe_, ps_a
            sh = 1
            while sh < NE:
                dv(lambda src=src, dst=dst, sh=sh: V.tensor_copy(out=dst[:, 0:sh], in_=src[:, 0:sh]))
                dv(lambda src=src, dst=dst, sh=sh: V.tensor_tensor(out=dst[:, sh:NE], in0=src[:, sh:NE], in1=src[:, 0:NE - sh], op=ALU.add))
                src, dst = dst, (ps_b if dst is ps_a else ps_a)
                if src is pe_:
                    pass
                sh *= 2
            p_end = src
            p_start = ps_b if p_end is ps_a else ps_a
            dv(lambda: V.tensor_tensor(out=p_start[:], in0=p_end[:], in1=pe_[:], op=ALU.subtract))
            dv(lambda: V.tensor_tensor(out=rank_all[:], in0=rank_all[:], in1=p_start[:].unsqueeze(1).to_broadcast([128, NT, NE]), op=ALU.add))
            for k_, oh in enumerate((oh1_all, oh2_all)):
                dv(lambda oh=oh: V.tensor_tensor(out=tmpb[:], in0=rank_all[:], in1=oh[:], op=ALU.mult))
                dv(lambda k_=k_: V.reduce_sum(out=slotf[:, :, k_], in_=tmpb[:], axis=AX.X))
            dv(lambda: V.tensor_copy(out=slot_i[:], in_=slotf[:]))
            m0 = CF_MISC * 128
            dv(lambda: V.tensor_scalar(out=pe_[:], in0=p_end[:], scalar1=cf[:, m0:m0 + 1], scalar2=None, op0=ALU.is_le))
            dv(lambda: V.reduce_sum(out=bef[:, 0:1], in_=pe_[:], axis=AX.X))
            dv(lambda: V.tensor_scalar(out=bdiag[:], in0=ident_f, scalar1=bef[:, 0:1], scalar2=None, op0=ALU.mult))
            pb, r_pb = pbc.nxt()
            P.op("pe", lambda: nc.tensor.matmul(pb[:], lhsT=ones_f, rhs=bdiag[:], start=True, stop=True), reads=RWR + [r_c], writes=[r_pb])
            P.op("dve", lambda: V.tensor_copy(out=bebc[:], in_=pb[:]), reads=[r_pb], writes=RWR)
            beadj = SB(st, "beadj", [128, 128])
            same = SB(st, "same", [128, 128])
            dv(lambda: V.tensor_copy(out=beadj[:], in_=bebc[:]))
            if NB > 2:
                dv(lambda: V.tensor_tensor(out=same[:, 2:NB], in0=bebc[:, 2:NB], in1=bebc[:, 0:NB - 2], op=ALU.is_equal))
                dv(lambda: V.scalar_tensor_tensor(out=beadj[:, 2:NB], in0=same[:, 2:NB], scalar=64.0, in1=bebc[:, 2:NB],
                                                  op0=ALU.mult, op1=ALU.add))
            for h2 in range(2):
                dv(lambda h2=h2: V.tensor_scalar(out=idxf[:, :, h2], in0=beadj[:, 0:NB], scalar1=256.0, scalar2=float(h2), op0=ALU.mult, op1=ALU.add))
                dv(lambda h2=h2: V.scalar_tensor_tensor(out=idxf[:, :, h2], in0=cf[:, m0 + 1:m0 + 2].to_broadcast([128, NB]), scalar=2.0,
                                                        in1=idxf[:, :, h2], op0=ALU.mult, op1=ALU.add))
            dv(lambda: V.tensor_copy(out=idx_g[:], in_=idxf[:, :, 0:2]))
            for fc in range(4):
                dv(lambda fc=fc: V.tensor_scalar(out=idxf[:, :, fc], in0=beadj[:, 0:NB], scalar1=512.0, scalar2=float(fc * 128), op0=ALU.mult, op1=ALU.add))
                dv(lambda fc=fc: V.tensor_tensor(out=idxf[:, :, fc], in0=idxf[:, :, fc], in1=cf[:, m0 + 1:m0 + 2].to_broadcast([128, NB]), op=ALU.add))
            dv(lambda: V.tensor_copy(out=idx_d[:], in_=idxf[:]))
            fence_t = SB(st, "fence_t", [128, 8])
            for _ in range(2):
                dv(lambda: V.memset(fence_t[:], 0.0))
            if dbg:
                dbgt = SB(st, "dbgt", [128, NT, 8])
                dv(lambda: V.memset(dbgt[:], 0.0))
                dv(lambda: V.tensor_copy(out=dbgt[:, :, 0:2], in_=slotf[:]))
                dv(lambda: V.tensor_copy(out=dbgt[:, :, 2:4], in_=g_all[:]))
                dv(lambda: V.tensor_copy(out=dbgt[:, :, 4:5], in_=bebc[:, 0:NT].unsqueeze(2)))
                dma("sp", dbg_d["dbg_route"], dbgt[:].rearrange("p a b -> p (a b)"), reads=RWR)
            xl = MB(st, "xl", [128, D], BF16, 3)
            bcs = {}

            def mk_bcs():
                bcs["s"] = nc.gpsimd.alloc_register("bc_slot")
                return nc.gpsimd.reg_mov(bcs["s"], NSLOT - 1)
            P.op("pool", mk_bcs)
            for ti in range(NT):
                t_, r_t = xl.nxt()
                dma("sp", t_[:], xn_d[ti * 128:(ti + 1) * 128, :], reads=[r_xn], writes=[r_t])
                for k_ in range(2):
                    P.op("pool", lambda t_=t_, ti=ti, k_=k_: nc.gpsimd.indirect_dma_start(
                        out=xs_d, out_offset=bass.IndirectOffsetOnAxis(ap=slot_i[:, ti, k_:k_ + 1], axis=0),
                        in_=t_[:], in_offset=None, bounds_check=bcs["s"], oob_is_err=False),
                        reads=[r_t, r_route, r_xs], writes=[r_xs], dma=True)

        P.barrier()
        with ExitStack() as st:
            Wg = MB(st, "Wg", [128, 2, 2048], BF16, 2)
            Wu = MB(st, "Wu", [128, 2, 2048], BF16, 2)
            Wd = MB(st, "Wd", [128, 4, 1024], BF16, 2)
            xb = MB(st, "xb", [128, D], BF16, 2 * NSUB)
            xbT = MB(st, "xbT", [128, 8, BS], BF16, 2)
            hd = MB(st, "hd", [128, 4, BS], BF16, 2)
            sg = MB(st, "sg", [128, BS], F32, 2)
            ysb = MB(st, "ysb", [128, D], BF16, 3)
            pT = MB(st, "pT3", [128, 8, 128], BF16, 2, psum=True)
            pG = MB(st, "pG", [128, BS], F32, 2, psum=True)
            pU = MB(st, "pU", [128, BS], F32, 2, psum=True)
            pYm = MB(st, "pYm", [128, 512], F32, 2, psum=True)
            bcr = {}

            def mk_bc():
                bcr["g"] = nc.gpsimd.alloc_register("bc_g")
                bcr["d"] = nc.gpsimd.alloc_register("bc_d")
                nc.gpsimd.reg_mov(bcr["g"], NE * 256 - 1)
                return nc.gpsimd.reg_mov(bcr["d"], NE * 512 - 1)
            P.op("pool", mk_bc)
            for b in range(NB):
                g_, r_g = Wg.nxt()
                u_, r_u = Wu.nxt()
                d_, r_d = Wd.nxt()
                for h2 in range(2):
                    P.op("pool", lambda g_=g_, b=b, h2=h2: nc.gpsimd.indirect_dma_start(
                        out=g_[:, h2, :], out_offset=None, in_=wg,
                        in_offset=bass.IndirectOffsetOnAxis(ap=idx_g[:, b, h2:h2 + 1], axis=0),
                        bounds_check=bcr["g"], oob_is_err=False),
                        reads=[r_route], writes=[r_g], dma=True)
                    P.op("pool", lambda u_=u_, b=b, h2=h2: nc.gpsimd.indirect_dma_start(
                        out=u_[:, h2, :], out_offset=None, in_=wu,
                        in_offset=bass.IndirectOffsetOnAxis(ap=idx_g[:, b, h2:h2 + 1], axis=0),
                        bounds_check=bcr["g"], oob_is_err=False),
                        reads=[r_route], writes=[r_u], dma=True)
                for fc in range(4):
                    P.op("pool", lambda d_=d_, b=b, fc=fc: nc.gpsimd.indirect_dma_start(
                        out=d_[:, fc, :], out_offset=None, in_=wd,
                        in_offset=bass.IndirectOffsetOnAxis(ap=idx_d[:, b, fc:fc + 1], axis=0),
                        bounds_check=bcr["d"], oob_is_err=False),
                        reads=[r_route], writes=[r_d], dma=True)
                xT_, r_xT = xbT.nxt()
                if b == 0:
                    xq = []
                    for sub in range(NSUB):
                        x_, r_x = xb.nxt()
                        dma("sp", x_[:], xs_d[sub * 128:(sub + 1) * 128, :], reads=[r_xs], writes=[r_x])
                        xq.append((x_, r_x))
                xcur = xq
                if b + 1 < NB:
                    xq = []
                    for sub in range(NSUB):
                        x_, r_x = xb.nxt()
                        r0 = (b + 1) * BS + sub * 128
                        dma("sp", x_[:], xs_d[r0:r0 + 128, :], reads=[r_xs], writes=[r_x])
                        xq.append((x_, r_x))
                for sub in range(NSUB):
                    x_, r_x = xcur[sub]
                    pt, r_p = pT.nxt()
                    for j in range(8):
                        P.op("pe", lambda j=j, pt=pt, x_=x_: nc.tensor.transpose(
                            out=pt[:, j, :], in_=x_[:].rearrange("s (p j) -> s j p", j=8)[:, j, :], identity=ident_b),
                            reads=[r_x, r_c], writes=[r_p])
                    evac(xT_[:, :, sub * 128:(sub + 1) * 128], pt[:], [r_p], [r_xT])
                h_, r_h_ = hd.nxt()
                gv = g_[:].rearrange("p a (j f) -> p (a j) f", f=512)
                uv = u_[:].rearrange("p a (j f) -> p (a j) f", f=512)
                for fc in range(4):
                    pg, r_pg = pG.nxt()
                    pu, r_pu = pU.nxt()
                    for j in range(8):
                        P.op("pe", lambda j=j, pg=pg, gv=gv, fc=fc, xT_=xT_: nc.tensor.matmul(
                            pg[:], lhsT=gv[:, j, fc * 128:(fc + 1) * 128], rhs=xT_[:, j, :], start=(j == 0), stop=(j == 7)),
                            reads=[r_g, r_xT], writes=[r_pg])
                    for j in range(8):
                        P.op("pe", lambda j=j, pu=pu, uv=uv, fc=fc, xT_=xT_: nc.tensor.matmul(
                            pu[:], lhsT=uv[:, j, fc * 128:(fc + 1) * 128], rhs=xT_[:, j, :], start=(j == 0), stop=(j == 7)),
                            reads=[r_u, r_xT], writes=[r_pu])
                    s_, r_s = sg.nxt()
                    P.op("act", lambda s_=s_, pg=pg: nc.scalar.activation(out=s_[:], in_=pg[:], func=AF.Silu), reads=[r_pg], writes=[r_s])
                    P.op("dve", lambda h_=h_, s_=s_, pu=pu, fc=fc: nc.vector.tensor_tensor(out=h_[:, fc, :], in0=s_[:], in1=pu[:], op=ALU.mult),
                         reads=[r_s, r_pu], writes=[r_h_])
                for sub in range(NSUB):
                    y_, r_y_ = ysb.nxt()
                    for half in range(2):
                        py, r_py = pYm.nxt()
                        for fc in range(4):
                            P.op("pe", lambda fc=fc, py=py, h_=h_, d_=d_, sub=sub, half=half: nc.tensor.matmul(
                                py[:], lhsT=h_[:, fc, sub * 128:(sub + 1) * 128], rhs=d_[:, fc, half * 512:(half + 1) * 512],
                                start=(fc == 0), stop=(fc == 3)), reads=[r_h_, r_d], writes=[r_py])
                        evac(y_[:, half * 512:(half + 1) * 512], py[:], [r_py], [r_y_])
                    r0 = b * BS + sub * 128
                    dma("sp", y_d[r0:r0 + 128, :], y_[:], reads=[r_y_], writes=[r_y])

        P.barrier()
        with ExitStack() as st:
            ht = MB(st, "ht3", [128, D], F32, 5)
            nlw_t = SB(st, "nlw_t", [128, D]); r_nl = R()
            dma("sp", nlw_t[:], nlw.partition_broadcast(128), writes=[r_nl])
            y1 = MB(st, "y1", [128, D], BF16, 4)
            y2 = MB(st, "y2", [128, D], BF16, 4)
            sq = SB(st, "sq3", [128, D])
            stat = MB(st, "stat3", [128, 2], F32, 4)
            ot = MB(st, "ot3", [128, D], F32, 3)
            outv = out.rearrange("s l d -> (s l) d")
            hq = []

            def load_h(ti):
                h_, r_ht = ht.nxt()
                dma("sp", h_[:], h_d[ti * 128:(ti + 1) * 128, :], reads=[r_h], writes=[r_ht])
                hq.append((h_, r_ht))
            PRE = 3
            for ti in range(min(PRE, NT)):
                load_h(ti)
            for ti in range(NT):
                if ti + PRE < NT:
                    load_h(ti + PRE)
                h_, r_ht = hq.pop(0)
                ys = []
                for k_, yb in enumerate((y1, y2)):
                    y_, r_y_ = yb.nxt()
                    P.op("pool", lambda y_=y_, ti=ti, k_=k_: nc.gpsimd.indirect_dma_start(
                        out=y_[:], out_offset=None, in_=y_d,
                        in_offset=bass.IndirectOffsetOnAxis(ap=slot_i[:, ti, k_:k_ + 1], axis=0),
                        bounds_check=bcs["s"], oob_is_err=False),
                        reads=[r_route, r_y], writes=[r_y_], dma=True)
                    ys.append((y_, r_y_))
                for k_ in range(2):
                    y_, r_y_ = ys[k_]
                    P.op("dve", lambda h_=h_, y_=y_, ti=ti, k_=k_: nc.vector.scalar_tensor_tensor(
                        out=h_[:], in0=y_[:], scalar=g_all[:, ti, k_:k_ + 1], in1=h_[:], op0=ALU.mult, op1=ALU.add),
                        reads=[r_y_, r_ht, r_route], writes=[r_ht])
                st_, r_st = stat.nxt()
                rms_rstd("z", h_[:], st_[:, 0:1], st_[:, 1:2], sq[:], r_ht, r_st, D)
                o_, r_o = ot.nxt()
                P.op("dve", lambda o_=o_, h_=h_, st_=st_: nc.vector.scalar_tensor_tensor(
                    out=o_[:], in0=h_[:], scalar=st_[:, 1:2], in1=nlw_t[:], op0=ALU.mult, op1=ALU.mult),
                    reads=[r_ht, r_st, r_nl], writes=[r_o])
                dma("sp", outv[ti * 128:(ti + 1) * 128, :], o_[:], reads=[r_o])
        P.emit(top)
    return nc


def host_inputs(inp, L, BS, n_cores, nseq):
    consts, _ = make_consts(L, BS)
    f = lambda a: np.ascontiguousarray(np.asarray(a, dtype=np.float32))
    common = dict(
        w_in=f(inp["w_in"][0]), w_out=f(inp["w_out"][0]),
        wr=f(np.concatenate([inp["w_router_group"][0], inp["w_router_exp"][0]], axis=1)),
        br=f(np.concatenate([inp["b_router_group"][0], inp["b_router_exp"][0]])[None, :]),
        wg=f(inp["w_exp_gate"][0]).reshape(NE * 128 * 2, 2048),
        wu=f(inp["w_exp_up"][0]).reshape(NE * 128 * 2, 2048),
        wd=f(inp["w_exp_down"][0]).reshape(NE * 512, 1024),
        nmw=f(inp["norm_mix_w"]), nfw=f(inp["norm_ffn_w"]), nlw=f(inp["norm_final_w"])[None, :],
        convw=f(np.asarray(inp["conv_w"][0]).T.reshape(8, 128, 5).transpose(1, 0, 2).reshape(128, 40)),
        convb=f(np.asarray(inp["conv_b"][0]).reshape(8, 128).T),
        dtb=f(np.concatenate([inp["dt_bias_fwd"][0], inp["dt_bias_bwd"][0]])[None, :]),
        alog=f(np.concatenate([inp["a_log_fwd"][0], inp["a_log_bwd"][0]])[None, :]),
        dsk=f(inp["ssd_d"]), snw=f(inp["ssd_norm_w"]),
        lam=f(np.stack([inp["lambda_q1"][0], inp["lambda_q2"][0], inp["lambda_k1"][0], inp["lambda_k2"][0]])),
        sub_w=f(inp["subln_w"]),
        **consts,
    )
    xs = f(inp["x"])
    maps = []
    for c in range(n_cores):
        m = dict(common)
        m["x"] = np.ascontiguousarray(xs[c * nseq:(c + 1) * nseq])
        maps.append(m)
    return maps


def kernel(**inputs):
    n_cores, nseq, L, BS = 8, 2, 4096, 512
    nc = build(nseq, L, BS)
    maps = host_inputs(inputs, L, BS, n_cores, nseq)
    res = run_bass_kernel_spmd(nc, maps, core_ids=list(range(n_cores)))
    return np.concatenate([np.asarray(r["out"], dtype=np.float32) for r in res.results], axis=0)
```

```python
import math
from contextlib import ExitStack
import numpy as np
import ml_dtypes
import concourse.bass as bass
import concourse.mybir as mybir
from concourse.bass_utils import run_bass_kernel_spmd

F32 = mybir.dt.float32
BF16 = mybir.dt.bfloat16
I32 = mybir.dt.int32
ALU = mybir.AluOpType
AF = mybir.ActivationFunctionType
AX = mybir.AxisListType

D = 1024
NE = 32
DE = 512
EPS = 1e-6
LAMBDA_INIT = 0.2
NEGBIG = 30000.0
STRICT_SYNC = True


class R:
    __slots__ = ("w", "rd")

    def __init__(self):
        self.w = None
        self.rd = {}


class Prog:
    ENG = ("pe", "act", "dve", "pool", "sp")

    def __init__(self, nc, n_dma_sems=14):
        self.nc = nc
        self.ops = []
        self.n_dma_sems = n_dma_sems

    def op(self, eng, emit, reads=(), writes=(), dma=False):
        reads = [r for r in reads if r is not None]
        writes = [w for w in writes if w is not None]
        self.ops.append(dict(eng=eng, emit=emit, reads=tuple(reads), writes=tuple(writes),
                             dma=dma, deps=set(), marked=False, barrier=False))

    def barrier(self):
        for e in self.ENG:
            self.ops.append(dict(eng=e, emit=None, reads=(), writes=(), dma=False, deps=set(),
                                 marked=False, barrier=True))

    def _analyze(self):
        ops = self.ops
        last_on_eng = {e: None for e in self.ENG}
        dmas = []
        for i, o in enumerate(ops):
            e = o["eng"]
            if o["barrier"]:
                for e2 in self.ENG:
                    if e2 != e and last_on_eng[e2] is not None:
                        o["deps"].add(last_on_eng[e2])
                o["deps"].update(dmas)
                if e == self.ENG[-1]:
                    dmas = []
                continue
            deps = set()
            for r in o["reads"]:
                if r.w is not None:
                    deps.add(("raw", r.w))
            for w in o["writes"]:
                if w.w is not None:
                    deps.add(("waw", w.w))
                for j in w.rd.values():
                    deps.add(("war", j))
            for kind, j in deps:
                if j == i:
                    continue
                oj = ops[j]
                if oj["eng"] == e and not oj["dma"] and not o["dma"]:
                    if e == "pe" or (kind != "raw" and not STRICT_SYNC):
                        continue
                o["deps"].add(j)
            for r in o["reads"]:
                r.rd[("dma", i) if o["dma"] else e] = i
            for w in o["writes"]:
                w.w = i
                w.rd = {}
            if o["dma"]:
                dmas.append(i)
            else:
                last_on_eng[e] = i
        for o in ops:
            for j in o["deps"]:
                ops[j]["marked"] = True

    def emit(self, stack):
        nc = self.nc
        self._analyze()
        ops = self.ops
        sems = {e: stack.enter_context(nc.semaphore("s_" + e)) for e in self.ENG}
        dq = ("sp", "pool", "act")
        dsem = {e: [stack.enter_context(nc.semaphore("d_%s_%d" % (e, k)))
                    for k in range(self.n_dma_sems)] for e in dq}
        cnt = {e: 0 for e in self.ENG}
        dcount = {e: 0 for e in dq}
        dval = {e: [0] * self.n_dma_sems for e in dq}
        prev_on_sem = {}
        for i, o in enumerate(ops):
            if o["barrier"]:
                continue
            e = o["eng"]
            if o["dma"]:
                k = dcount[e] % self.n_dma_sems
                dcount[e] += 1
                o["prev_same_sem"] = prev_on_sem.get((e, k))
                dval[e][k] += 16
                o["done"] = (dsem[e][k], dval[e][k], ("d", e, k))
                prev_on_sem[(e, k)] = i
            elif o["marked"]:
                cnt[e] += 1
                o["done"] = (sems[e], cnt[e], ("c", e))
        final_dma = [(dsem[e][k], dval[e][k]) for e in dq for k in range(self.n_dma_sems)
                     if dval[e][k] > 0]
        blk = stack.enter_context(nc.Block())
        for e in self.ENG:
            my = [(i, o) for i, o in enumerate(ops) if o["eng"] == e]

            def body(engine, my=my, e=e):
                seen = {}
                for i, o in my:
                    deps = set(o["deps"])
                    if o["dma"] and o.get("prev_same_sem") is not None:
                        deps.add(o["prev_same_sem"])
                    need = {}
                    for j in deps:
                        s, v, key = ops[j]["done"]
                        if seen.get(key, 0) >= v:
                            continue
                        if key not in need or need[key][1] < v:
                            need[key] = (s, v)
                    for key, (s, v) in need.items():
                        engine.wait_ge(s, v)
                        seen[key] = v
                    if o["barrier"]:
                        continue
                    ins = o["emit"]()
                    if "done" in o:
                        ins.then_inc(o["done"][0], 16 if o["dma"] else 1)
                if e == "sp":
                    for s, v in final_dma:
                        engine.wait_ge(s, v)

            getattr(blk, {"pe": "tensor", "act": "scalar", "dve": "vector",
                          "pool": "gpsimd", "sp": "sync"}[e])(body)


CF_IDENT, CF_ONES, CF_TF, CF_TB, CF_UF, CF_UB, CF_MF, CF_MB = range(8)
CF_D2 = 8
CF_MISC = 8 + 16
NCF = (8 + 16) * 128 + 16


def make_consts(L, BS):
    t = np.arange(128)
    a = t[:, None]
    b = t[None, :]
    cf = np.zeros((128, NCF), np.float32)

    def put(k, m):
        cf[:, k * 128:(k + 1) * 128] = m
    put(CF_IDENT, (a == b))
    put(CF_ONES, np.ones((128, 128)))
    put(CF_TF, (a <= b))
    put(CF_TB, (a >= b))
    put(CF_UF, (a > b))
    put(CF_UB, (a < b))
    put(CF_MF, (b >= a))
    put(CF_MB, (b <= a))
    ii = np.arange(512)[None, :]
    for dk in range(4):
        d2 = 2.0 * np.maximum(a + 128 * dk - ii, 0)
        cf[:, (CF_D2 + 4 * dk) * 128:(CF_D2 + 4 * dk + 4) * 128] = d2
    m0 = CF_MISC * 128
    cf[:, m0 + 0] = t * BS
    cf[:, m0 + 1] = t
    cb = np.zeros((128, 4 * 128), np.float32)
    cb[:, 384:512] = ((a < 64) == (b < 64))
    cb[:, 0:128] = (a == b)
    cb[:, 128:256] = 1.0
    cb[:, 256:384] = (a < b)
    cb = cb.astype(ml_dtypes.bfloat16)
    slopes = [2.0 ** (-8.0 * (h + 1) / 4) for h in range(4)]
    pos = np.arange(L)
    ip = pos % 512
    jp = pos % 128
    qrows = np.zeros((4, 3, L), np.float32)
    krA = np.zeros((4, 3, L), np.float32)
    for h in range(4):
        s8 = 8.0 * slopes[h]
        qrows[h, 0] = -s8 * (ip % 256)
        qrows[h, 1] = -s8 * 256 * (ip // 256)
        qrows[h, 2] = 1.0
        krA[h, 0] = 1.0
        krA[h, 1] = 1.0
        krA[h, 2] = s8 * jp
    krB = -krA
    nkt, nqb = L // 128, L // 512
    btab = np.zeros((4, nkt * nqb), np.float32)
    for h in range(4):
        for kt in range(nkt):
            for qb in range(nqb):
                if kt >= 4 * qb + 4:
                    v = -(128 * kt - 512 * qb)
                else:
                    v = -(512 * qb - 128 * kt)
                btab[h, kt * nqb + qb] = slopes[h] * v
    return dict(cf=cf, cb=cb, qrows=qrows.astype(ml_dtypes.bfloat16),
                krA=krA.astype(ml_dtypes.bfloat16), krB=krB.astype(ml_dtypes.bfloat16), btab=btab), slopes


def build(NSEQ, L, BS, dbg=False):
    nc = bass.Bass("TRN2", target_bir_lowering=False)
    T = NSEQ * L
    NT = T // 128
    NC = L // 128
    NQB = L // 512
    NB = (2 * T) // BS + NE
    NSUB = BS // 128
    NSLOT = NB * BS
    assert NB <= 128

    def din(name, shape, dt=F32):
        return nc.dram_tensor(name, list(shape), dt, kind="ExternalInput").ap()

    def dscr(name, shape, dt):
        return nc.dram_tensor(name, list(shape), dt, kind="Internal").ap()

    x = din("x", [NSEQ, L, D])
    w_in = din("w_in", [D, 3088])
    w_out = din("w_out", [D, D])
    wr = din("wr", [D, 36])
    br = din("br", [1, 36])
    wg = din("wg", [NE * 128 * 2, 2048])
    wu = din("wu", [NE * 128 * 2, 2048])
    wd = din("wd", [NE * 512, 1024])
    nmw = din("nmw", [1, D])
    nfw = din("nfw", [1, D])
    nlw = din("nlw", [1, D])
    convw = din("convw", [128, 40])
    convb = din("convb", [128, 8])
    dtb = din("dtb", [1, 16])
    alog = din("alog", [1, 16])
    dsk = din("dsk", [1, 8])
    snw = din("snw", [1, 512])
    lam = din("lam", [4, 64])
    sub_w = din("sub_w", [1, 128])
    cf_d = din("cf", [128, NCF])
    cb_d = din("cb", [128, 512], BF16)
    qrows_d = din("qrows", [4, 3, L], BF16)
    krA_d = din("krA", [4, 3, L], BF16)
    krB_d = din("krB", [4, 3, L], BF16)
    btab_d = din("btab", [4, NC * NQB])
    out = nc.dram_tensor("out", [NSEQ, L, D], F32, kind="ExternalOutput").ap()

    nmax_d = dscr("nmax_d", [128, 16], F32)
    xbc_d = dscr("xbc_d", [D, L + 4], BF16)
    qT_d = dscr("qT_d", [512, L], BF16)
    kT_d = dscr("kT_d", [512, L], BF16)
    v_d = dscr("v_d", [L, 512], BF16)
    z_d = dscr("z_d", [L, 512], F32)
    dt_d = dscr("dt_d", [L, 16], F32)
    mix_d = dscr("mix_d", [L, D], BF16)
    h_d = dscr("h_d", [T, D], F32)
    xn_d = dscr("xn_d", [T, D], BF16)
    xs_d = dscr("xs_d", [NSLOT, D], BF16)
    y_d = dscr("y_d", [NSLOT, D], BF16)
    dbg_d = {}
    if dbg:
        for nm, shp in (("dbg_mix", [NSEQ * L, D]), ("dbg_h", [T, D]), ("dbg_route", [128, NT * 8])):
            dbg_d[nm] = nc.dram_tensor(nm, shp, F32, kind="ExternalOutput").ap()

    P = Prog(nc)
    top = ExitStack()
    with top:
        uid = [0]

        def SB(st, name, shape, dt=F32):
            uid[0] += 1
            return st.enter_context(nc.sbuf_tensor("sb_%s_%d" % (name, uid[0]), list(shape), dt))

        def PS(st, name, shape, dt=F32):
            uid[0] += 1
            return st.enter_context(nc.psum_tensor("ps_%s_%d" % (name, uid[0]), list(shape), dt))

        class MB:
            def __init__(self, st, name, shape, dt=F32, n=2, psum=False):
                mk = PS if psum else SB
                self.t = [mk(st, "%s%d" % (name, i), shape, dt) for i in range(n)]
                self.r = [R() for _ in range(n)]
                self.n = n
                self.k = -1

            def nxt(self):
                self.k += 1
                i = self.k % self.n
                return self.t[i], self.r[i]

        rr = {"ev": 0}

        def evac(out_ap, in_ap, reads, writes, eng=None):
            if eng is None:
                eng = ("act", "dve")[rr["ev"] % 2]
                rr["ev"] += 1
            if eng == "act":
                P.op("act", lambda: nc.scalar.copy(out=out_ap, in_=in_ap), reads=reads, writes=writes)
            else:
                P.op("dve", lambda: nc.vector.tensor_copy(out=out_ap, in_=in_ap), reads=reads, writes=writes)

        def dma(eng, out_ap, in_ap, reads=(), writes=()):
            q = {"sp": nc.sync, "pool": nc.gpsimd, "act": nc.scalar}[eng]
            P.op(eng, lambda: q.dma_start(out=out_ap, in_=in_ap), reads=reads, writes=writes, dma=True)

        cf = SB(top, "cf", [128, NCF]); r_c = R()
        cb = SB(top, "cb", [128, 512], BF16)
        dma("sp", cf[:], cf_d, writes=[r_c])
        dma("sp", cb[:], cb_d, writes=[r_c])

        def CF(k, n=1):
            return cf[:, k * 128:(k + n) * 128]
        ident_f = CF(CF_IDENT)
        ones_f = CF(CF_ONES)
        ident_b = cb[:, 0:128]
        ones_b = cb[:, 128:256]
        tstrict_b = cb[:, 256:384]
        bones_c = cb[:, 384:512]
        eps_t = SB(top, "eps_t", [128, 1])
        P.op("pool", lambda: nc.gpsimd.memset(eps_t[:], EPS), writes=[r_c])
        neghalf = SB(top, "neghalf", [128, 1])
        P.op("pool", lambda: nc.gpsimd.memset(neghalf[:], -0.5), writes=[r_c])
        poshalf = SB(top, "poshalf", [128, 1])
        P.op("pool", lambda: nc.gpsimd.memset(poshalf[:], 0.5), writes=[r_c])
        zero_bf = SB(top, "zero_bf", [128, 2048], BF16)
        if dbg:
            pass
        P.op("pool", lambda: nc.gpsimd.memset(zero_bf[:], 0.0), writes=[r_c])
        xs_v = xs_d.rearrange("(a p) d -> p a d", p=128)
        r_xs = None
        na = NSLOT // 128
        r_xbc = None
        dma("sp", xbc_d[:, 0:2].rearrange("(a p) d -> p a d", p=128), zero_bf[:, 0:16].rearrange("p (a d) -> p a d", d=2),
            reads=[r_c], writes=[r_xbc])
        dma("sp", xbc_d[:, L + 2:L + 4].rearrange("(a p) d -> p a d", p=128), zero_bf[:, 0:16].rearrange("p (a d) -> p a d", d=2),
            reads=[r_c], writes=[r_xbc])


        oh1_all = SB(top, "oh1_all", [128, NT, NE], BF16)
        oh2_all = SB(top, "oh2_all", [128, NT, NE], BF16)
        rank_all = SB(top, "rank_all", [128, NT, NE])
        g_all = SB(top, "g_all", [128, NT, 2])
        carry = SB(top, "carry", [128, NE])
        slot_i = SB(top, "slot_i", [128, NT, 2], I32)
        idx_g = SB(top, "idx_g", [128, NB, 2], I32)
        idx_d = SB(top, "idx_d", [128, NB, 4], I32)
        r_route = R()
        P.op("pool", lambda: nc.gpsimd.memset(carry[:], 0.0), writes=[r_route])

        r_q = r_k = r_v = r_z = r_dt = r_mix = None
        r_h = r_xn = r_y = None

        def rms_rstd(st_name, src_ap, ss, rstd, sq_scr, r_src, r_s, n):
            P.op("act", lambda: nc.scalar.activation(out=sq_scr, in_=src_ap, func=AF.Square, accum_out=ss),
                 reads=[r_src], writes=[r_s])
            P.op("act", lambda: nc.scalar.activation(out=rstd, in_=ss, func=AF.Ln, bias=eps_t[:, 0:1],
                                                     scale=1.0 / n), reads=[r_s, r_c], writes=[r_s])
            P.op("act", lambda: nc.scalar.activation(out=rstd, in_=rstd, func=AF.Exp, scale=-0.5),
                 reads=[r_s], writes=[r_s])

        def seq_body(s):
            P.barrier()
            with ExitStack() as st:
                win = SB(st, "win", [128, 8, 3088], BF16); r_win = R()
                nmw_t = SB(st, "nmw_t", [128, D])
                dma("sp", nmw_t[:], nmw.partition_broadcast(128), writes=[r_win])
                wv = w_in.rearrange("(kc p) f -> p kc f", p=128)
                for c0 in range(0, 3088, 512):
                    c1 = min(3088, c0 + 512)
                    dma("pool", win[:, :, c0:c1], wv[:, :, c0:c1], writes=[r_win])
                xt = MB(st, "xt", [128, D], F32, 4)
                sq = SB(st, "sq", [128, D]); r_sq = R()
                stat = MB(st, "stat", [128, 2], F32, 4)
                ub = MB(st, "ub", [128, D], BF16, 4)
                uT = MB(st, "uT", [128, 8, 512], BF16, 2)
                pT = MB(st, "pT", [128, 8, 128], BF16, 2, psum=True)
                pA = MB(st, "pA", [128, 512], F32, 3, psum=True)
                pB = MB(st, "pB", [128, 512], F32, 2, psum=True)
                zs = MB(st, "zs", [128, 512], F32, 2)
                vs = MB(st, "vs", [128, 512], BF16, 2)
                ds = MB(st, "ds", [128, 16], F32, 2)
                fs = MB(st, "fs", [128, 512], BF16, 3)
                sqA = MB(st, "sqA", [128, 512], BF16, 5)
                pNA = MB(st, "pNA", [128, 512], F32, 1, psum=True)
                nmax = SB(st, "nmax", [128, 16]); nmt = SB(st, "nmt", [128, 1]); r_nm = R()
                P.op("pool", lambda: nc.gpsimd.memset(nmax[:], 0.0), writes=[r_nm])
                def prepA1(tb):
                    ubs = []
                    for j in range(4):
                        t0 = tb * 512 + j * 128
                        xtt, r_xt = xt.nxt()
                        dma("sp", xtt[:], x[s, t0:t0 + 128, :], writes=[r_xt])
                        stt, r_st = stat.nxt()
                        rms_rstd("a", xtt[:], stt[:, 0:1], stt[:, 1:2], sq[:], r_xt, r_st, D)
                        ubt, r_ub = ub.nxt()
                        P.op("dve", lambda ubt=ubt, xtt=xtt, stt=stt: nc.vector.scalar_tensor_tensor(
                            out=ubt[:], in0=xtt[:], scalar=stt[:, 1:2], in1=nmw_t[:], op0=ALU.mult, op1=ALU.mult),
                            reads=[r_xt, r_st, r_win], writes=[r_ub])
                        ubs.append((ubt, r_ub))
                    return ubs

                def prepA2(ubs):
                    uTt, r_uT = uT.nxt()
                    for j in range(4):
                        ubt, r_ub = ubs[j]
                        pTt, r_pT = pT.nxt()
                        for k in range(8):
                            P.op("pe", lambda k=k, pTt=pTt, ubt=ubt: nc.tensor.transpose(
                                out=pTt[:, k, :], in_=ubt[:, k * 128:(k + 1) * 128], identity=ident_b),
                                reads=[r_ub, r_c], writes=[r_pT])
                        evac(uTt[:, :, j * 128:(j + 1) * 128], pTt[:], [r_pT], [r_uT])
                    return uTt, r_uT

                pend_bm = []

                def emit_bm(sqb, r_sqb, cc):
                    pn, r_pn = pNA.nxt()
                    P.op("pe", lambda: nc.tensor.matmul(pn[:], lhsT=bones_c, rhs=sqb[:], start=True, stop=True),
                         reads=[r_sqb, r_c], writes=[r_pn])
                    P.op("dve", lambda: nc.vector.reduce_max(out=nmt[:, 0:1], in_=pn[:], axis=AX.X),
                         reads=[r_pn], writes=[r_nm])
                    P.op("dve", lambda: nc.vector.tensor_tensor(out=nmax[:, cc - 8:cc - 7], in0=nmax[:, cc - 8:cc - 7],
                                                                in1=nmt[:, 0:1], op=ALU.max), reads=[r_nm], writes=[r_nm])

                pendA = prepA2(prepA1(0))
                NTB = L // 512
                for tb in range(NTB):
                    uTt, r_uT = pendA
                    ubs_n = prepA1(tb + 1) if tb + 1 < NTB else None
                    for j in range(4):
                        t0 = tb * 512 + j * 128
                        for (c0, n, kind) in ((0, 512, "z"), (1536, 16, "dt"), (2576, 512, "v")):
                            pt, r_p = pA.nxt()
                            for k in range(8):
                                P.op("pe", lambda k=k, pt=pt, c0=c0, n=n, j=j, uTt=uTt: nc.tensor.matmul(
                                    pt[:, 0:n], lhsT=uTt[:, k, j * 128:(j + 1) * 128], rhs=win[:, k, c0:c0 + n],
                                    start=(k == 0), stop=(k == 7)), reads=[r_uT, r_win], writes=[r_p])
                            if kind == "z":
                                o, r_o = zs.nxt()
                                evac(o[:], pt[:], [r_p], [r_o])
                                dma("sp", z_d[t0:t0 + 128, :], o[:], reads=[r_o], writes=[r_z])
                            elif kind == "v":
                                o, r_o = vs.nxt()
                                evac(o[:], pt[:], [r_p], [r_o])
                                dma("sp", v_d[t0:t0 + 128, :], o[:], reads=[r_o], writes=[r_v])
                            else:
                                o, r_o = ds.nxt()
                                evac(o[:], pt[:, 0:16], [r_p], [r_o])
                                dma("sp", dt_d[t0:t0 + 128, :], o[:], reads=[r_o], writes=[r_dt])
                    for cc in range(16):
                        c0 = 512 + cc * 128 if cc < 8 else (1552 + (cc - 8) * 128)
                        pt, r_p = pB.nxt()
                        for k in range(8):
                            P.op("pe", lambda k=k, pt=pt, c0=c0, uTt=uTt: nc.tensor.matmul(
                                pt[:], lhsT=win[:, k, c0:c0 + 128], rhs=uTt[:, k, :],
                                start=(k == 0), stop=(k == 7)), reads=[r_uT, r_win], writes=[r_p])
                        if len(pend_bm) > 2:
                            emit_bm(*pend_bm.pop(0))
                        o, r_o = fs.nxt()
                        evac(o[:], pt[:], [r_p], [r_o])
                        if cc >= 8:
                            sqb, r_sqb = sqA.nxt()
                            P.op("pool", lambda sqb=sqb, o=o: nc.gpsimd.tensor_tensor(out=sqb[:], in0=o[:], in1=o[:], op=ALU.mult),
                                 reads=[r_o], writes=[r_sqb])
                            pend_bm.append((sqb, r_sqb, cc))
                        if cc < 8:
                            dma("sp", xbc_d[cc * 128:(cc + 1) * 128, 2 + tb * 512:2 + (tb + 1) * 512], o[:],
                                reads=[r_o], writes=[r_xbc])
                        elif cc < 12:
                            dma("sp", qT_d[(cc - 8) * 128:(cc - 7) * 128, tb * 512:(tb + 1) * 512], o[:],
                                reads=[r_o], writes=[r_q])
                        else:
                            dma("sp", kT_d[(cc - 12) * 128:(cc - 11) * 128, tb * 512:(tb + 1) * 512], o[:],
                                reads=[r_o], writes=[r_k])
                    if ubs_n is not None:
                        pendA = prepA2(ubs_n)
                while pend_bm:
                    emit_bm(*pend_bm.pop(0))
                dma("sp", nmax_d, nmax[:], reads=[r_nm])

            P.barrier()
            with ExitStack() as st:
                xs_tm = SB(st, "xs_tm", [128, NC, 512], BF16)
                B_tm = SB(st, "B_tm", [128, NC, 256], BF16)
                BT = SB(st, "BT", [128, 2, L], BF16)
                CT = SB(st, "CT", [128, 2, L], BF16)
                r_prep = R()
                cw = SB(st, "cw", [128, 8, 5]); cbias = SB(st, "cbias", [128, 8, 1]); r_cw = R()
                dma("sp", cw[:], convw.rearrange("p (cc k) -> p cc k", k=5), writes=[r_cw])
                dma("sp", cbias[:], convb.rearrange("p (cc k) -> p cc k", k=1), writes=[r_cw])
                dtr = SB(st, "dtr", [128, NC, 16]); r_dtr = R()
                dtt = SB(st, "dtt", [128, NC, 16])
                adt = SB(st, "adt", [128, NC, 16])
                cs = SB(st, "cs", [128, NC, 16])
                ecs = SB(st, "ecs", [128, NC, 16])
                dte = SB(st, "dte", [128, NC, 16])
                cd = SB(st, "cd", [128, NC, 16])
                tmp1 = SB(st, "tmp1", [128, NC, 16]); tmp2 = SB(st, "tmp2", [128, NC, 16])
                dtb_t = SB(st, "dtb_t", [128, 16]); a_t = SB(st, "a_t", [128, 16]); dsk_t = SB(st, "dsk_t", [128, 8])
                snw_t = SB(st, "snw_t", [128, 512])
                r_dtp = R()
                dma("sp", dtr[:], dt_d.rearrange("(c p) h -> p c h", p=128), reads=[r_dt], writes=[r_dtr])
                dma("sp", dtb_t[:], dtb.partition_broadcast(128), writes=[r_dtp])
                dma("sp", a_t[:], alog.partition_broadcast(128), writes=[r_dtp])
                dma("sp", dsk_t[:], dsk.partition_broadcast(128), writes=[r_dtp])
                dma("sp", snw_t[:], snw.partition_broadcast(128), writes=[r_dtp])
                bc16 = lambda t: t[:].unsqueeze(1).to_broadcast([128, NC, 16])
                P.op("act", lambda: nc.scalar.activation(out=a_t[:], in_=a_t[:], func=AF.Exp), reads=[r_dtp], writes=[r_dtp])
                P.op("dve", lambda: nc.vector.tensor_scalar(out=a_t[:], in0=a_t[:], scalar1=-1.0, scalar2=None, op0=ALU.mult),
                     reads=[r_dtp], writes=[r_dtp])
                P.op("dve", lambda: nc.vector.tensor_tensor(out=dtr[:], in0=dtr[:], in1=bc16(dtb_t), op=ALU.add),
                     reads=[r_dtr, r_dtp], writes=[r_dtr])
                P.op("dve", lambda: nc.vector.tensor_scalar(out=tmp1[:], in0=dtr[:], scalar1=30.0, scalar2=None, op0=ALU.min),
                     reads=[r_dtr], writes=[r_dtp])
                P.op("act", lambda: nc.scalar.activation(out=tmp1[:], in_=tmp1[:], func=AF.Exp),
                     reads=[r_dtp], writes=[r_dtp])
                P.op("dve", lambda: nc.vector.tensor_scalar(out=tmp1[:], in0=tmp1[:], scalar1=1.0, scalar2=None, op0=ALU.add),
                     reads=[r_dtp], writes=[r_dtp])
                P.op("act", lambda: nc.scalar.activation(out=tmp1[:], in_=tmp1[:], func=AF.Ln),
                     reads=[r_dtp], writes=[r_dtp])
                P.op("dve", lambda: nc.vector.tensor_tensor(out=dtt[:], in0=tmp1[:], in1=dtr[:], op=ALU.max),
                     reads=[r_dtp, r_dtr], writes=[r_dtp])
                P.op("dve", lambda: nc.vector.tensor_tensor(out=adt[:], in0=dtt[:], in1=bc16(a_t), op=ALU.mult),
                     reads=[r_dtp], writes=[r_dtp])
                stc = ExitStack()
                pC = MB(stc, "pC", [128, 512], F32, 2, psum=True)
                adt_f = adt[:].rearrange("p c h -> p (c h)")
                ncol = NC * 16
                for (lh, dst, lo, hi) in ((CF(CF_TF), cs, 0, 8), (CF(CF_TB), cs, 8, 16), (ones_f, tmp1, 0, 16)):
                    for c0 in range(0, ncol, 512):
                        c1 = min(ncol, c0 + 512)
                        pt, r_p = pC.nxt()
                        P.op("pe", lambda pt=pt, lh=lh, c0=c0, c1=c1: nc.tensor.matmul(
                            pt[:, 0:c1 - c0], lhsT=lh, rhs=adt_f[:, c0:c1], start=True, stop=True),
                            reads=[r_dtp, r_c], writes=[r_p])
                        ca, cb_ = c0 // 16, c1 // 16
                        evac(dst[:, ca:cb_, lo:hi], pt[:, 0:c1 - c0].rearrange("p (c h) -> p c h", h=16)[:, :, lo:hi],
                             [r_p], [r_dtp], eng="dve")
                P.op("act", lambda: nc.scalar.activation(out=ecs[:], in_=cs[:], func=AF.Exp), reads=[r_dtp], writes=[r_dtp])
                P.op("dve", lambda: nc.vector.tensor_tensor(out=tmp2[:], in0=tmp1[:], in1=cs[:], op=ALU.subtract),
                     reads=[r_dtp], writes=[r_dtp])
                P.op("act", lambda: nc.scalar.activation(out=dte[:], in_=tmp2[:], func=AF.Exp), reads=[r_dtp], writes=[r_dtp])
                P.op("act", lambda: nc.scalar.activation(out=cd[:], in_=tmp1[:], func=AF.Exp), reads=[r_dtp], writes=[r_dtp])

                with ExitStack() as st2:
                    raw = MB(st2, "raw", [128, L + 4], BF16, 2)
                    xsT = MB(st2, "xsT", [128, L], BF16, 2)
                    pTr = MB(st2, "pTr", [128, 4, 128], BF16, 2, psum=True)
                    pcv = MB(st2, "pcv", [128, 512], F32, 2, psum=True)
                    dgw = SB(st2, "dgw", [128, 8, 5, 128], BF16); r_dg = R()
                    for cc in range(8):
                        for k in range(5):
                            P.op("dve", lambda cc=cc, k=k: nc.vector.tensor_scalar(
                                out=dgw[:, cc, k, :], in0=ident_b, scalar1=cw[:, cc, k:k + 1], scalar2=None, op0=ALU.mult),
                                reads=[r_cw, r_c], writes=[r_dg])
                    for cc in range(8):
                        rw, r_rw = raw.nxt()
                        dma("sp", rw[:], xbc_d[cc * 128:(cc + 1) * 128, :], reads=[r_xbc], writes=[r_rw])
                        if cc < 4:
                            dst, r_d = xsT.nxt()
                            dstap = dst[:]
                        elif cc < 6:
                            dstap, r_d = BT[:, cc - 4, :], r_prep
                        else:
                            dstap, r_d = CT[:, cc - 6, :], r_prep
                        for tbk in range(L // 512):
                            o0 = tbk * 512
                            pc, r_pc = pcv.nxt()
                            for k in range(5):
                                P.op("pe", lambda pc=pc, rw=rw, cc=cc, k=k, o0=o0: nc.tensor.matmul(
                                    pc[:], lhsT=dgw[:, cc, k, :], rhs=rw[:, o0 + k:o0 + k + 512], start=(k == 0), stop=(k == 4)),
                                    reads=[r_rw, r_dg], writes=[r_pc])
                            P.op("act", lambda pc=pc, dstap=dstap, o0=o0, cc=cc: nc.scalar.activation(
                                out=dstap[:, o0:o0 + 512], in_=pc[:], func=AF.Silu, bias=cbias[:, cc, 0:1]),
                                reads=[r_pc, r_cw], writes=[r_d])
                        if cc < 6:
                            for c0 in range(0, NC, 4):
                                pt, r_p = pTr.nxt()
                                for i in range(4):
                                    P.op("pe", lambda pt=pt, i=i, c0=c0, dstap=dstap: nc.tensor.transpose(
                                        out=pt[:, i, :], in_=dstap[:, (c0 + i) * 128:(c0 + i + 1) * 128],
                                        identity=ident_b), reads=[r_d, r_c], writes=[r_p])
                                if cc < 4:
                                    evac(xs_tm[:, c0:c0 + 4, cc * 128:(cc + 1) * 128], pt[:], [r_p], [r_prep])
                                else:
                                    evac(B_tm[:, c0:c0 + 4, (cc - 4) * 128:(cc - 3) * 128], pt[:], [r_p], [r_prep])

                stc.close()
                P.barrier()
                with ExitStack() as st2:
                    Hb_all = SB(st2, "Hb_all", [128, NC, 2, 256], BF16); r_Hb = R()
                    Hf = [[SB(st2, "Hf%d%d" % (d_, g), [128, 256]) for g in range(2)] for d_ in range(2)]
                    r_H = [[R(), R()], [R(), R()]]
                    Hbf = MB(st2, "Hbf", [128, 256], BF16, 2)
                    Hbf_g = [None, None]
                    xdt = MB(st2, "xdt", [128, 512], BF16, 3)
                    xdte = MB(st2, "xdte", [128, 512], BF16, 3)
                    RH = MB(st2, "RH", [128, 4, 128], F32, 2)
                    Eb = MB(st2, "Eb", [128, 4, 128], BF16, 2)
                    MT = MB(st2, "MT", [128, 4, 128], BF16, 2)
                    cbm = MB(st2, "cbm", [128, 128], BF16, 4)
                    yacc = MB(st2, "yacc", [128, 512], F32, 2)
                    ytmp = MB(st2, "ytmp", [128, 256], F32, 2)
                    zt = MB(st2, "zt", [128, 512], F32, 3)
                    gsc = SB(st2, "gsc", [128, 512]); r_gsc = R()
                    gst = MB(st2, "gst", [128, 2], F32, 2)
                    yo = MB(st2, "yo", [128, 512], BF16, 2)
                    pS = MB(st2, "pS", [128, 256], F32, 2, psum=True)
                    pCB = MB(st2, "pCB", [128, 128], F32, 1, psum=True)
                    pSeg = MB(st2, "pSeg", [128, 512], F32, 2, psum=True)
                    pY = MB(st2, "pY", [128, 256], F32, 2, psum=True)
                    pYo = MB(st2, "pYo", [128, 256], F32, 1, psum=True)
                    for d_ in range(2):
                        for g in range(2):
                            P.op("pool", lambda d_=d_, g=g: nc.gpsimd.memset(Hf[d_][g][:], 0.0), writes=[r_H[d_][g]])

                    def xdt_ops(c, d_):
                        a, r_a = xdt.nxt()
                        b, r_b = xdte.nxt()
                        h0 = 8 * d_
                        P.op("pool", lambda: nc.gpsimd.tensor_tensor(
                            out=a[:].rearrange("p (h e) -> p h e", h=8),
                            in0=xs_tm[:, c, :].rearrange("p (h e) -> p h e", h=8),
                            in1=dtt[:, c, h0:h0 + 8].unsqueeze(2).to_broadcast([128, 8, 64]), op=ALU.mult),
                            reads=[r_prep, r_dtp], writes=[r_a])
                        P.op("pool", lambda: nc.gpsimd.tensor_tensor(
                            out=b[:].rearrange("p (h e) -> p h e", h=8),
                            in0=a[:].rearrange("p (h e) -> p h e", h=8),
                            in1=dte[:, c, h0:h0 + 8].unsqueeze(2).to_broadcast([128, 8, 64]), op=ALU.mult),
                            reads=[r_a, r_dtp], writes=[r_b])
                        return (a, r_a), (b, r_b)

                    def state_update(c, d_, g, xe, r_xe):
                        pt, r_p = pS.nxt()
                        P.op("pe", lambda: nc.tensor.matmul(pt[:], lhsT=B_tm[:, c, g * 128:(g + 1) * 128],
                                                            rhs=xe[:, g * 256:(g + 1) * 256], start=True, stop=True),
                             reads=[r_prep, r_xe], writes=[r_p])
                        h0 = 8 * d_ + 4 * g
                        H = Hf[d_][g]
                        P.op("dve", lambda: nc.vector.tensor_tensor(
                            out=H[:].rearrange("p (h e) -> p h e", h=4), in0=H[:].rearrange("p (h e) -> p h e", h=4),
                            in1=cd[:, c, h0:h0 + 4].unsqueeze(2).to_broadcast([128, 4, 64]), op=ALU.mult),
                            reads=[r_H[d_][g], r_dtp], writes=[r_H[d_][g]])
                        P.op("dve", lambda: nc.vector.tensor_tensor(out=H[:], in0=H[:], in1=pt[:], op=ALU.add),
                             reads=[r_H[d_][g], r_p], writes=[r_H[d_][g]])

                    for c in range(NC - 1, -1, -1):
                        for g in range(2):
                            P.op("act", lambda c=c, g=g: nc.scalar.copy(out=Hb_all[:, c, g, :], in_=Hf[1][g][:]),
                                 reads=[r_H[1][g]], writes=[r_Hb])
                        if c > 0:
                            (_, _), (xe, r_xe) = xdt_ops(c, 1)
                            for g in range(2):
                                state_update(c, 1, g, xe, r_xe)

                    zq = []

                    def load_z(c):
                        z, r_zt = zt.nxt()
                        dma("sp", z[:], z_d[c * 128:(c + 1) * 128, :], reads=[r_z], writes=[r_zt])
                        zq.append((z, r_zt))
                    load_z(0)
                    for c in range(NC):
                        if c + 1 < NC:
                            load_z(c + 1)
                        ya, r_ya = yacc.nxt()
                        P.op("dve", lambda ya=ya, c=c: nc.vector.tensor_tensor(
                            out=ya[:].rearrange("p (h e) -> p h e", h=8),
                            in0=xs_tm[:, c, :].rearrange("p (h e) -> p h e", h=8),
                            in1=dsk_t[:].unsqueeze(2).to_broadcast([128, 8, 64]), op=ALU.mult),
                            reads=[r_prep, r_dtp], writes=[r_ya])
                        xd = [None, None]
                        for d_ in range(2):
                            xd[d_] = xdt_ops(c, d_)
                        def run_combos(c, ya, r_ya, xd):
                            pre = {}

                            def preG(g):
                                pcb, r_pcb = pCB.nxt()
                                P.op("pe", lambda: nc.tensor.matmul(
                                    pcb[:], lhsT=BT[:, g, c * 128:(c + 1) * 128], rhs=CT[:, g, c * 128:(c + 1) * 128],
                                    start=True, stop=True), reads=[r_prep], writes=[r_pcb])
                                cms = []
                                for d_ in range(2):
                                    cm, r_cm = cbm.nxt()
                                    P.op("dve", lambda cm=cm, d_=d_: nc.vector.tensor_tensor(
                                        out=cm[:], in0=pcb[:], in1=CF(CF_MF + d_), op=ALU.mult),
                                        reads=[r_pcb, r_c], writes=[r_cm])
                                    cms.append((cm, r_cm))
                                hb, r_hb = Hbf.nxt()
                                P.op("act", lambda: nc.scalar.copy(out=hb[:], in_=Hf[0][g][:]),
                                     reads=[r_H[0][g]], writes=[r_hb])
                                pre[g] = (cms, hb, r_hb)

                            cst = {}

                            def stX(g, d_):
                                h0 = 8 * d_ + 4 * g
                                rh, r_rh = RH.nxt()
                                P.op("pool", lambda: nc.gpsimd.tensor_tensor(
                                    out=rh[:], in0=CF(CF_TF + d_).unsqueeze(1).to_broadcast([128, 4, 128]),
                                    in1=adt[:, c, h0:h0 + 4].unsqueeze(2).to_broadcast([128, 4, 128]), op=ALU.mult),
                                    reads=[r_c, r_dtp], writes=[r_rh])
                                cst[(g, d_)] = dict(rh=(rh, r_rh))

                            def stY(g, d_):
                                rh, r_rh = cst[(g, d_)]["rh"]
                                psg, r_psg = pSeg.nxt()
                                P.op("pe", lambda: nc.tensor.matmul(
                                    psg[:], lhsT=CF(CF_UF + d_), rhs=rh[:].rearrange("p h l -> p (h l)"),
                                    start=True, stop=True), reads=[r_rh, r_c], writes=[r_psg])
                                eb, r_eb = Eb.nxt()
                                P.op("act", lambda: nc.scalar.activation(
                                    out=eb[:].rearrange("p h l -> p (h l)"), in_=psg[:], func=AF.Exp),
                                    reads=[r_psg], writes=[r_eb])
                                mt, r_mt = MT.nxt()
                                cm, r_cm = pre[g][0][d_]
                                P.op("dve", lambda: nc.vector.tensor_tensor(
                                    out=mt[:], in0=eb[:], in1=cm[:].unsqueeze(1).to_broadcast([128, 4, 128]),
                                    op=ALU.mult), reads=[r_eb, r_cm], writes=[r_mt])
                                cst[(g, d_)]["mt"] = (mt, r_mt)

                            def stZ(g, d_):
                                h0 = 8 * d_ + 4 * g
                                mt, r_mt = cst[(g, d_)]["mt"]
                                (xa, r_xa), (xe, r_xe) = xd[d_]
                                _, hb, r_hb = pre[g]
                                py, r_py = pY.nxt()
                                for r_ in range(4):
                                    hh = 4 * g + r_
                                    P.op("pe", lambda r_=r_, hh=hh: nc.tensor.matmul(
                                        py[:, r_ * 64:(r_ + 1) * 64], lhsT=mt[:, r_, :], rhs=xa[:, hh * 64:(hh + 1) * 64],
                                        start=True, stop=True), reads=[r_mt, r_xa], writes=[r_py])
                                pyo, r_pyo = pYo.nxt()
                                if d_ == 0:
                                    rhs_h, r_rhs = hb[:], r_hb
                                else:
                                    rhs_h, r_rhs = Hb_all[:, c, g, :], r_Hb
                                P.op("pe", lambda: nc.tensor.matmul(
                                    pyo[:], lhsT=CT[:, g, c * 128:(c + 1) * 128], rhs=rhs_h, start=True, stop=True),
                                    reads=[r_prep, r_rhs], writes=[r_pyo])
                                yt, r_yt = ytmp.nxt()
                                P.op("dve", lambda: nc.vector.tensor_tensor(
                                    out=yt[:].rearrange("p (h e) -> p h e", h=4),
                                    in0=pyo[:].rearrange("p (h e) -> p h e", h=4),
                                    in1=ecs[:, c, h0:h0 + 4].unsqueeze(2).to_broadcast([128, 4, 64]), op=ALU.mult),
                                    reads=[r_pyo, r_dtp], writes=[r_yt])
                                yg = ya[:, g * 256:(g + 1) * 256]
                                P.op("dve", lambda: nc.vector.tensor_tensor(out=yg, in0=yg, in1=yt[:], op=ALU.add),
                                     reads=[r_yt, r_ya], writes=[r_ya])
                                P.op("dve", lambda: nc.vector.tensor_tensor(out=yg, in0=yg, in1=py[:], op=ALU.add),
                                     reads=[r_py, r_ya], writes=[r_ya])

                            preG(0)
                            preG(1)
                            K4 = [(0, 0), (0, 1), (1, 0), (1, 1)]
                            stX(*K4[0]); stX(*K4[1]); stY(*K4[0]); stX(*K4[2]); stY(*K4[1]); stZ(*K4[0])
                            stX(*K4[3]); stY(*K4[2]); stZ(*K4[1]); stY(*K4[3]); stZ(*K4[2]); stZ(*K4[3])
                            if c < NC - 1:
                                for g in range(2):
                                    state_update(c, 0, g, xd[0][1][0], xd[0][1][1])

                        run_combos(c, ya, r_ya, xd)
                        z, r_zt = zq.pop(0)
                        P.op("act", lambda z=z: nc.scalar.activation(out=z[:], in_=z[:], func=AF.Silu), reads=[r_zt], writes=[r_zt])
                        P.op("dve", lambda ya=ya, z=z: nc.vector.tensor_tensor(out=ya[:], in0=ya[:], in1=z[:], op=ALU.mult),
                             reads=[r_zt, r_ya], writes=[r_ya])
                        gs, r_gs = gst.nxt()
                        rms_rstd("g", ya[:], gs[:, 0:1], gs[:, 1:2], gsc[:], r_ya, r_gs, 512)
                        yob, r_yo = yo.nxt()
                        P.op("dve", lambda yob=yob, ya=ya, gs=gs: nc.vector.scalar_tensor_tensor(
                            out=yob[:], in0=ya[:], scalar=gs[:, 1:2], in1=snw_t[:], op0=ALU.mult, op1=ALU.mult),
                            reads=[r_ya, r_gs, r_dtp], writes=[r_yo])
                        dma("sp", mix_d[c * 128:(c + 1) * 128, 0:512], yob[:], reads=[r_yo], writes=[r_mix])

            P.barrier()
            with ExitStack() as st:
                QA = [MB(st, "QA%d" % c_, [67, L], BF16, 2) for c_ in range(2)]
                KAa = [MB(st, "KAa%d" % c_, [67, L], BF16, 2) for c_ in range(2)]
                KAb = [MB(st, "KAb%d" % c_, [67, L], BF16, 2) for c_ in range(2)]
                VA = MB(st, "VA", [128, NC, 129], BF16, 2)
                btab_t = MB(st, "btab_t", [128, NC * NQB], F32, 2)
                bias_t = [MB(st, "bias_t%d" % c_, [128, NC * NQB], F32, 2) for c_ in range(2)]
                lam_t = SB(st, "lam_t", [128, 4, 64]); r_lam = R()
                lam_s = SB(st, "lam_s", [128, 4])
                subw_t = SB(st, "subw_t", [128, 128])
                nrmb = MB(st, "nrmb", [128, 2, 16], F32, 2)
                nrm = SB(st, "nrm", [128, 8]); r_nrm = R()
                Mbc = MB(st, "Mbc", [128, 2], F32, 2)
                mdiag = SB(st, "mdiag", [128, 2])
                PT = [MB(st, "PT%d" % c_, [128, 512], BF16, 3) for c_ in range(2)]
                osq2 = MB(st, "osq2", [128, 128], F32, 2)
                Sfix = MB(st, "Sfix", [128, 512], F32, 2)
                pSc = [MB(st, "pSc%d" % c_, [128, 512], F32, 2, psum=True) for c_ in range(2)]
                pO = MB(st, "pO", [128, 2, 256], F32, 4, psum=True)
                osb = MB(st, "osb", [128, 2, 129], F32, 5)
                ot = MB(st, "ot", [128, 128], F32, 2)
                ost = MB(st, "ost", [128, 4], F32, 8)
                osq = SB(st, "osq", [128, 128]); r_osq = R()
                ob = MB(st, "ob", [128, 128], BF16, 5)
                zf_list = list(range(0, na, 2)) if s == 0 else []
                zf_per = -(-len(zf_list) // (4 * NQB)) if zf_list else 0
                dma("sp", lam_t[:], lam.rearrange("a d -> (a d)").partition_broadcast(128), writes=[r_lam])
                dma("sp", subw_t[:], sub_w.partition_broadcast(128), writes=[r_lam])
                P.op("dve", lambda: nc.vector.tensor_tensor(out=lam_t[:, 0:2, :], in0=lam_t[:, 0:2, :], in1=lam_t[:, 2:4, :], op=ALU.mult),
                     reads=[r_lam], writes=[r_lam])
                P.op("dve", lambda: nc.vector.reduce_sum(out=lam_s[:, 0:2], in_=lam_t[:, 0:2, :], axis=AX.X), reads=[r_lam], writes=[r_lam])
                P.op("act", lambda: nc.scalar.activation(out=lam_s[:, 0:2], in_=lam_s[:, 0:2], func=AF.Exp), reads=[r_lam], writes=[r_lam])
                P.op("dve", lambda: nc.vector.tensor_tensor(out=lam_s[:, 2:3], in0=lam_s[:, 1:2], in1=lam_s[:, 0:1], op=ALU.subtract),
                     reads=[r_lam], writes=[r_lam])
                P.op("dve", lambda: nc.vector.tensor_scalar(out=lam_s[:, 2:3], in0=lam_s[:, 2:3], scalar1=-LAMBDA_INIT, scalar2=None, op0=ALU.add),
                     reads=[r_lam], writes=[r_lam])
                P.op("dve", lambda: nc.vector.tensor_scalar(out=subw_t[:], in0=subw_t[:], scalar1=1.0 - LAMBDA_INIT, scalar2=None, op0=ALU.mult),
                     reads=[r_lam], writes=[r_lam])
                pN = pSc[0]
                hctx = {}

                def setup_loads(h):
                    qa, ka, kb = [], [], []
                    for c_ in range(2):
                        t_, r_ = QA[c_].nxt(); qa.append((t_, r_))
                        row0 = h * 128 + c_ * 64
                        dma("sp", t_[0:64, :], qT_d[row0:row0 + 64, :], reads=[r_q], writes=[r_])
                        dma("sp", t_[64:67, :], qrows_d[h], writes=[r_])
                        t_, r_ = KAa[c_].nxt(); ka.append((t_, r_))
                        dma("sp", t_[0:64, :], kT_d[row0:row0 + 64, :], reads=[r_k], writes=[r_])
                        dma("sp", t_[64:67, :], krA_d[h], writes=[r_])
                        t_, r_ = KAb[c_].nxt(); kb.append((t_, r_))
                        dma("sp", t_[0:64, :], kT_d[row0:row0 + 64, :], reads=[r_k], writes=[r_])
                        dma("sp", t_[64:67, :], krB_d[h], writes=[r_])
                    va, r_va = VA.nxt()
                    dma("sp", va[:, :, 0:128], v_d[:, h * 128:(h + 1) * 128].rearrange("(c p) e -> p c e", p=128),
                        reads=[r_v], writes=[r_va])
                    P.op("pool", lambda va=va: nc.gpsimd.memset(va[:, :, 128:129], 1.0), writes=[r_va])
                    bt, r_bt = btab_t.nxt()
                    dma("sp", bt[:], btab_d[h:h + 1, :].partition_broadcast(128), writes=[r_bt])
                    hpre[h] = dict(qa=qa, ka=ka, kb=kb, va=(va, r_va), bt=(bt, r_bt))

                def setup_final(h):
                    pr = hpre.pop(h)
                    qa, ka, kb, bt, r_bt = pr["qa"], pr["ka"], pr["kb"], pr["bt"][0], pr["bt"][1]
                    nr, r_nr = nrmb.nxt()
                    dma("sp", nr[:, 0, :], nmax_d[0:1, :].partition_broadcast(128), writes=[r_nr])
                    dma("sp", nr[:, 1, :], nmax_d[64:65, :].partition_broadcast(128), writes=[r_nr])
                    mb, r_mb = Mbc.nxt()
                    P.op("dve", lambda: nc.vector.tensor_tensor(out=mb[:], in0=nr[:, :, h], in1=nr[:, :, 4 + h], op=ALU.mult),
                         reads=[r_nr], writes=[r_mb])
                    P.op("pool", lambda: nc.gpsimd.tensor_tensor(out=mb[:], in0=mb[:], in1=poshalf[:, 0:1].to_broadcast([128, 2]), op=ALU.pow),
                         reads=[r_mb, r_c], writes=[r_mb])
                    P.op("dve", lambda: nc.vector.tensor_scalar(out=mb[:], in0=mb[:], scalar1=-0.125 * 1.02, scalar2=None, op0=ALU.mult),
                         reads=[r_mb], writes=[r_mb])
                    bi = []
                    for c_ in range(2):
                        b_, r_b = bias_t[c_].nxt()
                        P.op("dve", lambda b_=b_, c_=c_: nc.vector.tensor_scalar(
                            out=b_[:], in0=bt[:], scalar1=mb[:, c_:c_ + 1], scalar2=None, op0=ALU.add),
                            reads=[r_bt, r_mb], writes=[r_b])
                        bi.append((b_, r_b))
                    hctx[h] = dict(qa=qa, ka=ka, kb=kb, va=pr["va"], bi=bi, s8=8.0 * (2.0 ** (-8.0 * (h + 1) / 4)))

                hpre = {}
                def kt_order(qb):
                    dg = [kt for kt in range(NC) if 4 * qb <= kt < 4 * qb + 4]
                    return [kt for kt in range(NC) if kt not in dg] + dg
                steps = []
                for h in range(4):
                    for qb in range(NQB):
                        od = kt_order(qb)
                        for pos, kt in enumerate(od):
                            for c_ in range(2):
                                steps.append(dict(h=h, qb=qb, kt=kt, c=c_, first=(pos == 0), last=(pos == NC - 1)))
                qctx = {}

                def emit_qk(sp_):
                    h, qb, kt, c_ = sp_["h"], sp_["qb"], sp_["kt"], sp_["c"]
                    cx = hctx[h]
                    caseB = kt >= 4 * qb + 4
                    diag = (4 * qb <= kt < 4 * qb + 4)
                    kop, r_kop = (cx["kb"] if caseB else cx["ka"])[c_]
                    qop, r_qop = cx["qa"][c_]
                    ps_, r_ps = pSc[c_].nxt()
                    P.op("pe", lambda: nc.tensor.matmul(
                        ps_[:], lhsT=kop[:, kt * 128:(kt + 1) * 128], rhs=qop[:, qb * 512:(qb + 1) * 512],
                        start=True, stop=True), reads=[r_kop, r_qop], writes=[r_ps])
                    src_ap, r_src = ps_[:], r_ps
                    if diag:
                        sf, r_sf = Sfix.nxt()
                        dk = kt - 4 * qb
                        s8 = cx["s8"]
                        P.op("dve", lambda: nc.vector.scalar_tensor_tensor(
                            out=sf[:], in0=CF(CF_D2 + 4 * dk, 4), scalar=-s8, in1=ps_[:], op0=ALU.mult, op1=ALU.add),
                            reads=[r_ps, r_c], writes=[r_sf])
                        src_ap, r_src = sf[:], r_sf
                    sp_["src"] = (src_ap, r_src)

                def emit_exp(sp_):
                    h, qb, kt, c_ = sp_["h"], sp_["qb"], sp_["kt"], sp_["c"]
                    src_ap, r_src = sp_["src"]
                    pt_, r_pt = PT[c_].nxt()
                    b_, r_b = hctx[h]["bi"][c_]
                    col = kt * NQB + qb
                    P.op("act", lambda: nc.scalar.activation(
                        out=pt_[:], in_=src_ap, func=AF.Exp, bias=b_[:, col:col + 1], scale=0.125),
                        reads=[r_src, r_b], writes=[r_pt])
                    sp_["pt"] = (pt_, r_pt)

                def emit_pv(sp_):
                    h, qb, kt, c_ = sp_["h"], sp_["qb"], sp_["kt"], sp_["c"]
                    if sp_["first"] and c_ == 0:
                        qctx[(h, qb)] = [pO.nxt() for _ in range(4)]
                    po = qctx[(h, qb)]
                    pt_, r_pt = sp_["pt"]
                    va, r_va = hctx[h]["va"]
                    for sub in range(4):
                        P.op("pe", lambda sub=sub: nc.tensor.matmul(
                            po[sub][0][:, c_, 0:129], lhsT=pt_[:, sub * 128:(sub + 1) * 128], rhs=va[:, kt, :],
                            start=(sp_["first"] and c_ == 0), stop=(sp_["last"] and c_ == 1), skip_group_check=True),
                            reads=[r_pt, r_va], writes=[po[sub][1]])

                def emit_epilogue(h, qb):
                    po = qctx.pop((h, qb))
                    for _ in range(zf_per):
                        if zf_list:
                            a0 = zf_list.pop(0)
                            dma("sp", xs_v[:, a0:a0 + 2, :], zero_bf[:].rearrange("p (a d) -> p a d", a=2), reads=[r_c])
                    os_l = []
                    for sub in range(4):
                        pot, r_po = po[sub]
                        o_, r_o = osb.nxt()
                        evac(o_[:], pot[:, :, 0:129], [r_po], [r_o], eng="dve")
                        os_l.append((o_, r_o))
                    for sub in range(4):
                        o_, r_o = os_l[sub]
                        t0 = qb * 512 + sub * 128
                        os_, r_os = ost.nxt()
                        P.op("dve", lambda o_=o_, os_=os_: nc.vector.reciprocal(out=os_[:, 0:2], in_=o_[:, :, 128]),
                             reads=[r_o], writes=[r_os])
                        P.op("dve", lambda os_=os_: nc.vector.tensor_tensor(out=os_[:, 1:2], in0=os_[:, 1:2], in1=lam_s[:, 2:3], op=ALU.mult),
                             reads=[r_os, r_lam], writes=[r_os])
                        oo, r_oo = ot.nxt()
                        P.op("dve", lambda oo=oo, o_=o_, os_=os_: nc.vector.tensor_scalar(
                            out=oo[:], in0=o_[:, 0, 0:128], scalar1=os_[:, 0:1], scalar2=None, op0=ALU.mult),
                            reads=[r_o, r_os], writes=[r_oo])
                        P.op("dve", lambda oo=oo, o_=o_, os_=os_: nc.vector.scalar_tensor_tensor(
                            out=oo[:], in0=o_[:, 1, 0:128], scalar=os_[:, 1:2], in1=oo[:], op0=ALU.mult, op1=ALU.add),
                            reads=[r_o, r_os, r_oo], writes=[r_oo])
                        oq, r_oq = osq2.nxt()
                        P.op("dve", lambda oo=oo, oq=oq: nc.vector.tensor_tensor(out=oq[:], in0=oo[:], in1=oo[:], op=ALU.mult),
                             reads=[r_oo], writes=[r_oq])
                        P.op("dve", lambda oq=oq, os_=os_: nc.vector.reduce_sum(out=os_[:, 2:3], in_=oq[:], axis=AX.X),
                             reads=[r_oq], writes=[r_os])
                        P.op("dve", lambda os_=os_: nc.vector.tensor_scalar(out=os_[:, 3:4], in0=os_[:, 2:3], scalar1=1.0 / 128, scalar2=EPS,
                                                                         op0=ALU.mult, op1=ALU.add), reads=[r_os], writes=[r_os])
                        P.op("pool", lambda os_=os_: nc.gpsimd.tensor_tensor(out=os_[:, 3:4], in0=os_[:, 3:4], in1=neghalf[:, 0:1], op=ALU.pow),
                             reads=[r_os, r_c], writes=[r_os])
                        ob_, r_ob = ob.nxt()
                        P.op("dve", lambda ob_=ob_, oo=oo, os_=os_: nc.vector.scalar_tensor_tensor(
                            out=ob_[:], in0=oo[:], scalar=os_[:, 3:4], in1=subw_t[:], op0=ALU.mult, op1=ALU.mult),
                            reads=[r_oo, r_os, r_lam], writes=[r_ob])
                        dma("pool", mix_d[t0:t0 + 128, 512 + h * 128:512 + (h + 1) * 128], ob_[:], reads=[r_ob], writes=[r_mix])

                setup_loads(0)
                setup_final(0)
                AHEAD, LAG = 2, 2
                nst = len(steps)
                per_head = NQB * NC * 2
                pend_parts = []
                for i in range(min(AHEAD, nst)):
                    emit_qk(steps[i])
                for i in range(nst + LAG):
                    if i < nst:
                        sp_ = steps[i]
                        hh = sp_["h"]
                        rel_i = i - hh * per_head
                        if hh + 1 < 4:
                            if rel_i == 2:
                                setup_loads(hh + 1)
                            if rel_i == per_head // 2:
                                setup_final(hh + 1)
                        emit_exp(sp_)
                        if i + AHEAD < nst:
                            emit_qk(steps[i + AHEAD])
                    j = i - LAG
                    if j >= 0:
                        sj = steps[j]
                        emit_pv(sj)
                        if sj["last"] and sj["c"] == 1:
                            emit_epilogue(sj["h"], sj["qb"])

            P.barrier()
            with ExitStack() as st:
                wo = SB(st, "wo", [128, 8, D], BF16); r_wo = R()
                wov = w_out.rearrange("(kc p) f -> p kc f", p=128)
                for c0 in range(0, D, 512):
                    dma("pool", wo[:, :, c0:c0 + 512], wov[:, :, c0:c0 + 512], writes=[r_wo])
                wr_t = SB(st, "wr_t", [128, 8, 36]); br_t = SB(st, "br_t", [128, 36])
                nfw_t = SB(st, "nfw_t", [128, D])
                dma("sp", nfw_t[:], nfw.partition_broadcast(128), writes=[r_wo])
                dma("sp", wr_t[:], wr.rearrange("(kc p) f -> p kc f", p=128), writes=[r_wo])
                dma("sp", br_t[:], br.partition_broadcast(128), writes=[r_wo])
                mx = MB(st, "mx", [128, D], BF16, 4)
                mxT = MB(st, "mxT", [128, 8, 128], BF16, 2)
                xt = MB(st, "xt2", [128, D], F32, 4)
                ht = MB(st, "ht", [128, D], F32, 2)
                sq = SB(st, "sq2", [128, D]); r_sq = R()
                stat = MB(st, "stat2", [128, 2], F32, 2)
                xn = MB(st, "xn", [128, D], F32, 3)
                xnb = MB(st, "xnb", [128, D], BF16, 2)
                xnT = MB(st, "xnT", [128, 8, 128], F32, 3)
                pT = MB(st, "pT2", [128, 8, 128], BF16, 1, psum=True)
                pH = MB(st, "pH", [128, 512], F32, 2, psum=True)
                pTf = MB(st, "pTf", [128, 4, 128], F32, 2, psum=True)
                pL = MB(st, "pL", [128, 64], F32, 2, psum=True)
                lg_all = SB(st, "lg_all", [128, NC, 36])
                r_lg = R()

                ldq = []

                def loadD(c):
                    t0 = c * 128
                    m_, r_m = mx.nxt()
                    dma("sp", m_[:], mix_d[t0:t0 + 128, :], reads=[r_mix], writes=[r_m])
                    x_, r_x = xt.nxt()
                    dma("sp", x_[:], x[s, t0:t0 + 128, :], writes=[r_x])
                    ldq.append((m_, r_m, x_, r_x))

                def stageA1(c):
                    ti = s * NC + c
                    if c + 2 < NC:
                        loadD(c + 2)
                    m_, r_m, x_, r_x = ldq.pop(0)
                    if dbg:
                        dma("pool", dbg_d["dbg_mix"][ti * 128:(ti + 1) * 128, :], m_[:], reads=[r_m])
                    pt, r_p = pT.nxt()
                    for k in range(8):
                        P.op("pe", lambda k=k, pt=pt, m_=m_: nc.tensor.transpose(
                            out=pt[:, k, :], in_=m_[:, k * 128:(k + 1) * 128], identity=ident_b), reads=[r_m, r_c], writes=[r_p])
                    mt_, r_mt = mxT.nxt()
                    evac(mt_[:], pt[:], [r_p], [r_mt])
                    return mt_, r_mt, x_, r_x

                def stageA2(c, mt_, r_mt, x_, r_x):
                    ti = s * NC + c
                    h_, r_ht = ht.nxt()
                    for half in range(2):
                        ph, r_ph = pH.nxt()
                        for k in range(8):
                            P.op("pe", lambda k=k, ph=ph, half=half: nc.tensor.matmul(
                                ph[:], lhsT=mt_[:, k, :], rhs=wo[:, k, half * 512:(half + 1) * 512], start=(k == 0), stop=(k == 7)),
                                reads=[r_mt, r_wo], writes=[r_ph])
                        P.op("dve", lambda h_=h_, ph=ph, half=half: nc.vector.tensor_tensor(
                            out=h_[:, half * 512:(half + 1) * 512], in0=x_[:, half * 512:(half + 1) * 512], in1=ph[:], op=ALU.add),
                            reads=[r_x, r_ph], writes=[r_ht])
                    dma("sp", h_d[ti * 128:(ti + 1) * 128, :], h_[:], reads=[r_ht], writes=[r_h])
                    if dbg:
                        dma("sp", dbg_d["dbg_h"][ti * 128:(ti + 1) * 128, :], h_[:], reads=[r_ht])
                    st_, r_st = stat.nxt()
                    rms_rstd("f", h_[:], st_[:, 0:1], st_[:, 1:2], sq[:], r_ht, r_st, D)
                    xn_, r_xn_ = xn.nxt()
                    P.op("dve", lambda: nc.vector.scalar_tensor_tensor(
                        out=xn_[:], in0=h_[:], scalar=st_[:, 1:2], in1=nfw_t[:], op0=ALU.mult, op1=ALU.mult),
                        reads=[r_ht, r_st, r_wo], writes=[r_xn_])
                    xb_, r_xb = xnb.nxt()
                    P.op("act", lambda: nc.scalar.copy(out=xb_[:], in_=xn_[:]), reads=[r_xn_], writes=[r_xb])
                    dma("sp", xn_d[ti * 128:(ti + 1) * 128, :], xb_[:], reads=[r_xb], writes=[r_xn])
                    return xn_, r_xn_

                def stageB1(c, xn_, r_xn_):
                    xT_, r_xT = xnT.nxt()
                    for k0 in range(0, 8, 4):
                        ptf, r_ptf = pTf.nxt()
                        for k in range(4):
                            P.op("pe", lambda k=k, k0=k0, ptf=ptf: nc.tensor.transpose(
                                out=ptf[:, k, :], in_=xn_[:, (k0 + k) * 128:(k0 + k + 1) * 128], identity=ident_f),
                                reads=[r_xn_, r_c], writes=[r_ptf])
                        evac(xT_[:, k0:k0 + 4, :], ptf[:], [r_ptf], [r_xT])
                    return xT_, r_xT

                def stageB2(c, xT_, r_xT):
                    pl, r_pl = pL.nxt()
                    for k in range(8):
                        P.op("pe", lambda k=k: nc.tensor.matmul(
                            pl[:, 0:36], lhsT=xT_[:, k, :], rhs=wr_t[:, k, :], start=(k == 0), stop=(k == 7)),
                            reads=[r_xT, r_wo], writes=[r_pl])
                    P.op("dve", lambda: nc.vector.tensor_tensor(out=lg_all[:, c, :], in0=pl[:, 0:36], in1=br_t[:], op=ALU.add),
                         reads=[r_pl, r_wo], writes=[r_lg])

                loadD(0)
                if NC > 1:
                    loadD(1)
                pend = stageA2(0, *stageA1(0))
                for c in range(NC):
                    a1 = stageA1(c + 1) if c + 1 < NC else None
                    b1 = stageB1(c, *pend)
                    nxt_ = stageA2(c + 1, *a1) if a1 is not None else None
                    stageB2(c, *b1)
                    pend = nxt_

                def route_batch(T0, lg_all, st, r_lg):
                    V = nc.vector
                    RW = [r_lg, r_route]

                    def dv(f):
                        P.op("dve", f, reads=RW, writes=RW)
                    q8 = SB(st, "q8", [128, 8, NC])
                    g4 = SB(st, "g4", [128, NC, 4])
                    me = SB(st, "me", [128, NC, NE])
                    lgG = lg_all[:, :, 0:4]
                    lgE = lg_all[:, :, 4:36]
                    b4 = lambda ap: ap.unsqueeze(2).to_broadcast([128, NC, 4])
                    b32 = lambda ap: ap.unsqueeze(2).to_broadcast([128, NC, NE])
                    o1 = oh1_all[:, T0:T0 + NC, :]
                    o2 = oh2_all[:, T0:T0 + NC, :]
                    dv(lambda: V.reduce_max(out=q8[:, 0, :], in_=lgG, axis=AX.X))
                    dv(lambda: V.tensor_tensor(out=g4[:], in0=lgG, in1=b4(q8[:, 0, :]), op=ALU.subtract))
                    P.op("act", lambda: nc.scalar.activation(out=g4[:], in_=g4[:], func=AF.Exp), reads=RW, writes=RW)
                    dv(lambda: V.reduce_sum(out=q8[:, 1, :], in_=g4[:], axis=AX.X))
                    dv(lambda: V.reciprocal(out=q8[:, 2, :], in_=q8[:, 1, :]))
                    dv(lambda: V.tensor_tensor(out=g4[:], in0=lgG, in1=b4(q8[:, 0, :]), op=ALU.is_equal))
                    dv(lambda: V.tensor_scalar(out=g4[:], in0=g4[:], scalar1=-1.0, scalar2=NEGBIG, op0=ALU.add, op1=ALU.mult))
                    for g in range(4):
                        dv(lambda g=g: V.tensor_tensor(out=me[:, :, g * 8:(g + 1) * 8], in0=lgE[:, :, g * 8:(g + 1) * 8],
                                                       in1=g4[:, :, g].unsqueeze(2).to_broadcast([128, NC, 8]), op=ALU.add))
                    dv(lambda: V.reduce_max(out=q8[:, 3, :], in_=me[:], axis=AX.X))
                    dv(lambda: V.tensor_tensor(out=o1, in0=me[:], in1=b32(q8[:, 3, :]), op=ALU.is_equal))
                    dv(lambda: V.scalar_tensor_tensor(out=me[:], in0=o1, scalar=-NEGBIG, in1=me[:], op0=ALU.mult, op1=ALU.add))
                    dv(lambda: V.reduce_max(out=q8[:, 4, :], in_=me[:], axis=AX.X))
                    dv(lambda: V.tensor_tensor(out=o2, in0=me[:], in1=b32(q8[:, 4, :]), op=ALU.is_equal))
                    dv(lambda: V.tensor_tensor(out=q8[:, 5, :], in0=q8[:, 4, :], in1=q8[:, 3, :], op=ALU.subtract))
                    P.op("act", lambda: nc.scalar.activation(out=q8[:, 5, :], in_=q8[:, 5, :], func=AF.Exp), reads=RW, writes=RW)
                    dv(lambda: V.tensor_scalar(out=q8[:, 6, :], in0=q8[:, 5, :], scalar1=1.0, scalar2=None, op0=ALU.add))
                    dv(lambda: V.reciprocal(out=q8[:, 6, :], in_=q8[:, 6, :]))
                    dv(lambda: V.tensor_tensor(out=g_all[:, T0:T0 + NC, 0], in0=q8[:, 6, :], in1=q8[:, 2, :], op=ALU.mult))
                    dv(lambda: V.tensor_tensor(out=g_all[:, T0:T0 + NC, 1], in0=g_all[:, T0:T0 + NC, 0], in1=q8[:, 5, :], op=ALU.mult))
                    aoh_all = SB(st, "aoh_all", [128, NC, NE], BF16)
                    dv(lambda: V.tensor_tensor(out=aoh_all[:], in0=o1, in1=o2, op=ALU.add))
                    for c in range(NC):
                        ti = T0 + c
                        pl2, r_pl2 = pL.nxt()
                        P.op("pe", lambda pl2=pl2, c=c: nc.tensor.matmul(pl2[:, 0:32], lhsT=tstrict_b, rhs=aoh_all[:, c, :], start=True, stop=True),
                             reads=RW + [r_c], writes=[r_pl2])
                        P.op("pe", lambda pl2=pl2, c=c: nc.tensor.matmul(pl2[:, 32:64], lhsT=ones_b, rhs=aoh_all[:, c, :], start=True, stop=True),
                             reads=RW + [r_c], writes=[r_pl2])
                        P.op("dve", lambda pl2=pl2, ti=ti: V.tensor_tensor(out=rank_all[:, ti, :], in0=pl2[:, 0:32], in1=carry[:], op=ALU.add),
                             reads=[r_pl2, r_route], writes=[r_route])
                        P.op("dve", lambda pl2=pl2: V.tensor_tensor(out=carry[:], in0=carry[:], in1=pl2[:, 32:64], op=ALU.add),
                             reads=[r_pl2, r_route], writes=[r_route])


                route_batch(s * NC, lg_all, st, r_lg)

        for s_ in range(NSEQ):
            seq_body(s_)

        P.barrier()
        with ExitStack() as st:
            V = nc.vector
            pe_ = SB(st, "pe_", [128, NE]); ps_a = SB(st, "ps_a", [128, NE]); ps_b = SB(st, "ps_b", [128, NE])
            tmpb = SB(st, "tmpb", [128, NT, NE])
            slotf = SB(st, "slotf", [128, NT, 2])
            bef = SB(st, "bef", [128, 4]); bdiag = SB(st, "bdiag", [128, 128])
            bebc = SB(st, "bebc", [128, 128])
            idxf = SB(st, "idxf", [128, NB, 4])
            pbc = MB(st, "pbc", [128, 128], F32, 1, psum=True)
            RWR = [r_route]

            def dv(f):
                P.op("dve", f, reads=RWR + [r_c], writes=RWR)
            dv(lambda: V.tensor_scalar(out=pe_[:], in0=carry[:], scalar1=0.0, scalar2=None, op0=ALU.is_gt))
            for m_ in range(1, (T + BS - 1) // BS):
                dv(lambda m_=m_: V.scalar_tensor_tensor(out=pe_[:], in0=carry[:], scalar=float(m_ * BS), in1=pe_[:],
                                                        op0=ALU.is_gt, op1=ALU.add))
            dv(lambda: V.tensor_scalar(out=pe_[:], in0=pe_[:], scalar1=float(BS), scalar2=None, op0=ALU.mult))
            src, dst = pe_, ps_a
            sh = 1
            while sh < NE:
                dv(lambda src=src, dst=dst, sh=sh: V.tensor_copy(out=dst[:, 0:sh], in_=src[:, 0:sh]))
                dv(lambda src=src, dst=dst, sh=sh: V.tensor_tensor(out=dst[:, sh:NE], in0=src[:, sh:NE], in1=src[:, 0:NE - sh], op=ALU.add))
                src, dst = dst, (ps_b if dst is ps_a else ps_a)
                if src is pe_:
                    pass
                sh *= 2
            p_end = src
            p_start = ps_b if p_end is ps_a else ps_a
            dv(lambda: V.tensor_tensor(out=p_start[:], in0=p_end[:], in1=pe_[:], op=ALU.subtract))
            dv(lambda: V.tensor_tensor(out=rank_all[:], in0=rank_all[:], in1=p_start[:].unsqueeze(1).to_broadcast([128, NT, NE]), op=ALU.add))
            for k_, oh in enumerate((oh1_all, oh2_all)):
                dv(lambda oh=oh: V.tensor_tensor(out=tmpb[:], in0=rank_all[:], in1=oh[:], op=ALU.mult))
                dv(lambda k_=k_: V.reduce_sum(out=slotf[:, :, k_], in_=tmpb[:], axis=AX.X))
            dv(lambda: V.tensor_copy(out=slot_i[:], in_=slotf[:]))
            m0 = CF_MISC * 128
            dv(lambda: V.tensor_scalar(out=pe_[:], in0=p_end[:], scalar1=cf[:, m0:m0 + 1], scalar2=None, op0=ALU.is_le))
            dv(lambda: V.reduce_sum(out=bef[:, 0:1], in_=pe_[:], axis=AX.X))
            dv(lambda: V.tensor_scalar(out=bdiag[:], in0=ident_f, scalar1=bef[:, 0:1], scalar2=None, op0=ALU.mult))
            pb, r_pb = pbc.nxt()
            P.op("pe", lambda: nc.tensor.matmul(pb[:], lhsT=ones_f, rhs=bdiag[:], start=True, stop=True), reads=RWR + [r_c], writes=[r_pb])
            P.op("dve", lambda: V.tensor_copy(out=bebc[:], in_=pb[:]), reads=[r_pb], writes=RWR)
            beadj = SB(st, "beadj", [128, 128])
            same = SB(st, "same", [128, 128])
            dv(lambda: V.tensor_copy(out=beadj[:], in_=bebc[:]))
            if NB > 2:
                dv(lambda: V.tensor_tensor(out=same[:, 2:NB], in0=bebc[:, 2:NB], in1=bebc[:, 0:NB - 2], op=ALU.is_equal))
                dv(lambda: V.scalar_tensor_tensor(out=beadj[:, 2:NB], in0=same[:, 2:NB], scalar=64.0, in1=bebc[:, 2:NB],
                                                  op0=ALU.mult, op1=ALU.add))
            for h2 in range(2):
                dv(lambda h2=h2: V.tensor_scalar(out=idxf[:, :, h2], in0=beadj[:, 0:NB], scalar1=256.0, scalar2=float(h2), op0=ALU.mult, op1=ALU.add))
                dv(lambda h2=h2: V.scalar_tensor_tensor(out=idxf[:, :, h2], in0=cf[:, m0 + 1:m0 + 2].to_broadcast([128, NB]), scalar=2.0,
                                                        in1=idxf[:, :, h2], op0=ALU.mult, op1=ALU.add))
            dv(lambda: V.tensor_copy(out=idx_g[:], in_=idxf[:, :, 0:2]))
            for fc in range(4):
                dv(lambda fc=fc: V.tensor_scalar(out=idxf[:, :, fc], in0=beadj[:, 0:NB], scalar1=512.0, scalar2=float(fc * 128), op0=ALU.mult, op1=ALU.add))
                dv(lambda fc=fc: V.tensor_tensor(out=idxf[:, :, fc], in0=idxf[:, :, fc], in1=cf[:, m0 + 1:m0 + 2].to_broadcast([128, NB]), op=ALU.add))
            dv(lambda: V.tensor_copy(out=idx_d[:], in_=idxf[:]))
            fence_t = SB(st, "fence_t", [128, 8])
            for _ in range(2):
                dv(lambda: V.memset(fence_t[:], 0.0))
            if dbg:
                dbgt = SB(st, "dbgt", [128, NT, 8])
                dv(lambda: V.memset(dbgt[:], 0.0))
                dv(lambda: V.tensor_copy(out=dbgt[:, :, 0:2], in_=slotf[:]))
                dv(lambda: V.tensor_copy(out=dbgt[:, :, 2:4], in_=g_all[:]))
                dv(lambda: V.tensor_copy(out=dbgt[:, :, 4:5], in_=bebc[:, 0:NT].unsqueeze(2)))
                dma("sp", dbg_d["dbg_route"], dbgt[:].rearrange("p a b -> p (a b)"), reads=RWR)
            xl = MB(st, "xl", [128, D], BF16, 3)
            bcs = {}

            def mk_bcs():
                bcs["s"] = nc.gpsimd.alloc_register("bc_slot")
                return nc.gpsimd.reg_mov(bcs["s"], NSLOT - 1)
            P.op("pool", mk_bcs)
            for ti in range(NT):
                t_, r_t = xl.nxt()
                dma("sp", t_[:], xn_d[ti * 128:(ti + 1) * 128, :], reads=[r_xn], writes=[r_t])
                for k_ in range(2):
                    P.op("pool", lambda t_=t_, ti=ti, k_=k_: nc.gpsimd.indirect_dma_start(
                        out=xs_d, out_offset=bass.IndirectOffsetOnAxis(ap=slot_i[:, ti, k_:k_ + 1], axis=0),
                        in_=t_[:], in_offset=None, bounds_check=bcs["s"], oob_is_err=False),
                        reads=[r_t, r_route, r_xs], writes=[r_xs], dma=True)

        P.barrier()
        with ExitStack() as st:
            Wg = MB(st, "Wg", [128, 2, 2048], BF16, 2)
            Wu = MB(st, "Wu", [128, 2, 2048], BF16, 2)
            Wd = MB(st, "Wd", [128, 4, 1024], BF16, 2)
            xb = MB(st, "xb", [128, D], BF16, 2 * NSUB)
            xbT = MB(st, "xbT", [128, 8, BS], BF16, 2)
            hd = MB(st, "hd", [128, 4, BS], BF16, 2)
            sg = MB(st, "sg", [128, BS], F32, 2)
            ysb = MB(st, "ysb", [128, D], BF16, 3)
            pT = MB(st, "pT3", [128, 8, 128], BF16, 2, psum=True)
            pG = MB(st, "pG", [128, BS], F32, 2, psum=True)
            pU = MB(st, "pU", [128, BS], F32, 2, psum=True)
            pYm = MB(st, "pYm", [128, 512], F32, 2, psum=True)
            bcr = {}

            def mk_bc():
                bcr["g"] = nc.gpsimd.alloc_register("bc_g")
                bcr["d"] = nc.gpsimd.alloc_register("bc_d")
                nc.gpsimd.reg_mov(bcr["g"], NE * 256 - 1)
                return nc.gpsimd.reg_mov(bcr["d"], NE * 512 - 1)
            P.op("pool", mk_bc)
            def do_T(xtiles):
                xT_, r_xT = xbT.nxt()
                for sub in range(NSUB):
                    x_, r_x = xtiles[sub]
                    pt, r_p = pT.nxt()
                    for j in range(8):
                        P.op("pe", lambda j=j, pt=pt, x_=x_: nc.tensor.transpose(
                            out=pt[:, j, :], in_=x_[:].rearrange("s (p j) -> s j p", j=8)[:, j, :], identity=ident_b),
                            reads=[r_x, r_c], writes=[r_p])
                    evac(xT_[:, :, sub * 128:(sub + 1) * 128], pt[:], [r_p], [r_xT])
                return xT_, r_xT

            for b in range(NB):
                g_, r_g = Wg.nxt()
                u_, r_u = Wu.nxt()
                d_, r_d = Wd.nxt()
                for h2 in range(2):
                    P.op("pool", lambda g_=g_, b=b, h2=h2: nc.gpsimd.indirect_dma_start(
                        out=g_[:, h2, :], out_offset=None, in_=wg,
                        in_offset=bass.IndirectOffsetOnAxis(ap=idx_g[:, b, h2:h2 + 1], axis=0),
                        bounds_check=bcr["g"], oob_is_err=False),
                        reads=[r_route], writes=[r_g], dma=True)
                    P.op("pool", lambda u_=u_, b=b, h2=h2: nc.gpsimd.indirect_dma_start(
                        out=u_[:, h2, :], out_offset=None, in_=wu,
                        in_offset=bass.IndirectOffsetOnAxis(ap=idx_g[:, b, h2:h2 + 1], axis=0),
                        bounds_check=bcr["g"], oob_is_err=False),
                        reads=[r_route], writes=[r_u], dma=True)
                for fc in range(4):
                    P.op("pool", lambda d_=d_, b=b, fc=fc: nc.gpsimd.indirect_dma_start(
                        out=d_[:, fc, :], out_offset=None, in_=wd,
                        in_offset=bass.IndirectOffsetOnAxis(ap=idx_d[:, b, fc:fc + 1], axis=0),
                        bounds_check=bcr["d"], oob_is_err=False),
                        reads=[r_route], writes=[r_d], dma=True)
                if b == 0:
                    xq = []
                    for sub in range(NSUB):
                        x_, r_x = xb.nxt()
                        dma("sp", x_[:], xs_d[sub * 128:(sub + 1) * 128, :], reads=[r_xs], writes=[r_x])
                        xq.append((x_, r_x))
                    xT_pend = do_T(xq)
                xT_, r_xT = xT_pend
                if b + 1 < NB:
                    xq = []
                    for sub in range(NSUB):
                        x_, r_x = xb.nxt()
                        r0 = (b + 1) * BS + sub * 128
                        dma("sp", x_[:], xs_d[r0:r0 + 128, :], reads=[r_xs], writes=[r_x])
                        xq.append((x_, r_x))
                h_, r_h_ = hd.nxt()
                gv = g_[:].rearrange("p a (j f) -> p (a j) f", f=512)
                uv = u_[:].rearrange("p a (j f) -> p (a j) f", f=512)
                for fc in range(4):
                    pg, r_pg = pG.nxt()
                    pu, r_pu = pU.nxt()
                    for j in range(8):
                        P.op("pe", lambda j=j, pg=pg, gv=gv, fc=fc, xT_=xT_: nc.tensor.matmul(
                            pg[:], lhsT=gv[:, j, fc * 128:(fc + 1) * 128], rhs=xT_[:, j, :], start=(j == 0), stop=(j == 7)),
                            reads=[r_g, r_xT], writes=[r_pg])
                    for j in range(8):
                        P.op("pe", lambda j=j, pu=pu, uv=uv, fc=fc, xT_=xT_: nc.tensor.matmul(
                            pu[:], lhsT=uv[:, j, fc * 128:(fc + 1) * 128], rhs=xT_[:, j, :], start=(j == 0), stop=(j == 7)),
                            reads=[r_u, r_xT], writes=[r_pu])
                    s_, r_s = sg.nxt()
                    P.op("act", lambda s_=s_, pg=pg: nc.scalar.activation(out=s_[:], in_=pg[:], func=AF.Silu), reads=[r_pg], writes=[r_s])
                    P.op("dve", lambda h_=h_, s_=s_, pu=pu, fc=fc: nc.vector.tensor_tensor(out=h_[:, fc, :], in0=s_[:], in1=pu[:], op=ALU.mult),
                         reads=[r_s, r_pu], writes=[r_h_])
                if b + 1 < NB:
                    xT_pend = do_T(xq)
                for sub in range(NSUB):
                    y_, r_y_ = ysb.nxt()
                    for half in range(2):
                        py, r_py = pYm.nxt()
                        for fc in range(4):
                            P.op("pe", lambda fc=fc, py=py, h_=h_, d_=d_, sub=sub, half=half: nc.tensor.matmul(
                                py[:], lhsT=h_[:, fc, sub * 128:(sub + 1) * 128], rhs=d_[:, fc, half * 512:(half + 1) * 512],
                                start=(fc == 0), stop=(fc == 3)), reads=[r_h_, r_d], writes=[r_py])
                        evac(y_[:, half * 512:(half + 1) * 512], py[:], [r_py], [r_y_])
                    r0 = b * BS + sub * 128
                    dma("sp", y_d[r0:r0 + 128, :], y_[:], reads=[r_y_], writes=[r_y])

        P.barrier()
        with ExitStack() as st:
            ht = MB(st, "ht3", [128, D], F32, 5)
            nlw_t = SB(st, "nlw_t", [128, D]); r_nl = R()
            dma("sp", nlw_t[:], nlw.partition_broadcast(128), writes=[r_nl])
            y1 = MB(st, "y1", [128, D], BF16, 4)
            y2 = MB(st, "y2", [128, D], BF16, 4)
            sq = SB(st, "sq3", [128, D])
            stat = MB(st, "stat3", [128, 2], F32, 4)
            ot = MB(st, "ot3", [128, D], F32, 3)
            outv = out.rearrange("s l d -> (s l) d")
            hq = []

            def load_h(ti):
                h_, r_ht = ht.nxt()
                dma("sp", h_[:], h_d[ti * 128:(ti + 1) * 128, :], reads=[r_h], writes=[r_ht])
                hq.append((h_, r_ht))
            PRE = 3
            for ti in range(min(PRE, NT)):
                load_h(ti)
            for ti in range(NT):
                if ti + PRE < NT:
                    load_h(ti + PRE)
                h_, r_ht = hq.pop(0)
                ys = []
                for k_, yb in enumerate((y1, y2)):
                    y_, r_y_ = yb.nxt()
                    P.op("pool", lambda y_=y_, ti=ti, k_=k_: nc.gpsimd.indirect_dma_start(
                        out=y_[:], out_offset=None, in_=y_d,
                        in_offset=bass.IndirectOffsetOnAxis(ap=slot_i[:, ti, k_:k_ + 1], axis=0),
                        bounds_check=bcs["s"], oob_is_err=False),
                        reads=[r_route, r_y], writes=[r_y_], dma=True)
                    ys.append((y_, r_y_))
                for k_ in range(2):
                    y_, r_y_ = ys[k_]
                    P.op("dve", lambda h_=h_, y_=y_, ti=ti, k_=k_: nc.vector.scalar_tensor_tensor(
                        out=h_[:], in0=y_[:], scalar=g_all[:, ti, k_:k_ + 1], in1=h_[:], op0=ALU.mult, op1=ALU.add),
                        reads=[r_y_, r_ht, r_route], writes=[r_ht])
                st_, r_st = stat.nxt()
                rms_rstd("z", h_[:], st_[:, 0:1], st_[:, 1:2], sq[:], r_ht, r_st, D)
                o_, r_o = ot.nxt()
                P.op("dve", lambda o_=o_, h_=h_, st_=st_: nc.vector.scalar_tensor_tensor(
                    out=o_[:], in0=h_[:], scalar=st_[:, 1:2], in1=nlw_t[:], op0=ALU.mult, op1=ALU.mult),
                    reads=[r_ht, r_st, r_nl], writes=[r_o])
                dma("sp", outv[ti * 128:(ti + 1) * 128, :], o_[:], reads=[r_o])
        P.emit(top)
    return nc


def host_inputs(inp, L, BS, n_cores, nseq):
    consts, _ = make_consts(L, BS)
    f = lambda a: np.ascontiguousarray(np.asarray(a, dtype=np.float32))
    common = dict(
        w_in=f(inp["w_in"][0]), w_out=f(inp["w_out"][0]),
        wr=f(np.concatenate([inp["w_router_group"][0], inp["w_router_exp"][0]], axis=1)),
        br=f(np.concatenate([inp["b_router_group"][0], inp["b_router_exp"][0]])[None, :]),
        wg=f(inp["w_exp_gate"][0]).reshape(NE * 128 * 2, 2048),
        wu=f(inp["w_exp_up"][0]).reshape(NE * 128 * 2, 2048),
        wd=f(inp["w_exp_down"][0]).reshape(NE * 512, 1024),
        nmw=f(inp["norm_mix_w"]), nfw=f(inp["norm_ffn_w"]), nlw=f(inp["norm_final_w"])[None, :],
        convw=f(np.asarray(inp["conv_w"][0]).T.reshape(8, 128, 5).transpose(1, 0, 2).reshape(128, 40)),
        convb=f(np.asarray(inp["conv_b"][0]).reshape(8, 128).T),
        dtb=f(np.concatenate([inp["dt_bias_fwd"][0], inp["dt_bias_bwd"][0]])[None, :]),
        alog=f(np.concatenate([inp["a_log_fwd"][0], inp["a_log_bwd"][0]])[None, :]),
        dsk=f(inp["ssd_d"]), snw=f(inp["ssd_norm_w"]),
        lam=f(np.stack([inp["lambda_q1"][0], inp["lambda_q2"][0], inp["lambda_k1"][0], inp["lambda_k2"][0]])),
        sub_w=f(inp["subln_w"]),
        **consts,
    )
    xs = f(inp["x"])
    maps = []
    for c in range(n_cores):
        m = dict(common)
        m["x"] = np.ascontiguousarray(xs[c * nseq:(c + 1) * nseq])
        maps.append(m)
    return maps


def kernel(**inputs):
    n_cores, nseq, L, BS = 8, 2, 4096, 512
    nc = build(nseq, L, BS)
    maps = host_inputs(inputs, L, BS, n_cores, nseq)
    res = run_bass_kernel_spmd(nc, maps, core_ids=list(range(n_cores)))
    return np.concatenate([np.asarray(r["out"], dtype=np.float32) for r in res.results], axis=0)
```

```python
import math
from contextlib import ExitStack
import numpy as np
import ml_dtypes
import concourse.bass as bass
import concourse.mybir as mybir
from concourse.bass_utils import run_bass_kernel_spmd

F32 = mybir.dt.float32
BF16 = mybir.dt.bfloat16
I32 = mybir.dt.int32
ALU = mybir.AluOpType
AF = mybir.ActivationFunctionType
AX = mybir.AxisListType

D = 1024
NE = 32
DE = 512
EPS = 1e-6
LAMBDA_INIT = 0.2
NEGBIG = 30000.0
STRICT_SYNC = True


class R:
    __slots__ = ("w", "rd")

    def __init__(self):
        self.w = None
        self.rd = {}


class Prog:
    ENG = ("pe", "act", "dve", "pool", "sp")

    def __init__(self, nc, n_dma_sems=14):
        self.nc = nc
        self.ops = []
        self.n_dma_sems = n_dma_sems

    def op(self, eng, emit, reads=(), writes=(), dma=False):
        reads = [r for r in reads if r is not None]
        writes = [w for w in writes if w is not None]
        self.ops.append(dict(eng=eng, emit=emit, reads=tuple(reads), writes=tuple(writes),
                             dma=dma, deps=set(), marked=False, barrier=False))

    def barrier(self):
        for e in self.ENG:
            self.ops.append(dict(eng=e, emit=None, reads=(), writes=(), dma=False, deps=set(),
                                 marked=False, barrier=True))

    def _analyze(self):
        ops = self.ops
        last_on_eng = {e: None for e in self.ENG}
        dmas = []
        for i, o in enumerate(ops):
            e = o["eng"]
            if o["barrier"]:
                for e2 in self.ENG:
                    if e2 != e and last_on_eng[e2] is not None:
                        o["deps"].add(last_on_eng[e2])
                o["deps"].update(dmas)
                if e == self.ENG[-1]:
                    dmas = []
                continue
            deps = set()
            for r in o["reads"]:
                if r.w is not None:
                    deps.add(("raw", r.w))
            for w in o["writes"]:
                if w.w is not None:
                    deps.add(("waw", w.w))
                for j in w.rd.values():
                    deps.add(("war", j))
            for kind, j in deps:
                if j == i:
                    continue
                oj = ops[j]
                if oj["eng"] == e and not oj["dma"] and not o["dma"]:
                    if e == "pe" or (kind != "raw" and not STRICT_SYNC):
                        continue
                o["deps"].add(j)
            for r in o["reads"]:
                r.rd[("dma", i) if o["dma"] else e] = i
            for w in o["writes"]:
                w.w = i
                w.rd = {}
            if o["dma"]:
                dmas.append(i)
            else:
                last_on_eng[e] = i
        for o in ops:
            for j in o["deps"]:
                ops[j]["marked"] = True

    def emit(self, stack):
        nc = self.nc
        self._analyze()
        ops = self.ops
        sems = {e: stack.enter_context(nc.semaphore("s_" + e)) for e in self.ENG}
        dq = ("sp", "pool", "act")
        dsem = {e: [stack.enter_context(nc.semaphore("d_%s_%d" % (e, k)))
                    for k in range(self.n_dma_sems)] for e in dq}
        cnt = {e: 0 for e in self.ENG}
        dcount = {e: 0 for e in dq}
        dval = {e: [0] * self.n_dma_sems for e in dq}
        prev_on_sem = {}
        for i, o in enumerate(ops):
            if o["barrier"]:
                continue
            e = o["eng"]
            if o["dma"]:
                k = dcount[e] % self.n_dma_sems
                dcount[e] += 1
                o["prev_same_sem"] = prev_on_sem.get((e, k))
                dval[e][k] += 16
                o["done"] = (dsem[e][k], dval[e][k], ("d", e, k))
                prev_on_sem[(e, k)] = i
            elif o["marked"]:
                cnt[e] += 1
                o["done"] = (sems[e], cnt[e], ("c", e))
        final_dma = [(dsem[e][k], dval[e][k]) for e in dq for k in range(self.n_dma_sems)
                     if dval[e][k] > 0]
        blk = stack.enter_context(nc.Block())
        for e in self.ENG:
            my = [(i, o) for i, o in enumerate(ops) if o["eng"] == e]

            def body(engine, my=my, e=e):
                seen = {}
                for i, o in my:
                    deps = set(o["deps"])
                    if o["dma"] and o.get("prev_same_sem") is not None:
                        deps.add(o["prev_same_sem"])
                    need = {}
                    for j in deps:
                        s, v, key = ops[j]["done"]
                        if seen.get(key, 0) >= v:
                            continue
                        if key not in need or need[key][1] < v:
                            need[key] = (s, v)
                    for key, (s, v) in need.items():
                        engine.wait_ge(s, v)
                        seen[key] = v
                    if o["barrier"]:
                        continue
                    ins = o["emit"]()
                    if "done" in o:
                        ins.then_inc(o["done"][0], 16 if o["dma"] else 1)
                if e == "sp":
                    for s, v in final_dma:
                        engine.wait_ge(s, v)

            getattr(blk, {"pe": "tensor", "act": "scalar", "dve": "vector",
                          "pool": "gpsimd", "sp": "sync"}[e])(body)


CF_IDENT, CF_ONES, CF_TF, CF_TB, CF_UF, CF_UB, CF_MF, CF_MB = range(8)
CF_D2 = 8
CF_MISC = 8 + 16
NCF = (8 + 16) * 128 + 16


def make_consts(L, BS):
    t = np.arange(128)
    a = t[:, None]
    b = t[None, :]
    cf = np.zeros((128, NCF), np.float32)

    def put(k, m):
        cf[:, k * 128:(k + 1) * 128] = m
    put(CF_IDENT, (a == b))
    put(CF_ONES, np.ones((128, 128)))
    put(CF_TF, (a <= b))
    put(CF_TB, (a >= b))
    put(CF_UF, (a > b))
    put(CF_UB, (a < b))
    put(CF_MF, (b >= a))
    put(CF_MB, (b <= a))
    ii = np.arange(512)[None, :]
    for dk in range(4):
        d2 = 2.0 * np.maximum(a + 128 * dk - ii, 0)
        cf[:, (CF_D2 + 4 * dk) * 128:(CF_D2 + 4 * dk + 4) * 128] = d2
    m0 = CF_MISC * 128
    cf[:, m0 + 0] = t * BS
    cf[:, m0 + 1] = t
    cb = np.zeros((128, 4 * 128), np.float32)
    cb[:, 384:512] = ((a < 64) == (b < 64))
    cb[:, 0:128] = (a == b)
    cb[:, 128:256] = 1.0
    cb[:, 256:384] = (a < b)
    cb = cb.astype(ml_dtypes.bfloat16)
    slopes = [2.0 ** (-8.0 * (h + 1) / 4) for h in range(4)]
    pos = np.arange(L)
    ip = pos % 512
    jp = pos % 128
    qrows = np.zeros((4, 3, L), np.float32)
    krA = np.zeros((4, 3, L), np.float32)
    for h in range(4):
        s8 = 8.0 * slopes[h]
        qrows[h, 0] = -s8 * (ip % 256)
        qrows[h, 1] = -s8 * 256 * (ip // 256)
        qrows[h, 2] = 1.0
        krA[h, 0] = 1.0
        krA[h, 1] = 1.0
        krA[h, 2] = s8 * jp
    krB = -krA
    nkt, nqb = L // 128, L // 512
    btab = np.zeros((4, nkt * nqb), np.float32)
    for h in range(4):
        for kt in range(nkt):
            for qb in range(nqb):
                if kt >= 4 * qb + 4:
                    v = -(128 * kt - 512 * qb)
                else:
                    v = -(512 * qb - 128 * kt)
                btab[h, kt * nqb + qb] = slopes[h] * v
    return dict(cf=cf, cb=cb, qrows=qrows.astype(ml_dtypes.bfloat16),
                krA=krA.astype(ml_dtypes.bfloat16), krB=krB.astype(ml_dtypes.bfloat16), btab=btab), slopes


def build(NSEQ, L, BS, dbg=False):
    nc = bass.Bass("TRN2", target_bir_lowering=False)
    T = NSEQ * L
    NT = T // 128
    NC = L // 128
    NQB = L // 512
    NB = (2 * T) // BS + NE
    NSUB = BS // 128
    NSLOT = NB * BS
    assert NB <= 128

    def din(name, shape, dt=F32):
        return nc.dram_tensor(name, list(shape), dt, kind="ExternalInput").ap()

    def dscr(name, shape, dt):
        return nc.dram_tensor(name, list(shape), dt, kind="Internal").ap()

    x = din("x", [NSEQ, L, D])
    w_in = din("w_in", [D, 3088])
    w_out = din("w_out", [D, D])
    wr = din("wr", [D, 36])
    br = din("br", [1, 36])
    wg = din("wg", [NE * 128 * 2, 2048])
    wu = din("wu", [NE * 128 * 2, 2048])
    wd = din("wd", [NE * 512, 1024])
    nmw = din("nmw", [1, D])
    nfw = din("nfw", [1, D])
    nlw = din("nlw", [1, D])
    convw = din("convw", [128, 40])
    convb = din("convb", [128, 8])
    dtb = din("dtb", [1, 16])
    alog = din("alog", [1, 16])
    dsk = din("dsk", [1, 8])
    snw = din("snw", [1, 512])
    lam = din("lam", [4, 64])
    sub_w = din("sub_w", [1, 128])
    cf_d = din("cf", [128, NCF])
    cb_d = din("cb", [128, 512], BF16)
    qrows_d = din("qrows", [4, 3, L], BF16)
    krA_d = din("krA", [4, 3, L], BF16)
    krB_d = din("krB", [4, 3, L], BF16)
    btab_d = din("btab", [4, NC * NQB])
    out = nc.dram_tensor("out", [NSEQ, L, D], F32, kind="ExternalOutput").ap()

    nmax_d = dscr("nmax_d", [128, 16], F32)
    xbc_d = dscr("xbc_d", [D, L + 4], BF16)
    qT_d = dscr("qT_d", [512, L], BF16)
    kT_d = dscr("kT_d", [512, L], BF16)
    v_d = dscr("v_d", [L, 512], BF16)
    z_d = dscr("z_d", [L, 512], F32)
    dt_d = dscr("dt_d", [L, 16], F32)
    mix_d = dscr("mix_d", [L, D], BF16)
    h_d = dscr("h_d", [T, D], F32)
    xn_d = dscr("xn_d", [T, D], BF16)
    xs_d = dscr("xs_d", [NSLOT, D], BF16)
    y_d = dscr("y_d", [NSLOT, D], BF16)
    dbg_d = {}
    if dbg:
        for nm, shp in (("dbg_mix", [NSEQ * L, D]), ("dbg_h", [T, D]), ("dbg_route", [128, NT * 8])):
            dbg_d[nm] = nc.dram_tensor(nm, shp, F32, kind="ExternalOutput").ap()

    P = Prog(nc)
    top = ExitStack()
    with top:
        uid = [0]

        def SB(st, name, shape, dt=F32):
            uid[0] += 1
            return st.enter_context(nc.sbuf_tensor("sb_%s_%d" % (name, uid[0]), list(shape), dt))

        def PS(st, name, shape, dt=F32):
            uid[0] += 1
            return st.enter_context(nc.psum_tensor("ps_%s_%d" % (name, uid[0]), list(shape), dt))

        class MB:
            def __init__(self, st, name, shape, dt=F32, n=2, psum=False):
                mk = PS if psum else SB
                self.t = [mk(st, "%s%d" % (name, i), shape, dt) for i in range(n)]
                self.r = [R() for _ in range(n)]
                self.n = n
                self.k = -1

            def nxt(self):
                self.k += 1
                i = self.k % self.n
                return self.t[i], self.r[i]

        rr = {"ev": 0}

        def evac(out_ap, in_ap, reads, writes, eng=None):
            if eng is None:
                eng = ("act", "dve")[rr["ev"] % 2]
                rr["ev"] += 1
            if eng == "act":
                P.op("act", lambda: nc.scalar.copy(out=out_ap, in_=in_ap), reads=reads, writes=writes)
            else:
                P.op("dve", lambda: nc.vector.tensor_copy(out=out_ap, in_=in_ap), reads=reads, writes=writes)

        def dma(eng, out_ap, in_ap, reads=(), writes=()):
            q = {"sp": nc.sync, "pool": nc.gpsimd, "act": nc.scalar}[eng]
            P.op(eng, lambda: q.dma_start(out=out_ap, in_=in_ap), reads=reads, writes=writes, dma=True)

        cf = SB(top, "cf", [128, NCF]); r_c = R()
        cb = SB(top, "cb", [128, 512], BF16)
        dma("sp", cf[:], cf_d, writes=[r_c])
        dma("sp", cb[:], cb_d, writes=[r_c])

        def CF(k, n=1):
            return cf[:, k * 128:(k + n) * 128]
        ident_f = CF(CF_IDENT)
        ones_f = CF(CF_ONES)
        ident_b = cb[:, 0:128]
        ones_b = cb[:, 128:256]
        tstrict_b = cb[:, 256:384]
        bones_c = cb[:, 384:512]
        eps_t = SB(top, "eps_t", [128, 1])
        P.op("pool", lambda: nc.gpsimd.memset(eps_t[:], EPS), writes=[r_c])
        neghalf = SB(top, "neghalf", [128, 1])
        P.op("pool", lambda: nc.gpsimd.memset(neghalf[:], -0.5), writes=[r_c])
        poshalf = SB(top, "poshalf", [128, 1])
        P.op("pool", lambda: nc.gpsimd.memset(poshalf[:], 0.5), writes=[r_c])
        zero_bf = SB(top, "zero_bf", [128, 2048], BF16)
        if dbg:
            pass
        P.op("pool", lambda: nc.gpsimd.memset(zero_bf[:], 0.0), writes=[r_c])
        xs_v = xs_d.rearrange("(a p) d -> p a d", p=128)
        r_xs = None
        na = NSLOT // 128
        r_xbc = None
        dma("sp", xbc_d[:, 0:2].rearrange("(a p) d -> p a d", p=128), zero_bf[:, 0:16].rearrange("p (a d) -> p a d", d=2),
            reads=[r_c], writes=[r_xbc])
        dma("sp", xbc_d[:, L + 2:L + 4].rearrange("(a p) d -> p a d", p=128), zero_bf[:, 0:16].rearrange("p (a d) -> p a d", d=2),
            reads=[r_c], writes=[r_xbc])


        oh1_all = SB(top, "oh1_all", [128, NT, NE], BF16)
        oh2_all = SB(top, "oh2_all", [128, NT, NE], BF16)
        rank_all = SB(top, "rank_all", [128, NT, NE])
        g_all = SB(top, "g_all", [128, NT, 2])
        carry = SB(top, "carry", [128, NE])
        slot_i = SB(top, "slot_i", [128, NT, 2], I32)
        idx_g = SB(top, "idx_g", [128, NB, 2], I32)
        idx_d = SB(top, "idx_d", [128, NB, 4], I32)
        r_route = R()
        P.op("pool", lambda: nc.gpsimd.memset(carry[:], 0.0), writes=[r_route])

        r_q = r_k = r_v = r_z = r_dt = r_mix = None
        r_h = r_xn = r_y = None

        def rms_rstd(st_name, src_ap, ss, rstd, sq_scr, r_src, r_s, n):
            P.op("act", lambda: nc.scalar.activation(out=sq_scr, in_=src_ap, func=AF.Square, accum_out=ss),
                 reads=[r_src], writes=[r_s])
            P.op("act", lambda: nc.scalar.activation(out=rstd, in_=ss, func=AF.Ln, bias=eps_t[:, 0:1],
                                                     scale=1.0 / n), reads=[r_s, r_c], writes=[r_s])
            P.op("act", lambda: nc.scalar.activation(out=rstd, in_=rstd, func=AF.Exp, scale=-0.5),
                 reads=[r_s], writes=[r_s])

        def seq_body(s):
            P.barrier()
            with ExitStack() as st:
                win = SB(st, "win", [128, 8, 3088], BF16); r_win = R()
                nmw_t = SB(st, "nmw_t", [128, D])
                r_wparts = [R()]
                dma("sp", nmw_t[:], nmw.partition_broadcast(128), writes=[r_wparts[0]])
                wv = w_in.rearrange("(kc p) f -> p kc f", p=128)
                for c0 in range(0, 3088, 512):
                    c1 = min(3088, c0 + 512)
                    r_wparts.append(R())
                    dma("pool", win[:, :, c0:c1], wv[:, :, c0:c1], writes=[r_wparts[-1]])
                jn = SB(st, "jn", [128, 2])
                P.op("pool", lambda: nc.gpsimd.memset(jn[:], 0.0), reads=r_wparts, writes=[r_win])
                xt = MB(st, "xt", [128, D], F32, 4)
                sq = SB(st, "sq", [128, D]); r_sq = R()
                stat = MB(st, "stat", [128, 2], F32, 4)
                ub = MB(st, "ub", [128, D], BF16, 4)
                uT = MB(st, "uT", [128, 8, 512], BF16, 2)
                pT = MB(st, "pT", [128, 8, 128], BF16, 2, psum=True)
                pA = MB(st, "pA", [128, 512], F32, 3, psum=True)
                pB = MB(st, "pB", [128, 512], F32, 2, psum=True)
                zs = MB(st, "zs", [128, 512], F32, 2)
                vs = MB(st, "vs", [128, 512], BF16, 2)
                ds = MB(st, "ds", [128, 16], F32, 2)
                fs = MB(st, "fs", [128, 512], BF16, 3)
                sqA = MB(st, "sqA", [128, 512], BF16, 5)
                pNA = MB(st, "pNA", [128, 512], F32, 1, psum=True)
                nmax = SB(st, "nmax", [128, 16]); nmt = SB(st, "nmt", [128, 1]); r_nm = R()
                P.op("pool", lambda: nc.gpsimd.memset(nmax[:], 0.0), writes=[r_nm])
                def prepA1(tb):
                    ubs = []
                    for j in range(4):
                        t0 = tb * 512 + j * 128
                        xtt, r_xt = xt.nxt()
                        dma("sp", xtt[:], x[s, t0:t0 + 128, :], writes=[r_xt])
                        stt, r_st = stat.nxt()
                        rms_rstd("a", xtt[:], stt[:, 0:1], stt[:, 1:2], sq[:], r_xt, r_st, D)
                        ubt, r_ub = ub.nxt()
                        P.op("dve", lambda ubt=ubt, xtt=xtt, stt=stt: nc.vector.scalar_tensor_tensor(
                            out=ubt[:], in0=xtt[:], scalar=stt[:, 1:2], in1=nmw_t[:], op0=ALU.mult, op1=ALU.mult),
                            reads=[r_xt, r_st, r_win], writes=[r_ub])
                        ubs.append((ubt, r_ub))
                    return ubs

                def prepA2(ubs):
                    uTt, r_uT = uT.nxt()
                    for j in range(4):
                        ubt, r_ub = ubs[j]
                        pTt, r_pT = pT.nxt()
                        for k in range(8):
                            P.op("pe", lambda k=k, pTt=pTt, ubt=ubt: nc.tensor.transpose(
                                out=pTt[:, k, :], in_=ubt[:, k * 128:(k + 1) * 128], identity=ident_b),
                                reads=[r_ub, r_c], writes=[r_pT])
                        evac(uTt[:, :, j * 128:(j + 1) * 128], pTt[:], [r_pT], [r_uT])
                    return uTt, r_uT

                pend_bm = []

                def emit_bm(sqb, r_sqb, cc):
                    pn, r_pn = pNA.nxt()
                    P.op("pe", lambda: nc.tensor.matmul(pn[:], lhsT=bones_c, rhs=sqb[:], start=True, stop=True),
                         reads=[r_sqb, r_c], writes=[r_pn])
                    P.op("dve", lambda: nc.vector.reduce_max(out=nmt[:, 0:1], in_=pn[:], axis=AX.X),
                         reads=[r_pn], writes=[r_nm])
                    P.op("dve", lambda: nc.vector.tensor_tensor(out=nmax[:, cc - 8:cc - 7], in0=nmax[:, cc - 8:cc - 7],
                                                                in1=nmt[:, 0:1], op=ALU.max), reads=[r_nm], writes=[r_nm])

                pendA = prepA2(prepA1(0))
                NTB = L // 512
                for tb in range(NTB):
                    uTt, r_uT = pendA
                    ubs_n = prepA1(tb + 1) if tb + 1 < NTB else None
                    for j in range(4):
                        t0 = tb * 512 + j * 128
                        for (c0, n, kind) in ((0, 512, "z"), (1536, 16, "dt"), (2576, 512, "v")):
                            pt, r_p = pA.nxt()
                            for k in range(8):
                                P.op("pe", lambda k=k, pt=pt, c0=c0, n=n, j=j, uTt=uTt: nc.tensor.matmul(
                                    pt[:, 0:n], lhsT=uTt[:, k, j * 128:(j + 1) * 128], rhs=win[:, k, c0:c0 + n],
                                    start=(k == 0), stop=(k == 7)), reads=[r_uT, r_win], writes=[r_p])
                            if kind == "z":
                                o, r_o = zs.nxt()
                                evac(o[:], pt[:], [r_p], [r_o])
                                dma("sp", z_d[t0:t0 + 128, :], o[:], reads=[r_o], writes=[r_z])
                            elif kind == "v":
                                o, r_o = vs.nxt()
                                evac(o[:], pt[:], [r_p], [r_o])
                                dma("sp", v_d[t0:t0 + 128, :], o[:], reads=[r_o], writes=[r_v])
                            else:
                                o, r_o = ds.nxt()
                                evac(o[:], pt[:, 0:16], [r_p], [r_o])
                                dma("sp", dt_d[t0:t0 + 128, :], o[:], reads=[r_o], writes=[r_dt])
                    for cc in range(16):
                        c0 = 512 + cc * 128 if cc < 8 else (1552 + (cc - 8) * 128)
                        pt, r_p = pB.nxt()
                        for k in range(8):
                            P.op("pe", lambda k=k, pt=pt, c0=c0, uTt=uTt: nc.tensor.matmul(
                                pt[:], lhsT=win[:, k, c0:c0 + 128], rhs=uTt[:, k, :],
                                start=(k == 0), stop=(k == 7)), reads=[r_uT, r_win], writes=[r_p])
                        if len(pend_bm) > 2:
                            emit_bm(*pend_bm.pop(0))
                        o, r_o = fs.nxt()
                        evac(o[:], pt[:], [r_p], [r_o])
                        if cc >= 8:
                            sqb, r_sqb = sqA.nxt()
                            P.op("pool", lambda sqb=sqb, o=o: nc.gpsimd.tensor_tensor(out=sqb[:], in0=o[:], in1=o[:], op=ALU.mult),
                                 reads=[r_o], writes=[r_sqb])
                            pend_bm.append((sqb, r_sqb, cc))
                        if cc < 8:
                            dma("sp", xbc_d[cc * 128:(cc + 1) * 128, 2 + tb * 512:2 + (tb + 1) * 512], o[:],
                                reads=[r_o], writes=[r_xbc])
                        elif cc < 12:
                            dma("sp", qT_d[(cc - 8) * 128:(cc - 7) * 128, tb * 512:(tb + 1) * 512], o[:],
                                reads=[r_o], writes=[r_q])
                        else:
                            dma("sp", kT_d[(cc - 12) * 128:(cc - 11) * 128, tb * 512:(tb + 1) * 512], o[:],
                                reads=[r_o], writes=[r_k])
                    if ubs_n is not None:
                        pendA = prepA2(ubs_n)
                while pend_bm:
                    emit_bm(*pend_bm.pop(0))
                dma("sp", nmax_d, nmax[:], reads=[r_nm])

            P.barrier()
            with ExitStack() as st:
                xs_tm = SB(st, "xs_tm", [128, NC, 512], BF16)
                B_tm = SB(st, "B_tm", [128, NC, 256], BF16)
                BT = SB(st, "BT", [128, 2, L], BF16)
                CT = SB(st, "CT", [128, 2, L], BF16)
                r_prep = R()
                cw = SB(st, "cw", [128, 8, 5]); cbias = SB(st, "cbias", [128, 8, 1]); r_cw = R()
                dma("sp", cw[:], convw.rearrange("p (cc k) -> p cc k", k=5), writes=[r_cw])
                dma("sp", cbias[:], convb.rearrange("p (cc k) -> p cc k", k=1), writes=[r_cw])
                dtr = SB(st, "dtr", [128, NC, 16]); r_dtr = R()
                dtt = SB(st, "dtt", [128, NC, 16])
                adt = SB(st, "adt", [128, NC, 16])
                cs = SB(st, "cs", [128, NC, 16])
                ecs = SB(st, "ecs", [128, NC, 16])
                dte = SB(st, "dte", [128, NC, 16])
                cd = SB(st, "cd", [128, NC, 16])
                tmp1 = SB(st, "tmp1", [128, NC, 16]); tmp2 = SB(st, "tmp2", [128, NC, 16])
                dtb_t = SB(st, "dtb_t", [128, 16]); a_t = SB(st, "a_t", [128, 16]); dsk_t = SB(st, "dsk_t", [128, 8])
                snw_t = SB(st, "snw_t", [128, 512])
                r_dtp = R()
                dma("sp", dtr[:], dt_d.rearrange("(c p) h -> p c h", p=128), reads=[r_dt], writes=[r_dtr])
                dma("sp", dtb_t[:], dtb.partition_broadcast(128), writes=[r_dtp])
                dma("sp", a_t[:], alog.partition_broadcast(128), writes=[r_dtp])
                dma("sp", dsk_t[:], dsk.partition_broadcast(128), writes=[r_dtp])
                dma("sp", snw_t[:], snw.partition_broadcast(128), writes=[r_dtp])
                bc16 = lambda t: t[:].unsqueeze(1).to_broadcast([128, NC, 16])
                P.op("act", lambda: nc.scalar.activation(out=a_t[:], in_=a_t[:], func=AF.Exp), reads=[r_dtp], writes=[r_dtp])
                P.op("dve", lambda: nc.vector.tensor_scalar(out=a_t[:], in0=a_t[:], scalar1=-1.0, scalar2=None, op0=ALU.mult),
                     reads=[r_dtp], writes=[r_dtp])
                P.op("dve", lambda: nc.vector.tensor_tensor(out=dtr[:], in0=dtr[:], in1=bc16(dtb_t), op=ALU.add),
                     reads=[r_dtr, r_dtp], writes=[r_dtr])
                P.op("dve", lambda: nc.vector.tensor_scalar(out=tmp1[:], in0=dtr[:], scalar1=30.0, scalar2=None, op0=ALU.min),
                     reads=[r_dtr], writes=[r_dtp])
                P.op("act", lambda: nc.scalar.activation(out=tmp1[:], in_=tmp1[:], func=AF.Exp),
                     reads=[r_dtp], writes=[r_dtp])
                P.op("dve", lambda: nc.vector.tensor_scalar(out=tmp1[:], in0=tmp1[:], scalar1=1.0, scalar2=None, op0=ALU.add),
                     reads=[r_dtp], writes=[r_dtp])
                P.op("act", lambda: nc.scalar.activation(out=tmp1[:], in_=tmp1[:], func=AF.Ln),
                     reads=[r_dtp], writes=[r_dtp])
                P.op("dve", lambda: nc.vector.tensor_tensor(out=dtt[:], in0=tmp1[:], in1=dtr[:], op=ALU.max),
                     reads=[r_dtp, r_dtr], writes=[r_dtp])
                P.op("dve", lambda: nc.vector.tensor_tensor(out=adt[:], in0=dtt[:], in1=bc16(a_t), op=ALU.mult),
                     reads=[r_dtp], writes=[r_dtp])
                stc = ExitStack()
                pC = MB(stc, "pC", [128, 512], F32, 2, psum=True)
                adt_f = adt[:].rearrange("p c h -> p (c h)")
                ncol = NC * 16
                for (lh, dst, lo, hi) in ((CF(CF_TF), cs, 0, 8), (CF(CF_TB), cs, 8, 16), (ones_f, tmp1, 0, 16)):
                    for c0 in range(0, ncol, 512):
                        c1 = min(ncol, c0 + 512)
                        pt, r_p = pC.nxt()
                        P.op("pe", lambda pt=pt, lh=lh, c0=c0, c1=c1: nc.tensor.matmul(
                            pt[:, 0:c1 - c0], lhsT=lh, rhs=adt_f[:, c0:c1], start=True, stop=True),
                            reads=[r_dtp, r_c], writes=[r_p])
                        ca, cb_ = c0 // 16, c1 // 16
                        evac(dst[:, ca:cb_, lo:hi], pt[:, 0:c1 - c0].rearrange("p (c h) -> p c h", h=16)[:, :, lo:hi],
                             [r_p], [r_dtp], eng="dve")
                P.op("act", lambda: nc.scalar.activation(out=ecs[:], in_=cs[:], func=AF.Exp), reads=[r_dtp], writes=[r_dtp])
                P.op("dve", lambda: nc.vector.tensor_tensor(out=tmp2[:], in0=tmp1[:], in1=cs[:], op=ALU.subtract),
                     reads=[r_dtp], writes=[r_dtp])
                P.op("act", lambda: nc.scalar.activation(out=dte[:], in_=tmp2[:], func=AF.Exp), reads=[r_dtp], writes=[r_dtp])
                P.op("act", lambda: nc.scalar.activation(out=cd[:], in_=tmp1[:], func=AF.Exp), reads=[r_dtp], writes=[r_dtp])

                with ExitStack() as st2:
                    raw = MB(st2, "raw", [128, L + 4], BF16, 2)
                    xsT = MB(st2, "xsT", [128, L], BF16, 2)
                    pTr = MB(st2, "pTr", [128, 4, 128], BF16, 2, psum=True)
                    pcv = MB(st2, "pcv", [128, 512], F32, 2, psum=True)
                    dgw = SB(st2, "dgw", [128, 8, 5, 128], BF16); r_dg = R()
                    for cc in range(8):
                        for k in range(5):
                            P.op("dve", lambda cc=cc, k=k: nc.vector.tensor_scalar(
                                out=dgw[:, cc, k, :], in0=ident_b, scalar1=cw[:, cc, k:k + 1], scalar2=None, op0=ALU.mult),
                                reads=[r_cw, r_c], writes=[r_dg])
                    for cc in range(8):
                        rw, r_rw = raw.nxt()
                        dma("sp", rw[:], xbc_d[cc * 128:(cc + 1) * 128, :], reads=[r_xbc], writes=[r_rw])
                        if cc < 4:
                            dst, r_d = xsT.nxt()
                            dstap = dst[:]
                        elif cc < 6:
                            dstap, r_d = BT[:, cc - 4, :], r_prep
                        else:
                            dstap, r_d = CT[:, cc - 6, :], r_prep
                        for tbk in range(L // 512):
                            o0 = tbk * 512
                            pc, r_pc = pcv.nxt()
                            for k in range(5):
                                P.op("pe", lambda pc=pc, rw=rw, cc=cc, k=k, o0=o0: nc.tensor.matmul(
                                    pc[:], lhsT=dgw[:, cc, k, :], rhs=rw[:, o0 + k:o0 + k + 512], start=(k == 0), stop=(k == 4)),
                                    reads=[r_rw, r_dg], writes=[r_pc])
                            P.op("act", lambda pc=pc, dstap=dstap, o0=o0, cc=cc: nc.scalar.activation(
                                out=dstap[:, o0:o0 + 512], in_=pc[:], func=AF.Silu, bias=cbias[:, cc, 0:1]),
                                reads=[r_pc, r_cw], writes=[r_d])
                        if cc < 6:
                            for c0 in range(0, NC, 4):
                                pt, r_p = pTr.nxt()
                                for i in range(4):
                                    P.op("pe", lambda pt=pt, i=i, c0=c0, dstap=dstap: nc.tensor.transpose(
                                        out=pt[:, i, :], in_=dstap[:, (c0 + i) * 128:(c0 + i + 1) * 128],
                                        identity=ident_b), reads=[r_d, r_c], writes=[r_p])
                                if cc < 4:
                                    evac(xs_tm[:, c0:c0 + 4, cc * 128:(cc + 1) * 128], pt[:], [r_p], [r_prep])
                                else:
                                    evac(B_tm[:, c0:c0 + 4, (cc - 4) * 128:(cc - 3) * 128], pt[:], [r_p], [r_prep])

                stc.close()
                P.barrier()
                with ExitStack() as st2:
                    Hb_all = SB(st2, "Hb_all", [128, NC, 2, 256], BF16); r_Hb = R()
                    Hf = [[SB(st2, "Hf%d%d" % (d_, g), [128, 256]) for g in range(2)] for d_ in range(2)]
                    r_H = [[R(), R()], [R(), R()]]
                    Hbf = MB(st2, "Hbf", [128, 256], BF16, 2)
                    Hbf_g = [None, None]
                    xdt = MB(st2, "xdt", [128, 512], BF16, 3)
                    xdte = MB(st2, "xdte", [128, 512], BF16, 3)
                    RH = MB(st2, "RH", [128, 4, 128], F32, 2)
                    Eb = MB(st2, "Eb", [128, 4, 128], BF16, 2)
                    MT = MB(st2, "MT", [128, 4, 128], BF16, 2)
                    cbm = MB(st2, "cbm", [128, 128], BF16, 4)
                    yacc = MB(st2, "yacc", [128, 512], F32, 2)
                    ytmp = MB(st2, "ytmp", [128, 256], F32, 2)
                    zt = MB(st2, "zt", [128, 512], F32, 3)
                    gsc = SB(st2, "gsc", [128, 512]); r_gsc = R()
                    gst = MB(st2, "gst", [128, 2], F32, 2)
                    yo = MB(st2, "yo", [128, 512], BF16, 2)
                    pS = MB(st2, "pS", [128, 256], F32, 2, psum=True)
                    pCB = MB(st2, "pCB", [128, 128], F32, 1, psum=True)
                    pSeg = MB(st2, "pSeg", [128, 512], F32, 2, psum=True)
                    pY = MB(st2, "pY", [128, 256], F32, 2, psum=True)
                    pYo = MB(st2, "pYo", [128, 256], F32, 1, psum=True)
                    for d_ in range(2):
                        for g in range(2):
                            P.op("pool", lambda d_=d_, g=g: nc.gpsimd.memset(Hf[d_][g][:], 0.0), writes=[r_H[d_][g]])

                    def xdt_ops(c, d_):
                        a, r_a = xdt.nxt()
                        b, r_b = xdte.nxt()
                        h0 = 8 * d_
                        P.op("pool", lambda: nc.gpsimd.tensor_tensor(
                            out=a[:].rearrange("p (h e) -> p h e", h=8),
                            in0=xs_tm[:, c, :].rearrange("p (h e) -> p h e", h=8),
                            in1=dtt[:, c, h0:h0 + 8].unsqueeze(2).to_broadcast([128, 8, 64]), op=ALU.mult),
                            reads=[r_prep, r_dtp], writes=[r_a])
                        P.op("pool", lambda: nc.gpsimd.tensor_tensor(
                            out=b[:].rearrange("p (h e) -> p h e", h=8),
                            in0=a[:].rearrange("p (h e) -> p h e", h=8),
                            in1=dte[:, c, h0:h0 + 8].unsqueeze(2).to_broadcast([128, 8, 64]), op=ALU.mult),
                            reads=[r_a, r_dtp], writes=[r_b])
                        return (a, r_a), (b, r_b)

                    def state_update(c, d_, g, xe, r_xe):
                        pt, r_p = pS.nxt()
                        P.op("pe", lambda: nc.tensor.matmul(pt[:], lhsT=B_tm[:, c, g * 128:(g + 1) * 128],
                                                            rhs=xe[:, g * 256:(g + 1) * 256], start=True, stop=True),
                             reads=[r_prep, r_xe], writes=[r_p])
                        h0 = 8 * d_ + 4 * g
                        H = Hf[d_][g]
                        P.op("dve", lambda: nc.vector.tensor_tensor(
                            out=H[:].rearrange("p (h e) -> p h e", h=4), in0=H[:].rearrange("p (h e) -> p h e", h=4),
                            in1=cd[:, c, h0:h0 + 4].unsqueeze(2).to_broadcast([128, 4, 64]), op=ALU.mult),
                            reads=[r_H[d_][g], r_dtp], writes=[r_H[d_][g]])
                        P.op("dve", lambda: nc.vector.tensor_tensor(out=H[:], in0=H[:], in1=pt[:], op=ALU.add),
                             reads=[r_H[d_][g], r_p], writes=[r_H[d_][g]])

                    for c in range(NC - 1, -1, -1):
                        for g in range(2):
                            P.op("act", lambda c=c, g=g: nc.scalar.copy(out=Hb_all[:, c, g, :], in_=Hf[1][g][:]),
                                 reads=[r_H[1][g]], writes=[r_Hb])
                        if c > 0:
                            (_, _), (xe, r_xe) = xdt_ops(c, 1)
                            for g in range(2):
                                state_update(c, 1, g, xe, r_xe)

                    zq = []

                    def load_z(c):
                        z, r_zt = zt.nxt()
                        dma("sp", z[:], z_d[c * 128:(c + 1) * 128, :], reads=[r_z], writes=[r_zt])
                        zq.append((z, r_zt))
                    load_z(0)
                    for c in range(NC):
                        if c + 1 < NC:
                            load_z(c + 1)
                        ya, r_ya = yacc.nxt()
                        P.op("dve", lambda ya=ya, c=c: nc.vector.tensor_tensor(
                            out=ya[:].rearrange("p (h e) -> p h e", h=8),
                            in0=xs_tm[:, c, :].rearrange("p (h e) -> p h e", h=8),
                            in1=dsk_t[:].unsqueeze(2).to_broadcast([128, 8, 64]), op=ALU.mult),
                            reads=[r_prep, r_dtp], writes=[r_ya])
                        xd = [None, None]
                        for d_ in range(2):
                            xd[d_] = xdt_ops(c, d_)
                        def run_combos(c, ya, r_ya, xd):
                            pre = {}

                            def preG(g):
                                pcb, r_pcb = pCB.nxt()
                                P.op("pe", lambda: nc.tensor.matmul(
                                    pcb[:], lhsT=BT[:, g, c * 128:(c + 1) * 128], rhs=CT[:, g, c * 128:(c + 1) * 128],
                                    start=True, stop=True), reads=[r_prep], writes=[r_pcb])
                                cms = []
                                for d_ in range(2):
                                    cm, r_cm = cbm.nxt()
                                    P.op("dve", lambda cm=cm, d_=d_: nc.vector.tensor_tensor(
                                        out=cm[:], in0=pcb[:], in1=CF(CF_MF + d_), op=ALU.mult),
                                        reads=[r_pcb, r_c], writes=[r_cm])
                                    cms.append((cm, r_cm))
                                hb, r_hb = Hbf.nxt()
                                P.op("act", lambda: nc.scalar.copy(out=hb[:], in_=Hf[0][g][:]),
                                     reads=[r_H[0][g]], writes=[r_hb])
                                pre[g] = (cms, hb, r_hb)

                            cst = {}

                            def stX(g, d_):
                                h0 = 8 * d_ + 4 * g
                                rh, r_rh = RH.nxt()
                                P.op("pool", lambda: nc.gpsimd.tensor_tensor(
                                    out=rh[:], in0=CF(CF_TF + d_).unsqueeze(1).to_broadcast([128, 4, 128]),
                                    in1=adt[:, c, h0:h0 + 4].unsqueeze(2).to_broadcast([128, 4, 128]), op=ALU.mult),
                                    reads=[r_c, r_dtp], writes=[r_rh])
                                cst[(g, d_)] = dict(rh=(rh, r_rh))

                            def stY(g, d_):
                                rh, r_rh = cst[(g, d_)]["rh"]
                                psg, r_psg = pSeg.nxt()
                                P.op("pe", lambda: nc.tensor.matmul(
                                    psg[:], lhsT=CF(CF_UF + d_), rhs=rh[:].rearrange("p h l -> p (h l)"),
                                    start=True, stop=True), reads=[r_rh, r_c], writes=[r_psg])
                                eb, r_eb = Eb.nxt()
                                P.op("act", lambda: nc.scalar.activation(
                                    out=eb[:].rearrange("p h l -> p (h l)"), in_=psg[:], func=AF.Exp),
                                    reads=[r_psg], writes=[r_eb])
                                mt, r_mt = MT.nxt()
                                cm, r_cm = pre[g][0][d_]
                                P.op("dve", lambda: nc.vector.tensor_tensor(
                                    out=mt[:], in0=eb[:], in1=cm[:].unsqueeze(1).to_broadcast([128, 4, 128]),
                                    op=ALU.mult), reads=[r_eb, r_cm], writes=[r_mt])
                                cst[(g, d_)]["mt"] = (mt, r_mt)

                            def stZ(g, d_):
                                h0 = 8 * d_ + 4 * g
                                mt, r_mt = cst[(g, d_)]["mt"]
                                (xa, r_xa), (xe, r_xe) = xd[d_]
                                _, hb, r_hb = pre[g]
                                py, r_py = pY.nxt()
                                for r_ in range(4):
                                    hh = 4 * g + r_
                                    P.op("pe", lambda r_=r_, hh=hh: nc.tensor.matmul(
                                        py[:, r_ * 64:(r_ + 1) * 64], lhsT=mt[:, r_, :], rhs=xa[:, hh * 64:(hh + 1) * 64],
                                        start=True, stop=True), reads=[r_mt, r_xa], writes=[r_py])
                                pyo, r_pyo = pYo.nxt()
                                if d_ == 0:
                                    rhs_h, r_rhs = hb[:], r_hb
                                else:
                                    rhs_h, r_rhs = Hb_all[:, c, g, :], r_Hb
                                P.op("pe", lambda: nc.tensor.matmul(
                                    pyo[:], lhsT=CT[:, g, c * 128:(c + 1) * 128], rhs=rhs_h, start=True, stop=True),
                                    reads=[r_prep, r_rhs], writes=[r_pyo])
                                yt, r_yt = ytmp.nxt()
                                P.op("dve", lambda: nc.vector.tensor_tensor(
                                    out=yt[:].rearrange("p (h e) -> p h e", h=4),
                                    in0=pyo[:].rearrange("p (h e) -> p h e", h=4),
                                    in1=ecs[:, c, h0:h0 + 4].unsqueeze(2).to_broadcast([128, 4, 64]), op=ALU.mult),
                                    reads=[r_pyo, r_dtp], writes=[r_yt])
                                yg = ya[:, g * 256:(g + 1) * 256]
                                P.op("dve", lambda: nc.vector.tensor_tensor(out=yg, in0=yg, in1=yt[:], op=ALU.add),
                                     reads=[r_yt, r_ya], writes=[r_ya])
                                P.op("dve", lambda: nc.vector.tensor_tensor(out=yg, in0=yg, in1=py[:], op=ALU.add),
                                     reads=[r_py, r_ya], writes=[r_ya])

                            preG(0)
                            preG(1)
                            K4 = [(0, 0), (0, 1), (1, 0), (1, 1)]
                            stX(*K4[0]); stX(*K4[1]); stY(*K4[0]); stX(*K4[2]); stY(*K4[1]); stZ(*K4[0])
                            stX(*K4[3]); stY(*K4[2]); stZ(*K4[1]); stY(*K4[3]); stZ(*K4[2]); stZ(*K4[3])
                            if c < NC - 1:
                                for g in range(2):
                                    state_update(c, 0, g, xd[0][1][0], xd[0][1][1])

                        run_combos(c, ya, r_ya, xd)
                        z, r_zt = zq.pop(0)
                        P.op("act", lambda z=z: nc.scalar.activation(out=z[:], in_=z[:], func=AF.Silu), reads=[r_zt], writes=[r_zt])
                        P.op("dve", lambda ya=ya, z=z: nc.vector.tensor_tensor(out=ya[:], in0=ya[:], in1=z[:], op=ALU.mult),
                             reads=[r_zt, r_ya], writes=[r_ya])
                        gs, r_gs = gst.nxt()
                        rms_rstd("g", ya[:], gs[:, 0:1], gs[:, 1:2], gsc[:], r_ya, r_gs, 512)
                        yob, r_yo = yo.nxt()
                        P.op("dve", lambda yob=yob, ya=ya, gs=gs: nc.vector.scalar_tensor_tensor(
                            out=yob[:], in0=ya[:], scalar=gs[:, 1:2], in1=snw_t[:], op0=ALU.mult, op1=ALU.mult),
                            reads=[r_ya, r_gs, r_dtp], writes=[r_yo])
                        dma("sp", mix_d[c * 128:(c + 1) * 128, 0:512], yob[:], reads=[r_yo], writes=[r_mix])

            P.barrier()
            with ExitStack() as st:
                QA = [MB(st, "QA%d" % c_, [67, L], BF16, 2) for c_ in range(2)]
                KAa = [MB(st, "KAa%d" % c_, [67, L], BF16, 2) for c_ in range(2)]
                KAb = [MB(st, "KAb%d" % c_, [67, L], BF16, 2) for c_ in range(2)]
                VA = MB(st, "VA", [128, NC, 129], BF16, 2)
                btab_t = MB(st, "btab_t", [128, NC * NQB], F32, 2)
                bias_t = [MB(st, "bias_t%d" % c_, [128, NC * NQB], F32, 2) for c_ in range(2)]
                lam_t = SB(st, "lam_t", [128, 4, 64]); r_lam = R()
                lam_s = SB(st, "lam_s", [128, 4])
                subw_t = SB(st, "subw_t", [128, 128])
                nrmb = MB(st, "nrmb", [128, 2, 16], F32, 2)
                nrm = SB(st, "nrm", [128, 8]); r_nrm = R()
                Mbc = MB(st, "Mbc", [128, 2], F32, 2)
                mdiag = SB(st, "mdiag", [128, 2])
                PT = [MB(st, "PT%d" % c_, [128, 512], BF16, 3) for c_ in range(2)]
                osq2 = MB(st, "osq2", [128, 128], F32, 2)
                Sfix = MB(st, "Sfix", [128, 512], F32, 2)
                pSc = [MB(st, "pSc%d" % c_, [128, 512], F32, 2, psum=True) for c_ in range(2)]
                pO = MB(st, "pO", [128, 2, 256], F32, 4, psum=True)
                osb = MB(st, "osb", [128, 2, 129], F32, 5)
                ot = MB(st, "ot", [128, 128], F32, 2)
                ost = MB(st, "ost", [128, 4], F32, 8)
                osq = SB(st, "osq", [128, 128]); r_osq = R()
                ob = MB(st, "ob", [128, 128], BF16, 5)
                zf_list = list(range(0, na, 2)) if s == 0 else []
                zf_per = -(-len(zf_list) // (4 * NQB)) if zf_list else 0
                dma("sp", lam_t[:], lam.rearrange("a d -> (a d)").partition_broadcast(128), writes=[r_lam])
                dma("sp", subw_t[:], sub_w.partition_broadcast(128), writes=[r_lam])
                P.op("dve", lambda: nc.vector.tensor_tensor(out=lam_t[:, 0:2, :], in0=lam_t[:, 0:2, :], in1=lam_t[:, 2:4, :], op=ALU.mult),
                     reads=[r_lam], writes=[r_lam])
                P.op("dve", lambda: nc.vector.reduce_sum(out=lam_s[:, 0:2], in_=lam_t[:, 0:2, :], axis=AX.X), reads=[r_lam], writes=[r_lam])
                P.op("act", lambda: nc.scalar.activation(out=lam_s[:, 0:2], in_=lam_s[:, 0:2], func=AF.Exp), reads=[r_lam], writes=[r_lam])
                P.op("dve", lambda: nc.vector.tensor_tensor(out=lam_s[:, 2:3], in0=lam_s[:, 1:2], in1=lam_s[:, 0:1], op=ALU.subtract),
                     reads=[r_lam], writes=[r_lam])
                P.op("dve", lambda: nc.vector.tensor_scalar(out=lam_s[:, 2:3], in0=lam_s[:, 2:3], scalar1=-LAMBDA_INIT, scalar2=None, op0=ALU.add),
                     reads=[r_lam], writes=[r_lam])
                P.op("dve", lambda: nc.vector.tensor_scalar(out=subw_t[:], in0=subw_t[:], scalar1=1.0 - LAMBDA_INIT, scalar2=None, op0=ALU.mult),
                     reads=[r_lam], writes=[r_lam])
                pN = pSc[0]
                hctx = {}

                def setup_loads(h):
                    qa, ka, kb = [], [], []
                    for c_ in range(2):
                        t_, r_ = QA[c_].nxt(); qa.append((t_, r_))
                        row0 = h * 128 + c_ * 64
                        dma("sp", t_[0:64, :], qT_d[row0:row0 + 64, :], reads=[r_q], writes=[r_])
                        dma("sp", t_[64:67, :], qrows_d[h], writes=[r_])
                        t_, r_ = KAa[c_].nxt(); ka.append((t_, r_))
                        dma("sp", t_[0:64, :], kT_d[row0:row0 + 64, :], reads=[r_k], writes=[r_])
                        dma("sp", t_[64:67, :], krA_d[h], writes=[r_])
                        t_, r_ = KAb[c_].nxt(); kb.append((t_, r_))
                        dma("sp", t_[0:64, :], kT_d[row0:row0 + 64, :], reads=[r_k], writes=[r_])
                        dma("sp", t_[64:67, :], krB_d[h], writes=[r_])
                    va, r_va = VA.nxt()
                    dma("sp", va[:, :, 0:128], v_d[:, h * 128:(h + 1) * 128].rearrange("(c p) e -> p c e", p=128),
                        reads=[r_v], writes=[r_va])
                    P.op("pool", lambda va=va: nc.gpsimd.memset(va[:, :, 128:129], 1.0), writes=[r_va])
                    bt, r_bt = btab_t.nxt()
                    dma("sp", bt[:], btab_d[h:h + 1, :].partition_broadcast(128), writes=[r_bt])
                    hpre[h] = dict(qa=qa, ka=ka, kb=kb, va=(va, r_va), bt=(bt, r_bt))

                def setup_final(h):
                    pr = hpre.pop(h)
                    qa, ka, kb, bt, r_bt = pr["qa"], pr["ka"], pr["kb"], pr["bt"][0], pr["bt"][1]
                    nr, r_nr = nrmb.nxt()
                    dma("sp", nr[:, 0, :], nmax_d[0:1, :].partition_broadcast(128), writes=[r_nr])
                    dma("sp", nr[:, 1, :], nmax_d[64:65, :].partition_broadcast(128), writes=[r_nr])
                    mb, r_mb = Mbc.nxt()
                    P.op("dve", lambda: nc.vector.tensor_tensor(out=mb[:], in0=nr[:, :, h], in1=nr[:, :, 4 + h], op=ALU.mult),
                         reads=[r_nr], writes=[r_mb])
                    P.op("pool", lambda: nc.gpsimd.tensor_tensor(out=mb[:], in0=mb[:], in1=poshalf[:, 0:1].to_broadcast([128, 2]), op=ALU.pow),
                         reads=[r_mb, r_c], writes=[r_mb])
                    P.op("dve", lambda: nc.vector.tensor_scalar(out=mb[:], in0=mb[:], scalar1=-0.125 * 1.02, scalar2=None, op0=ALU.mult),
                         reads=[r_mb], writes=[r_mb])
                    bi = []
                    for c_ in range(2):
                        b_, r_b = bias_t[c_].nxt()
                        P.op("dve", lambda b_=b_, c_=c_: nc.vector.tensor_scalar(
                            out=b_[:], in0=bt[:], scalar1=mb[:, c_:c_ + 1], scalar2=None, op0=ALU.add),
                            reads=[r_bt, r_mb], writes=[r_b])
                        bi.append((b_, r_b))
                    hctx[h] = dict(qa=qa, ka=ka, kb=kb, va=pr["va"], bi=bi, s8=8.0 * (2.0 ** (-8.0 * (h + 1) / 4)))

                hpre = {}
                def kt_order(qb):
                    dg = [kt for kt in range(NC) if 4 * qb <= kt < 4 * qb + 4]
                    return [kt for kt in range(NC) if kt not in dg] + dg
                steps = []
                for h in range(4):
                    for qb in range(NQB):
                        od = kt_order(qb)
                        for pos, kt in enumerate(od):
                            for c_ in range(2):
                                steps.append(dict(h=h, qb=qb, kt=kt, c=c_, first=(pos == 0), last=(pos == NC - 1)))
                qctx = {}

                def emit_qk(sp_):
                    h, qb, kt, c_ = sp_["h"], sp_["qb"], sp_["kt"], sp_["c"]
                    cx = hctx[h]
                    caseB = kt >= 4 * qb + 4
                    diag = (4 * qb <= kt < 4 * qb + 4)
                    kop, r_kop = (cx["kb"] if caseB else cx["ka"])[c_]
                    qop, r_qop = cx["qa"][c_]
                    ps_, r_ps = pSc[c_].nxt()
                    P.op("pe", lambda: nc.tensor.matmul(
                        ps_[:], lhsT=kop[:, kt * 128:(kt + 1) * 128], rhs=qop[:, qb * 512:(qb + 1) * 512],
                        start=True, stop=True), reads=[r_kop, r_qop], writes=[r_ps])
                    src_ap, r_src = ps_[:], r_ps
                    if diag:
                        sf, r_sf = Sfix.nxt()
                        dk = kt - 4 * qb
                        s8 = cx["s8"]
                        P.op("dve", lambda: nc.vector.scalar_tensor_tensor(
                            out=sf[:], in0=CF(CF_D2 + 4 * dk, 4), scalar=-s8, in1=ps_[:], op0=ALU.mult, op1=ALU.add),
                            reads=[r_ps, r_c], writes=[r_sf])
                        src_ap, r_src = sf[:], r_sf
                    sp_["src"] = (src_ap, r_src)

                def emit_exp(sp_):
                    h, qb, kt, c_ = sp_["h"], sp_["qb"], sp_["kt"], sp_["c"]
                    src_ap, r_src = sp_["src"]
                    pt_, r_pt = PT[c_].nxt()
                    b_, r_b = hctx[h]["bi"][c_]
                    col = kt * NQB + qb
                    P.op("act", lambda: nc.scalar.activation(
                        out=pt_[:], in_=src_ap, func=AF.Exp, bias=b_[:, col:col + 1], scale=0.125),
                        reads=[r_src, r_b], writes=[r_pt])
                    sp_["pt"] = (pt_, r_pt)

                def emit_pv(sp_):
                    h, qb, kt, c_ = sp_["h"], sp_["qb"], sp_["kt"], sp_["c"]
                    if sp_["first"] and c_ == 0:
                        qctx[(h, qb)] = [pO.nxt() for _ in range(4)]
                    po = qctx[(h, qb)]
                    pt_, r_pt = sp_["pt"]
                    va, r_va = hctx[h]["va"]
                    for sub in range(4):
                        P.op("pe", lambda sub=sub: nc.tensor.matmul(
                            po[sub][0][:, c_, 0:129], lhsT=pt_[:, sub * 128:(sub + 1) * 128], rhs=va[:, kt, :],
                            start=(sp_["first"] and c_ == 0), stop=(sp_["last"] and c_ == 1), skip_group_check=True),
                            reads=[r_pt, r_va], writes=[po[sub][1]])

                def emit_epilogue(h, qb):
                    po = qctx.pop((h, qb))
                    for _ in range(zf_per):
                        if zf_list:
                            a0 = zf_list.pop(0)
                            dma("sp", xs_v[:, a0:a0 + 2, :], zero_bf[:].rearrange("p (a d) -> p a d", a=2), reads=[r_c])
                    os_l = []
                    for sub in range(4):
                        pot, r_po = po[sub]
                        o_, r_o = osb.nxt()
                        evac(o_[:], pot[:, :, 0:129], [r_po], [r_o], eng="dve")
                        os_l.append((o_, r_o))
                    for sub in range(4):
                        o_, r_o = os_l[sub]
                        t0 = qb * 512 + sub * 128
                        os_, r_os = ost.nxt()
                        P.op("dve", lambda o_=o_, os_=os_: nc.vector.reciprocal(out=os_[:, 0:2], in_=o_[:, :, 128]),
                             reads=[r_o], writes=[r_os])
                        P.op("dve", lambda os_=os_: nc.vector.tensor_tensor(out=os_[:, 1:2], in0=os_[:, 1:2], in1=lam_s[:, 2:3], op=ALU.mult),
                             reads=[r_os, r_lam], writes=[r_os])
                        oo, r_oo = ot.nxt()
                        P.op("dve", lambda oo=oo, o_=o_, os_=os_: nc.vector.tensor_scalar(
                            out=oo[:], in0=o_[:, 0, 0:128], scalar1=os_[:, 0:1], scalar2=None, op0=ALU.mult),
                            reads=[r_o, r_os], writes=[r_oo])
                        P.op("dve", lambda oo=oo, o_=o_, os_=os_: nc.vector.scalar_tensor_tensor(
                            out=oo[:], in0=o_[:, 1, 0:128], scalar=os_[:, 1:2], in1=oo[:], op0=ALU.mult, op1=ALU.add),
                            reads=[r_o, r_os, r_oo], writes=[r_oo])
                        oq, r_oq = osq2.nxt()
                        P.op("dve", lambda oo=oo, oq=oq: nc.vector.tensor_tensor(out=oq[:], in0=oo[:], in1=oo[:], op=ALU.mult),
                             reads=[r_oo], writes=[r_oq])
                        P.op("dve", lambda oq=oq, os_=os_: nc.vector.reduce_sum(out=os_[:, 2:3], in_=oq[:], axis=AX.X),
                             reads=[r_oq], writes=[r_os])
                        P.op("dve", lambda os_=os_: nc.vector.tensor_scalar(out=os_[:, 3:4], in0=os_[:, 2:3], scalar1=1.0 / 128, scalar2=EPS,
                                                                         op0=ALU.mult, op1=ALU.add), reads=[r_os], writes=[r_os])
                        P.op("pool", lambda os_=os_: nc.gpsimd.tensor_tensor(out=os_[:, 3:4], in0=os_[:, 3:4], in1=neghalf[:, 0:1], op=ALU.pow),
                             reads=[r_os, r_c], writes=[r_os])
                        ob_, r_ob = ob.nxt()
                        P.op("dve", lambda ob_=ob_, oo=oo, os_=os_: nc.vector.scalar_tensor_tensor(
                            out=ob_[:], in0=oo[:], scalar=os_[:, 3:4], in1=subw_t[:], op0=ALU.mult, op1=ALU.mult),
                            reads=[r_oo, r_os, r_lam], writes=[r_ob])
                        dma("pool", mix_d[t0:t0 + 128, 512 + h * 128:512 + (h + 1) * 128], ob_[:], reads=[r_ob], writes=[r_mix])

                setup_loads(0)
                setup_final(0)
                AHEAD, LAG = 2, 2
                nst = len(steps)
                per_head = NQB * NC * 2
                pend_parts = []
                for i in range(min(AHEAD, nst)):
                    emit_qk(steps[i])
                for i in range(nst + LAG):
                    if i < nst:
                        sp_ = steps[i]
                        hh = sp_["h"]
                        rel_i = i - hh * per_head
                        if hh + 1 < 4:
                            if rel_i == 2:
                                setup_loads(hh + 1)
                            if rel_i == per_head // 2:
                                setup_final(hh + 1)
                        emit_exp(sp_)
                        if i + AHEAD < nst:
                            emit_qk(steps[i + AHEAD])
                    j = i - LAG
                    if j >= 0:
                        sj = steps[j]
                        emit_pv(sj)
                        if sj["last"] and sj["c"] == 1:
                            emit_epilogue(sj["h"], sj["qb"])

            P.barrier()
            with ExitStack() as st:
                wo = SB(st, "wo", [128, 8, D], BF16); r_wo = R()
                wov = w_out.rearrange("(kc p) f -> p kc f", p=128)
                for c0 in range(0, D, 512):
                    dma("pool", wo[:, :, c0:c0 + 512], wov[:, :, c0:c0 + 512], writes=[r_wo])
                wr_t = SB(st, "wr_t", [128, 8, 36]); br_t = SB(st, "br_t", [128, 36])
                nfw_t = SB(st, "nfw_t", [128, D])
                dma("sp", nfw_t[:], nfw.partition_broadcast(128), writes=[r_wo])
                dma("sp", wr_t[:], wr.rearrange("(kc p) f -> p kc f", p=128), writes=[r_wo])
                dma("sp", br_t[:], br.partition_broadcast(128), writes=[r_wo])
                mx = MB(st, "mx", [128, D], BF16, 4)
                mxT = MB(st, "mxT", [128, 8, 128], BF16, 2)
                xt = MB(st, "xt2", [128, D], F32, 4)
                ht = MB(st, "ht", [128, D], F32, 2)
                sq = SB(st, "sq2", [128, D]); r_sq = R()
                stat = MB(st, "stat2", [128, 2], F32, 2)
                xn = MB(st, "xn", [128, D], F32, 3)
                xnb = MB(st, "xnb", [128, D], BF16, 2)
                xnT = MB(st, "xnT", [128, 8, 128], F32, 3)
                pT = MB(st, "pT2", [128, 8, 128], BF16, 1, psum=True)
                pH = MB(st, "pH", [128, 512], F32, 2, psum=True)
                pTf = MB(st, "pTf", [128, 4, 128], F32, 2, psum=True)
                pL = MB(st, "pL", [128, 64], F32, 2, psum=True)
                lg_all = SB(st, "lg_all", [128, NC, 36])
                r_lg = R()

                ldq = []

                def loadD(c):
                    t0 = c * 128
                    m_, r_m = mx.nxt()
                    dma("sp", m_[:], mix_d[t0:t0 + 128, :], reads=[r_mix], writes=[r_m])
                    x_, r_x = xt.nxt()
                    dma("sp", x_[:], x[s, t0:t0 + 128, :], writes=[r_x])
                    ldq.append((m_, r_m, x_, r_x))

                def stageA1(c):
                    ti = s * NC + c
                    if c + 2 < NC:
                        loadD(c + 2)
                    m_, r_m, x_, r_x = ldq.pop(0)
                    if dbg:
                        dma("pool", dbg_d["dbg_mix"][ti * 128:(ti + 1) * 128, :], m_[:], reads=[r_m])
                    pt, r_p = pT.nxt()
                    for k in range(8):
                        P.op("pe", lambda k=k, pt=pt, m_=m_: nc.tensor.transpose(
                            out=pt[:, k, :], in_=m_[:, k * 128:(k + 1) * 128], identity=ident_b), reads=[r_m, r_c], writes=[r_p])
                    mt_, r_mt = mxT.nxt()
                    evac(mt_[:], pt[:], [r_p], [r_mt])
                    return mt_, r_mt, x_, r_x

                def stageA2(c, mt_, r_mt, x_, r_x):
                    ti = s * NC + c
                    h_, r_ht = ht.nxt()
                    for half in range(2):
                        ph, r_ph = pH.nxt()
                        for k in range(8):
                            P.op("pe", lambda k=k, ph=ph, half=half: nc.tensor.matmul(
                                ph[:], lhsT=mt_[:, k, :], rhs=wo[:, k, half * 512:(half + 1) * 512], start=(k == 0), stop=(k == 7)),
                                reads=[r_mt, r_wo], writes=[r_ph])
                        P.op("dve", lambda h_=h_, ph=ph, half=half: nc.vector.tensor_tensor(
                            out=h_[:, half * 512:(half + 1) * 512], in0=x_[:, half * 512:(half + 1) * 512], in1=ph[:], op=ALU.add),
                            reads=[r_x, r_ph], writes=[r_ht])
                    dma("sp", h_d[ti * 128:(ti + 1) * 128, :], h_[:], reads=[r_ht], writes=[r_h])
                    if dbg:
                        dma("sp", dbg_d["dbg_h"][ti * 128:(ti + 1) * 128, :], h_[:], reads=[r_ht])
                    st_, r_st = stat.nxt()
                    rms_rstd("f", h_[:], st_[:, 0:1], st_[:, 1:2], sq[:], r_ht, r_st, D)
                    xn_, r_xn_ = xn.nxt()
                    P.op("dve", lambda: nc.vector.scalar_tensor_tensor(
                        out=xn_[:], in0=h_[:], scalar=st_[:, 1:2], in1=nfw_t[:], op0=ALU.mult, op1=ALU.mult),
                        reads=[r_ht, r_st, r_wo], writes=[r_xn_])
                    xb_, r_xb = xnb.nxt()
                    P.op("act", lambda: nc.scalar.copy(out=xb_[:], in_=xn_[:]), reads=[r_xn_], writes=[r_xb])
                    dma("sp", xn_d[ti * 128:(ti + 1) * 128, :], xb_[:], reads=[r_xb], writes=[r_xn])
                    return xn_, r_xn_

                def stageB1(c, xn_, r_xn_):
                    xT_, r_xT = xnT.nxt()
                    for k0 in range(0, 8, 4):
                        ptf, r_ptf = pTf.nxt()
                        for k in range(4):
                            P.op("pe", lambda k=k, k0=k0, ptf=ptf: nc.tensor.transpose(
                                out=ptf[:, k, :], in_=xn_[:, (k0 + k) * 128:(k0 + k + 1) * 128], identity=ident_f),
                                reads=[r_xn_, r_c], writes=[r_ptf])
                        evac(xT_[:, k0:k0 + 4, :], ptf[:], [r_ptf], [r_xT])
                    return xT_, r_xT

                def stageB2(c, xT_, r_xT):
                    pl, r_pl = pL.nxt()
                    for k in range(8):
                        P.op("pe", lambda k=k: nc.tensor.matmul(
                            pl[:, 0:36], lhsT=xT_[:, k, :], rhs=wr_t[:, k, :], start=(k == 0), stop=(k == 7)),
                            reads=[r_xT, r_wo], writes=[r_pl])
                    P.op("dve", lambda: nc.vector.tensor_tensor(out=lg_all[:, c, :], in0=pl[:, 0:36], in1=br_t[:], op=ALU.add),
                         reads=[r_pl, r_wo], writes=[r_lg])

                loadD(0)
                if NC > 1:
                    loadD(1)
                pend = stageA2(0, *stageA1(0))
                for c in range(NC):
                    a1 = stageA1(c + 1) if c + 1 < NC else None
                    b1 = stageB1(c, *pend)
                    nxt_ = stageA2(c + 1, *a1) if a1 is not None else None
                    stageB2(c, *b1)
                    pend = nxt_

                def route_batch(T0, lg_all, st, r_lg):
                    V = nc.vector
                    RW = [r_lg, r_route]

                    def dv(f):
                        P.op("dve", f, reads=RW, writes=RW)
                    q8 = SB(st, "q8", [128, 8, NC])
                    g4 = SB(st, "g4", [128, NC, 4])
                    me = SB(st, "me", [128, NC, NE])
                    lgG = lg_all[:, :, 0:4]
                    lgE = lg_all[:, :, 4:36]
                    b4 = lambda ap: ap.unsqueeze(2).to_broadcast([128, NC, 4])
                    b32 = lambda ap: ap.unsqueeze(2).to_broadcast([128, NC, NE])
                    o1 = oh1_all[:, T0:T0 + NC, :]
                    o2 = oh2_all[:, T0:T0 + NC, :]
                    dv(lambda: V.reduce_max(out=q8[:, 0, :], in_=lgG, axis=AX.X))
                    dv(lambda: V.tensor_tensor(out=g4[:], in0=lgG, in1=b4(q8[:, 0, :]), op=ALU.subtract))
                    P.op("act", lambda: nc.scalar.activation(out=g4[:], in_=g4[:], func=AF.Exp), reads=RW, writes=RW)
                    dv(lambda: V.reduce_sum(out=q8[:, 1, :], in_=g4[:], axis=AX.X))
                    dv(lambda: V.reciprocal(out=q8[:, 2, :], in_=q8[:, 1, :]))
                    dv(lambda: V.tensor_tensor(out=g4[:], in0=lgG, in1=b4(q8[:, 0, :]), op=ALU.is_equal))
                    dv(lambda: V.tensor_scalar(out=g4[:], in0=g4[:], scalar1=-1.0, scalar2=NEGBIG, op0=ALU.add, op1=ALU.mult))
                    for g in range(4):
                        dv(lambda g=g: V.tensor_tensor(out=me[:, :, g * 8:(g + 1) * 8], in0=lgE[:, :, g * 8:(g + 1) * 8],
                                                       in1=g4[:, :, g].unsqueeze(2).to_broadcast([128, NC, 8]), op=ALU.add))
                    dv(lambda: V.reduce_max(out=q8[:, 3, :], in_=me[:], axis=AX.X))
                    dv(lambda: V.tensor_tensor(out=o1, in0=me[:], in1=b32(q8[:, 3, :]), op=ALU.is_equal))
                    dv(lambda: V.scalar_tensor_tensor(out=me[:], in0=o1, scalar=-NEGBIG, in1=me[:], op0=ALU.mult, op1=ALU.add))
                    dv(lambda: V.reduce_max(out=q8[:, 4, :], in_=me[:], axis=AX.X))
                    dv(lambda: V.tensor_tensor(out=o2, in0=me[:], in1=b32(q8[:, 4, :]), op=ALU.is_equal))
                    dv(lambda: V.tensor_tensor(out=q8[:, 5, :], in0=q8[:, 4, :], in1=q8[:, 3, :], op=ALU.subtract))
                    P.op("act", lambda: nc.scalar.activation(out=q8[:, 5, :], in_=q8[:, 5, :], func=AF.Exp), reads=RW, writes=RW)
                    dv(lambda: V.tensor_scalar(out=q8[:, 6, :], in0=q8[:, 5, :], scalar1=1.0, scalar2=None, op0=ALU.add))
                    dv(lambda: V.reciprocal(out=q8[:, 6, :], in_=q8[:, 6, :]))
                    dv(lambda: V.tensor_tensor(out=g_all[:, T0:T0 + NC, 0], in0=q8[:, 6, :], in1=q8[:, 2, :], op=ALU.mult))
                    dv(lambda: V.tensor_tensor(out=g_all[:, T0:T0 + NC, 1], in0=g_all[:, T0:T0 + NC, 0], in1=q8[:, 5, :], op=ALU.mult))
                    aoh_all = SB(st, "aoh_all", [128, NC, NE], BF16)
                    dv(lambda: V.tensor_tensor(out=aoh_all[:], in0=o1, in1=o2, op=ALU.add))
                    for c in range(NC):
                        ti = T0 + c
                        pl2, r_pl2 = pL.nxt()
                        P.op("pe", lambda pl2=pl2, c=c: nc.tensor.matmul(pl2[:, 0:32], lhsT=tstrict_b, rhs=aoh_all[:, c, :], start=True, stop=True),
                             reads=RW + [r_c], writes=[r_pl2])
                        P.op("pe", lambda pl2=pl2, c=c: nc.tensor.matmul(pl2[:, 32:64], lhsT=ones_b, rhs=aoh_all[:, c, :], start=True, stop=True),
                             reads=RW + [r_c], writes=[r_pl2])
                        P.op("dve", lambda pl2=pl2, ti=ti: V.tensor_tensor(out=rank_all[:, ti, :], in0=pl2[:, 0:32], in1=carry[:], op=ALU.add),
                             reads=[r_pl2, r_route], writes=[r_route])
                        P.op("dve", lambda pl2=pl2: V.tensor_tensor(out=carry[:], in0=carry[:], in1=pl2[:, 32:64], op=ALU.add),
                             reads=[r_pl2, r_route], writes=[r_route])


                route_batch(s * NC, lg_all, st, r_lg)

        for s_ in range(NSEQ):
            seq_body(s_)

        P.barrier()
        with ExitStack() as st:
            V = nc.vector
            pe_ = SB(st, "pe_", [128, NE]); ps_a = SB(st, "ps_a", [128, NE]); ps_b = SB(st, "ps_b", [128, NE])
            tmpb = SB(st, "tmpb", [128, NT, NE])
            slotf = SB(st, "slotf", [128, NT, 2])
            bef = SB(st, "bef", [128, 4]); bdiag = SB(st, "bdiag", [128, 128])
            bebc = SB(st, "bebc", [128, 128])
            idxf = SB(st, "idxf", [128, NB, 4])
            pbc = MB(st, "pbc", [128, 128], F32, 1, psum=True)
            RWR = [r_route]

            def dv(f):
                P.op("dve", f, reads=RWR + [r_c], writes=RWR)
            dv(lambda: V.tensor_scalar(out=pe_[:], in0=carry[:], scalar1=0.0, scalar2=None, op0=ALU.is_gt))
            for m_ in range(1, (T + BS - 1) // BS):
                dv(lambda m_=m_: V.scalar_tensor_tensor(out=pe_[:], in0=carry[:], scalar=float(m_ * BS), in1=pe_[:],
                                                        op0=ALU.is_gt, op1=ALU.add))
            dv(lambda: V.tensor_scalar(out=pe_[:], in0=pe_[:], scalar1=float(BS), scalar2=None, op0=ALU.mult))
            src, dst = pe_, ps_a
            sh = 1
            while sh < NE:
                dv(lambda src=src, dst=dst, sh=sh: V.tensor_copy(out=dst[:, 0:sh], in_=src[:, 0:sh]))
                dv(lambda src=src, dst=dst, sh=sh: V.tensor_tensor(out=dst[:, sh:NE], in0=src[:, sh:NE], in1=src[:, 0:NE - sh], op=ALU.add))
                src, dst = dst, (ps_b if dst is ps_a else ps_a)
                if src is pe_:
                    pass
                sh *= 2
            p_end = src
            p_start = ps_b if p_end is ps_a else ps_a
            dv(lambda: V.tensor_tensor(out=p_start[:], in0=p_end[:], in1=pe_[:], op=ALU.subtract))
            dv(lambda: V.tensor_tensor(out=rank_all[:], in0=rank_all[:], in1=p_start[:].unsqueeze(1).to_broadcast([128, NT, NE]), op=ALU.add))
            for k_, oh in enumerate((oh1_all, oh2_all)):
                dv(lambda oh=oh: V.tensor_tensor(out=tmpb[:], in0=rank_all[:], in1=oh[:], op=ALU.mult))
                dv(lambda k_=k_: V.reduce_sum(out=slotf[:, :, k_], in_=tmpb[:], axis=AX.X))
            dv(lambda: V.tensor_copy(out=slot_i[:], in_=slotf[:]))
            m0 = CF_MISC * 128
            dv(lambda: V.tensor_scalar(out=pe_[:], in0=p_end[:], scalar1=cf[:, m0:m0 + 1], scalar2=None, op0=ALU.is_le))
            dv(lambda: V.reduce_sum(out=bef[:, 0:1], in_=pe_[:], axis=AX.X))
            dv(lambda: V.tensor_scalar(out=bdiag[:], in0=ident_f, scalar1=bef[:, 0:1], scalar2=None, op0=ALU.mult))
            pb, r_pb = pbc.nxt()
            P.op("pe", lambda: nc.tensor.matmul(pb[:], lhsT=ones_f, rhs=bdiag[:], start=True, stop=True), reads=RWR + [r_c], writes=[r_pb])
            P.op("dve", lambda: V.tensor_copy(out=bebc[:], in_=pb[:]), reads=[r_pb], writes=RWR)
            beadj = SB(st, "beadj", [128, 128])
            same = SB(st, "same", [128, 128])
            dv(lambda: V.tensor_copy(out=beadj[:], in_=bebc[:]))
            if NB > 2:
                dv(lambda: V.tensor_tensor(out=same[:, 2:NB], in0=bebc[:, 2:NB], in1=bebc[:, 0:NB - 2], op=ALU.is_equal))
                dv(lambda: V.scalar_tensor_tensor(out=beadj[:, 2:NB], in0=same[:, 2:NB], scalar=64.0, in1=bebc[:, 2:NB],
                                                  op0=ALU.mult, op1=ALU.add))
            for h2 in range(2):
                dv(lambda h2=h2: V.tensor_scalar(out=idxf[:, :, h2], in0=beadj[:, 0:NB], scalar1=256.0, scalar2=float(h2), op0=ALU.mult, op1=ALU.add))
                dv(lambda h2=h2: V.scalar_tensor_tensor(out=idxf[:, :, h2], in0=cf[:, m0 + 1:m0 + 2].to_broadcast([128, NB]), scalar=2.0,
                                                        in1=idxf[:, :, h2], op0=ALU.mult, op1=ALU.add))
            dv(lambda: V.tensor_copy(out=idx_g[:], in_=idxf[:, :, 0:2]))
            for fc in range(4):
                dv(lambda fc=fc: V.tensor_scalar(out=idxf[:, :, fc], in0=beadj[:, 0:NB], scalar1=512.0, scalar2=float(fc * 128), op0=ALU.mult, op1=ALU.add))
                dv(lambda fc=fc: V.tensor_tensor(out=idxf[:, :, fc], in0=idxf[:, :, fc], in1=cf[:, m0 + 1:m0 + 2].to_broadcast([128, NB]), op=ALU.add))
            dv(lambda: V.tensor_copy(out=idx_d[:], in_=idxf[:]))
            fence_t = SB(st, "fence_t", [128, 8])
            for _ in range(2):
                dv(lambda: V.memset(fence_t[:], 0.0))
            if dbg:
                dbgt = SB(st, "dbgt", [128, NT, 8])
                dv(lambda: V.memset(dbgt[:], 0.0))
                dv(lambda: V.tensor_copy(out=dbgt[:, :, 0:2], in_=slotf[:]))
                dv(lambda: V.tensor_copy(out=dbgt[:, :, 2:4], in_=g_all[:]))
                dv(lambda: V.tensor_copy(out=dbgt[:, :, 4:5], in_=bebc[:, 0:NT].unsqueeze(2)))
                dma("sp", dbg_d["dbg_route"], dbgt[:].rearrange("p a b -> p (a b)"), reads=RWR)
            xl = MB(st, "xl", [128, D], BF16, 3)
            bcs = {}

            def mk_bcs():
                bcs["s"] = nc.gpsimd.alloc_register("bc_slot")
                return nc.gpsimd.reg_mov(bcs["s"], NSLOT - 1)
            P.op("pool", mk_bcs)
            for ti in range(NT):
                t_, r_t = xl.nxt()
                dma("sp", t_[:], xn_d[ti * 128:(ti + 1) * 128, :], reads=[r_xn], writes=[r_t])
                for k_ in range(2):
                    P.op("pool", lambda t_=t_, ti=ti, k_=k_: nc.gpsimd.indirect_dma_start(
                        out=xs_d, out_offset=bass.IndirectOffsetOnAxis(ap=slot_i[:, ti, k_:k_ + 1], axis=0),
                        in_=t_[:], in_offset=None, bounds_check=bcs["s"], oob_is_err=False),
                        reads=[r_t, r_route, r_xs], writes=[r_xs], dma=True)

        P.barrier()
        with ExitStack() as st:
            Wg = MB(st, "Wg", [128, 2, 2048], BF16, 2)
            Wu = MB(st, "Wu", [128, 2, 2048], BF16, 2)
            Wd = MB(st, "Wd", [128, 4, 1024], BF16, 2)
            xb = MB(st, "xb", [128, D], BF16, 2 * NSUB)
            xbT = MB(st, "xbT", [128, 8, BS], BF16, 2)
            hd = MB(st, "hd", [128, 4, BS], BF16, 2)
            sg = MB(st, "sg", [128, BS], F32, 2)
            ysb = MB(st, "ysb", [128, D], BF16, 3)
            pT = MB(st, "pT3", [128, 8, 128], BF16, 2, psum=True)
            pG = MB(st, "pG", [128, BS], F32, 2, psum=True)
            pU = MB(st, "pU", [128, BS], F32, 2, psum=True)
            pYm = MB(st, "pYm", [128, 512], F32, 2, psum=True)
            bcr = {}

            def mk_bc():
                bcr["g"] = nc.gpsimd.alloc_register("bc_g")
                bcr["d"] = nc.gpsimd.alloc_register("bc_d")
                nc.gpsimd.reg_mov(bcr["g"], NE * 256 - 1)
                return nc.gpsimd.reg_mov(bcr["d"], NE * 512 - 1)
            P.op("pool", mk_bc)
            def do_T(xtiles):
                xT_, r_xT = xbT.nxt()
                for sub in range(NSUB):
                    x_, r_x = xtiles[sub]
                    pt, r_p = pT.nxt()
                    for j in range(8):
                        P.op("pe", lambda j=j, pt=pt, x_=x_: nc.tensor.transpose(
                            out=pt[:, j, :], in_=x_[:].rearrange("s (p j) -> s j p", j=8)[:, j, :], identity=ident_b),
                            reads=[r_x, r_c], writes=[r_p])
                    evac(xT_[:, :, sub * 128:(sub + 1) * 128], pt[:], [r_p], [r_xT])
                return xT_, r_xT

            for b in range(NB):
                g_, r_g = Wg.nxt()
                u_, r_u = Wu.nxt()
                d_, r_d = Wd.nxt()
                for h2 in range(2):
                    P.op("pool", lambda g_=g_, b=b, h2=h2: nc.gpsimd.indirect_dma_start(
                        out=g_[:, h2, :], out_offset=None, in_=wg,
                        in_offset=bass.IndirectOffsetOnAxis(ap=idx_g[:, b, h2:h2 + 1], axis=0),
                        bounds_check=bcr["g"], oob_is_err=False),
                        reads=[r_route], writes=[r_g], dma=True)
                    P.op("pool", lambda u_=u_, b=b, h2=h2: nc.gpsimd.indirect_dma_start(
                        out=u_[:, h2, :], out_offset=None, in_=wu,
                        in_offset=bass.IndirectOffsetOnAxis(ap=idx_g[:, b, h2:h2 + 1], axis=0),
                        bounds_check=bcr["g"], oob_is_err=False),
                        reads=[r_route], writes=[r_u], dma=True)
                for fc in range(4):
                    P.op("pool", lambda d_=d_, b=b, fc=fc: nc.gpsimd.indirect_dma_start(
                        out=d_[:, fc, :], out_offset=None, in_=wd,
                        in_offset=bass.IndirectOffsetOnAxis(ap=idx_d[:, b, fc:fc + 1], axis=0),
                        bounds_check=bcr["d"], oob_is_err=False),
                        reads=[r_route], writes=[r_d], dma=True)
                if b == 0:
                    xq = []
                    for sub in range(NSUB):
                        x_, r_x = xb.nxt()
                        dma("sp", x_[:], xs_d[sub * 128:(sub + 1) * 128, :], reads=[r_xs], writes=[r_x])
                        xq.append((x_, r_x))
                    xT_pend = do_T(xq)
                xT_, r_xT = xT_pend
                if b + 1 < NB:
                    xq = []
                    for sub in range(NSUB):
                        x_, r_x = xb.nxt()
                        r0 = (b + 1) * BS + sub * 128
                        dma("sp", x_[:], xs_d[r0:r0 + 128, :], reads=[r_xs], writes=[r_x])
                        xq.append((x_, r_x))
                h_, r_h_ = hd.nxt()
                gv = g_[:].rearrange("p a (j f) -> p (a j) f", f=512)
                uv = u_[:].rearrange("p a (j f) -> p (a j) f", f=512)
                for fc in range(4):
                    pg, r_pg = pG.nxt()
                    pu, r_pu = pU.nxt()
                    for j in range(8):
                        P.op("pe", lambda j=j, pg=pg, gv=gv, fc=fc, xT_=xT_: nc.tensor.matmul(
                            pg[:], lhsT=gv[:, j, fc * 128:(fc + 1) * 128], rhs=xT_[:, j, :], start=(j == 0), stop=(j == 7)),
                            reads=[r_g, r_xT], writes=[r_pg])
                    for j in range(8):
                        P.op("pe", lambda j=j, pu=pu, uv=uv, fc=fc, xT_=xT_: nc.tensor.matmul(
                            pu[:], lhsT=uv[:, j, fc * 128:(fc + 1) * 128], rhs=xT_[:, j, :], start=(j == 0), stop=(j == 7)),
                            reads=[r_u, r_xT], writes=[r_pu])
                    s_, r_s = sg.nxt()
                    P.op("act", lambda s_=s_, pg=pg: nc.scalar.activation(out=s_[:], in_=pg[:], func=AF.Silu), reads=[r_pg], writes=[r_s])
                    P.op("dve", lambda h_=h_, s_=s_, pu=pu, fc=fc: nc.vector.tensor_tensor(out=h_[:, fc, :], in0=s_[:], in1=pu[:], op=ALU.mult),
                         reads=[r_s, r_pu], writes=[r_h_])
                if b + 1 < NB:
                    xT_pend = do_T(xq)
                for sub in range(NSUB):
                    y_, r_y_ = ysb.nxt()
                    for half in range(2):
                        py, r_py = pYm.nxt()
                        for fc in range(4):
                            P.op("pe", lambda fc=fc, py=py, h_=h_, d_=d_, sub=sub, half=half: nc.tensor.matmul(
                                py[:], lhsT=h_[:, fc, sub * 128:(sub + 1) * 128], rhs=d_[:, fc, half * 512:(half + 1) * 512],
                                start=(fc == 0), stop=(fc == 3)), reads=[r_h_, r_d], writes=[r_py])
                        evac(y_[:, half * 512:(half + 1) * 512], py[:], [r_py], [r_y_])
                    r0 = b * BS + sub * 128
                    dma("sp", y_d[r0:r0 + 128, :], y_[:], reads=[r_y_], writes=[r_y])

        P.barrier()
        with ExitStack() as st:
            ht = MB(st, "ht3", [128, D], F32, 5)
            nlw_t = SB(st, "nlw_t", [128, D]); r_nl = R()
            dma("sp", nlw_t[:], nlw.partition_broadcast(128), writes=[r_nl])
            y1 = MB(st, "y1", [128, D], BF16, 4)
            y2 = MB(st, "y2", [128, D], BF16, 4)
            sq = SB(st, "sq3", [128, D])
            stat = MB(st, "stat3", [128, 2], F32, 4)
            ot = MB(st, "ot3", [128, D], F32, 3)
            outv = out.rearrange("s l d -> (s l) d")
            hq = []

            def load_h(ti):
                h_, r_ht = ht.nxt()
                dma("sp", h_[:], h_d[ti * 128:(ti + 1) * 128, :], reads=[r_h], writes=[r_ht])
                hq.append((h_, r_ht))
            PRE = 3
            for ti in range(min(PRE, NT)):
                load_h(ti)
            for ti in range(NT):
                if ti + PRE < NT:
                    load_h(ti + PRE)
                h_, r_ht = hq.pop(0)
                ys = []
                for k_, yb in enumerate((y1, y2)):
                    y_, r_y_ = yb.nxt()
                    P.op("pool", lambda y_=y_, ti=ti, k_=k_: nc.gpsimd.indirect_dma_start(
                        out=y_[:], out_offset=None, in_=y_d,
                        in_offset=bass.IndirectOffsetOnAxis(ap=slot_i[:, ti, k_:k_ + 1], axis=0),
                        bounds_check=bcs["s"], oob_is_err=False),
                        reads=[r_route, r_y], writes=[r_y_], dma=True)
                    ys.append((y_, r_y_))
                for k_ in range(2):
                    y_, r_y_ = ys[k_]
                    P.op("dve", lambda h_=h_, y_=y_, ti=ti, k_=k_: nc.vector.scalar_tensor_tensor(
                        out=h_[:], in0=y_[:], scalar=g_all[:, ti, k_:k_ + 1], in1=h_[:], op0=ALU.mult, op1=ALU.add),
                        reads=[r_y_, r_ht, r_route], writes=[r_ht])
                st_, r_st = stat.nxt()
                rms_rstd("z", h_[:], st_[:, 0:1], st_[:, 1:2], sq[:], r_ht, r_st, D)
                o_, r_o = ot.nxt()
                P.op("dve", lambda o_=o_, h_=h_, st_=st_: nc.vector.scalar_tensor_tensor(
                    out=o_[:], in0=h_[:], scalar=st_[:, 1:2], in1=nlw_t[:], op0=ALU.mult, op1=ALU.mult),
                    reads=[r_ht, r_st, r_nl], writes=[r_o])
                dma("sp", outv[ti * 128:(ti + 1) * 128, :], o_[:], reads=[r_o])
        P.emit(top)
    return nc


def host_inputs(inp, L, BS, n_cores, nseq):
    consts, _ = make_consts(L, BS)
    f = lambda a: np.ascontiguousarray(np.asarray(a, dtype=np.float32))
    common = dict(
        w_in=f(inp["w_in"][0]), w_out=f(inp["w_out"][0]),
        wr=f(np.concatenate([inp["w_router_group"][0], inp["w_router_exp"][0]], axis=1)),
        br=f(np.concatenate([inp["b_router_group"][0], inp["b_router_exp"][0]])[None, :]),
        wg=f(inp["w_exp_gate"][0]).reshape(NE * 128 * 2, 2048),
        wu=f(inp["w_exp_up"][0]).reshape(NE * 128 * 2, 2048),
        wd=f(inp["w_exp_down"][0]).reshape(NE * 512, 1024),
        nmw=f(inp["norm_mix_w"]), nfw=f(inp["norm_ffn_w"]), nlw=f(inp["norm_final_w"])[None, :],
        convw=f(np.asarray(inp["conv_w"][0]).T.reshape(8, 128, 5).transpose(1, 0, 2).reshape(128, 40)),
        convb=f(np.asarray(inp["conv_b"][0]).reshape(8, 128).T),
        dtb=f(np.concatenate([inp["dt_bias_fwd"][0], inp["dt_bias_bwd"][0]])[None, :]),
        alog=f(np.concatenate([inp["a_log_fwd"][0], inp["a_log_bwd"][0]])[None, :]),
        dsk=f(inp["ssd_d"]), snw=f(inp["ssd_norm_w"]),
        lam=f(np.stack([inp["lambda_q1"][0], inp["lambda_q2"][0], inp["lambda_k1"][0], inp["lambda_k2"][0]])),
        sub_w=f(inp["subln_w"]),
        **consts,
    )
    xs = f(inp["x"])
    maps = []
    for c in range(n_cores):
        m = dict(common)
        m["x"] = np.ascontiguousarray(xs[c * nseq:(c + 1) * nseq])
        maps.append(m)
    return maps


def kernel(**inputs):
    n_cores, nseq, L, BS = 8, 2, 4096, 512
    nc = build(nseq, L, BS)
    maps = host_inputs(inputs, L, BS, n_cores, nseq)
    res = run_bass_kernel_spmd(nc, maps, core_ids=list(range(n_cores)))
    return np.concatenate([np.asarray(r["out"], dtype=np.float32) for r in res.results], axis=0)
```

```python
import math
from contextlib import ExitStack
import numpy as np
import ml_dtypes
import concourse.bass as bass
import concourse.mybir as mybir
from concourse.bass_utils import run_bass_kernel_spmd

F32 = mybir.dt.float32
BF16 = mybir.dt.bfloat16
I32 = mybir.dt.int32
ALU = mybir.AluOpType
AF = mybir.ActivationFunctionType
AX = mybir.AxisListType

D = 1024
NE = 32
DE = 512
EPS = 1e-6
LAMBDA_INIT = 0.2
NEGBIG = 30000.0
STRICT_SYNC = True


class R:
    __slots__ = ("w", "rd")

    def __init__(self):
        self.w = None
        self.rd = {}


class Prog:
    ENG = ("pe", "act", "dve", "pool", "sp")

    def __init__(self, nc, n_dma_sems=14):
        self.nc = nc
        self.ops = []
        self.n_dma_sems = n_dma_sems

    def op(self, eng, emit, reads=(), writes=(), dma=False):
        reads = [r for r in reads if r is not None]
        writes = [w for w in writes if w is not None]
        self.ops.append(dict(eng=eng, emit=emit, reads=tuple(reads), writes=tuple(writes),
                             dma=dma, deps=set(), marked=False, barrier=False))

    def barrier(self):
        for e in self.ENG:
            self.ops.append(dict(eng=e, emit=None, reads=(), writes=(), dma=False, deps=set(),
                                 marked=False, barrier=True))

    def _analyze(self):
        ops = self.ops
        last_on_eng = {e: None for e in self.ENG}
        dmas = []
        for i, o in enumerate(ops):
            e = o["eng"]
            if o["barrier"]:
                for e2 in self.ENG:
                    if e2 != e and last_on_eng[e2] is not None:
                        o["deps"].add(last_on_eng[e2])
                o["deps"].update(dmas)
                if e == self.ENG[-1]:
                    dmas = []
                continue
            deps = set()
            for r in o["reads"]:
                if r.w is not None:
                    deps.add(("raw", r.w))
            for w in o["writes"]:
                if w.w is not None:
                    deps.add(("waw", w.w))
                for j in w.rd.values():
                    deps.add(("war", j))
            for kind, j in deps:
                if j == i:
                    continue
                oj = ops[j]
                if oj["eng"] == e and not oj["dma"] and not o["dma"]:
                    if e == "pe" or (kind != "raw" and not STRICT_SYNC):
                        continue
                o["deps"].add(j)
            for r in o["reads"]:
                r.rd[("dma", i) if o["dma"] else e] = i
            for w in o["writes"]:
                w.w = i
                w.rd = {}
            if o["dma"]:
                dmas.append(i)
            else:
                last_on_eng[e] = i
        for o in ops:
            for j in o["deps"]:
                ops[j]["marked"] = True

    def emit(self, stack):
        nc = self.nc
        self._analyze()
        ops = self.ops
        sems = {e: stack.enter_context(nc.semaphore("s_" + e)) for e in self.ENG}
        dq = ("sp", "pool", "act")
        dsem = {e: [stack.enter_context(nc.semaphore("d_%s_%d" % (e, k)))
                    for k in range(self.n_dma_sems)] for e in dq}
        cnt = {e: 0 for e in self.ENG}
        dcount = {e: 0 for e in dq}
        dval = {e: [0] * self.n_dma_sems for e in dq}
        prev_on_sem = {}
        for i, o in enumerate(ops):
            if o["barrier"]:
                continue
            e = o["eng"]
            if o["dma"]:
                k = dcount[e] % self.n_dma_sems
                dcount[e] += 1
                o["prev_same_sem"] = prev_on_sem.get((e, k))
                dval[e][k] += 16
                o["done"] = (dsem[e][k], dval[e][k], ("d", e, k))
                prev_on_sem[(e, k)] = i
            elif o["marked"]:
                cnt[e] += 1
                o["done"] = (sems[e], cnt[e], ("c", e))
        final_dma = [(dsem[e][k], dval[e][k]) for e in dq for k in range(self.n_dma_sems)
                     if dval[e][k] > 0]
        blk = stack.enter_context(nc.Block())
        for e in self.ENG:
            my = [(i, o) for i, o in enumerate(ops) if o["eng"] == e]

            def body(engine, my=my, e=e):
                seen = {}
                for i, o in my:
                    deps = set(o["deps"])
                    if o["dma"] and o.get("prev_same_sem") is not None:
                        deps.add(o["prev_same_sem"])
                    need = {}
                    for j in deps:
                        s, v, key = ops[j]["done"]
                        if seen.get(key, 0) >= v:
                            continue
                        if key not in need or need[key][1] < v:
                            need[key] = (s, v)
                    for key, (s, v) in need.items():
                        engine.wait_ge(s, v)
                        seen[key] = v
                    if o["barrier"]:
                        continue
                    ins = o["emit"]()
                    if "done" in o:
                        ins.then_inc(o["done"][0], 16 if o["dma"] else 1)
                if e == "sp":
                    for s, v in final_dma:
                        engine.wait_ge(s, v)

            getattr(blk, {"pe": "tensor", "act": "scalar", "dve": "vector",
                          "pool": "gpsimd", "sp": "sync"}[e])(body)


CF_IDENT, CF_ONES, CF_TF, CF_TB, CF_UF, CF_UB, CF_MF, CF_MB = range(8)
CF_D2 = 8
CF_MISC = 8 + 16
NCF = (8 + 16) * 128 + 16


def make_consts(L, BS):
    t = np.arange(128)
    a = t[:, None]
    b = t[None, :]
    cf = np.zeros((128, NCF), np.float32)

    def put(k, m):
        cf[:, k * 128:(k + 1) * 128] = m
    put(CF_IDENT, (a == b))
    put(CF_ONES, np.ones((128, 128)))
    put(CF_TF, (a <= b))
    put(CF_TB, (a >= b))
    put(CF_UF, (a > b))
    put(CF_UB, (a < b))
    put(CF_MF, (b >= a))
    put(CF_MB, (b <= a))
    ii = np.arange(512)[None, :]
    for dk in range(4):
        d2 = 2.0 * np.maximum(a + 128 * dk - ii, 0)
        cf[:, (CF_D2 + 4 * dk) * 128:(CF_D2 + 4 * dk + 4) * 128] = d2
    m0 = CF_MISC * 128
    cf[:, m0 + 0] = t * BS
    cf[:, m0 + 1] = t
    cb = np.zeros((128, 4 * 128), np.float32)
    cb[:, 384:512] = ((a < 64) == (b < 64))
    cb[:, 0:128] = (a == b)
    cb[:, 128:256] = 1.0
    cb[:, 256:384] = (a < b)
    cb = cb.astype(ml_dtypes.bfloat16)
    slopes = [2.0 ** (-8.0 * (h + 1) / 4) for h in range(4)]
    pos = np.arange(L)
    ip = pos % 512
    jp = pos % 128
    qrows = np.zeros((4, 3, L), np.float32)
    krA = np.zeros((4, 3, L), np.float32)
    for h in range(4):
        s8 = 8.0 * slopes[h]
        qrows[h, 0] = -s8 * (ip % 256)
        qrows[h, 1] = -s8 * 256 * (ip // 256)
        qrows[h, 2] = 1.0
        krA[h, 0] = 1.0
        krA[h, 1] = 1.0
        krA[h, 2] = s8 * jp
    krB = -krA
    nkt, nqb = L // 128, L // 512
    btab = np.zeros((4, nkt * nqb), np.float32)
    for h in range(4):
        for kt in range(nkt):
            for qb in range(nqb):
                if kt >= 4 * qb + 4:
                    v = -(128 * kt - 512 * qb)
                else:
                    v = -(512 * qb - 128 * kt)
                btab[h, kt * nqb + qb] = slopes[h] * v
    return dict(cf=cf, cb=cb, qrows=qrows.astype(ml_dtypes.bfloat16),
                krA=krA.astype(ml_dtypes.bfloat16), krB=krB.astype(ml_dtypes.bfloat16), btab=btab), slopes


def build(NSEQ, L, BS, dbg=False):
    nc = bass.Bass("TRN2", target_bir_lowering=False)
    T = NSEQ * L
    NT = T // 128
    NC = L // 128
    NQB = L // 512
    NB = (2 * T) // BS + NE
    NSUB = BS // 128
    NSLOT = NB * BS
    assert NB <= 128

    def din(name, shape, dt=F32):
        return nc.dram_tensor(name, list(shape), dt, kind="ExternalInput").ap()

    def dscr(name, shape, dt):
        return nc.dram_tensor(name, list(shape), dt, kind="Internal").ap()

    x = din("x", [NSEQ, L, D])
    w_in = din("w_in", [D, 3088])
    w_out = din("w_out", [D, D])
    wr = din("wr", [D, 36])
    br = din("br", [1, 36])
    wg = din("wg", [NE * 128 * 2, 2048])
    wu = din("wu", [NE * 128 * 2, 2048])
    wd = din("wd", [NE * 512, 1024])
    nmw = din("nmw", [1, D])
    nfw = din("nfw", [1, D])
    nlw = din("nlw", [1, D])
    convw = din("convw", [128, 40])
    convb = din("convb", [128, 8])
    dtb = din("dtb", [1, 16])
    alog = din("alog", [1, 16])
    dsk = din("dsk", [1, 8])
    snw = din("snw", [1, 512])
    lam = din("lam", [4, 64])
    sub_w = din("sub_w", [1, 128])
    cf_d = din("cf", [128, NCF])
    cb_d = din("cb", [128, 512], BF16)
    qrows_d = din("qrows", [4, 3, L], BF16)
    krA_d = din("krA", [4, 3, L], BF16)
    krB_d = din("krB", [4, 3, L], BF16)
    btab_d = din("btab", [4, NC * NQB])
    out = nc.dram_tensor("out", [NSEQ, L, D], F32, kind="ExternalOutput").ap()

    nmax_d = dscr("nmax_d", [128, 16], F32)
    xbc_d = dscr("xbc_d", [D, L + 4], BF16)
    qT_d = dscr("qT_d", [512, L], BF16)
    kT_d = dscr("kT_d", [512, L], BF16)
    v_d = dscr("v_d", [L, 512], BF16)
    z_d = dscr("z_d", [L, 512], F32)
    dt_d = dscr("dt_d", [L, 16], F32)
    mix_d = dscr("mix_d", [L, D], BF16)
    h_d = dscr("h_d", [T, D], F32)
    xn_d = dscr("xn_d", [T, D], BF16)
    xs_d = dscr("xs_d", [NSLOT, D], BF16)
    y_d = dscr("y_d", [NSLOT, D], BF16)
    dbg_d = {}
    if dbg:
        for nm, shp in (("dbg_mix", [NSEQ * L, D]), ("dbg_h", [T, D]), ("dbg_route", [128, NT * 8])):
            dbg_d[nm] = nc.dram_tensor(nm, shp, F32, kind="ExternalOutput").ap()

    P = Prog(nc)
    top = ExitStack()
    with top:
        uid = [0]

        def SB(st, name, shape, dt=F32):
            uid[0] += 1
            return st.enter_context(nc.sbuf_tensor("sb_%s_%d" % (name, uid[0]), list(shape), dt))

        def PS(st, name, shape, dt=F32):
            uid[0] += 1
            return st.enter_context(nc.psum_tensor("ps_%s_%d" % (name, uid[0]), list(shape), dt))

        class MB:
            def __init__(self, st, name, shape, dt=F32, n=2, psum=False):
                mk = PS if psum else SB
                self.t = [mk(st, "%s%d" % (name, i), shape, dt) for i in range(n)]
                self.r = [R() for _ in range(n)]
                self.n = n
                self.k = -1

            def nxt(self):
                self.k += 1
                i = self.k % self.n
                return self.t[i], self.r[i]

        rr = {"ev": 0}

        def evac(out_ap, in_ap, reads, writes, eng=None):
            if eng is None:
                eng = ("act", "dve")[rr["ev"] % 2]
                rr["ev"] += 1
            if eng == "act":
                P.op("act", lambda: nc.scalar.copy(out=out_ap, in_=in_ap), reads=reads, writes=writes)
            else:
                P.op("dve", lambda: nc.vector.tensor_copy(out=out_ap, in_=in_ap), reads=reads, writes=writes)

        def dma(eng, out_ap, in_ap, reads=(), writes=()):
            q = {"sp": nc.sync, "pool": nc.gpsimd, "act": nc.scalar}[eng]
            P.op(eng, lambda: q.dma_start(out=out_ap, in_=in_ap), reads=reads, writes=writes, dma=True)

        cf = SB(top, "cf", [128, NCF]); r_c = R()
        cb = SB(top, "cb", [128, 512], BF16)
        dma("sp", cf[:], cf_d, writes=[r_c])
        dma("sp", cb[:], cb_d, writes=[r_c])

        def CF(k, n=1):
            return cf[:, k * 128:(k + n) * 128]
        ident_f = CF(CF_IDENT)
        ones_f = CF(CF_ONES)
        ident_b = cb[:, 0:128]
        ones_b = cb[:, 128:256]
        tstrict_b = cb[:, 256:384]
        bones_c = cb[:, 384:512]
        eps_t = SB(top, "eps_t", [128, 1])
        P.op("pool", lambda: nc.gpsimd.memset(eps_t[:], EPS), writes=[r_c])
        neghalf = SB(top, "neghalf", [128, 1])
        P.op("pool", lambda: nc.gpsimd.memset(neghalf[:], -0.5), writes=[r_c])
        poshalf = SB(top, "poshalf", [128, 1])
        P.op("pool", lambda: nc.gpsimd.memset(poshalf[:], 0.5), writes=[r_c])
        zero_bf = SB(top, "zero_bf", [128, 2048], BF16)
        if dbg:
            pass
        P.op("pool", lambda: nc.gpsimd.memset(zero_bf[:], 0.0), writes=[r_c])
        xs_v = xs_d.rearrange("(a p) d -> p a d", p=128)
        r_xs = None
        na = NSLOT // 128
        r_xbc = None
        dma("sp", xbc_d[:, 0:2].rearrange("(a p) d -> p a d", p=128), zero_bf[:, 0:16].rearrange("p (a d) -> p a d", d=2),
            reads=[r_c], writes=[r_xbc])
        dma("sp", xbc_d[:, L + 2:L + 4].rearrange("(a p) d -> p a d", p=128), zero_bf[:, 0:16].rearrange("p (a d) -> p a d", d=2),
            reads=[r_c], writes=[r_xbc])


        oh1_all = SB(top, "oh1_all", [128, NT, NE], BF16)
        oh2_all = SB(top, "oh2_all", [128, NT, NE], BF16)
        rank_all = SB(top, "rank_all", [128, NT, NE])
        g_all = SB(top, "g_all", [128, NT, 2])
        carry = SB(top, "carry", [128, NE])
        slot_i = SB(top, "slot_i", [128, NT, 2], I32)
        idx_g = SB(top, "idx_g", [128, NB, 2], I32)
        idx_d = SB(top, "idx_d", [128, NB, 4], I32)
        r_route = R()
        P.op("pool", lambda: nc.gpsimd.memset(carry[:], 0.0), writes=[r_route])

        r_q = r_k = r_v = r_z = r_dt = r_mix = None
        r_h = r_xn = r_y = None

        def rms_rstd(st_name, src_ap, ss, rstd, sq_scr, r_src, r_s, n):
            P.op("act", lambda: nc.scalar.activation(out=sq_scr, in_=src_ap, func=AF.Square, accum_out=ss),
                 reads=[r_src], writes=[r_s])
            P.op("act", lambda: nc.scalar.activation(out=rstd, in_=ss, func=AF.Ln, bias=eps_t[:, 0:1],
                                                     scale=1.0 / n), reads=[r_s, r_c], writes=[r_s])
            P.op("act", lambda: nc.scalar.activation(out=rstd, in_=rstd, func=AF.Exp, scale=-0.5),
                 reads=[r_s], writes=[r_s])

        def seq_body(s):
            P.barrier()
            with ExitStack() as st:
                win = SB(st, "win", [128, 8, 3088], BF16); r_win = R()
                nmw_t = SB(st, "nmw_t", [128, D])
                r_wparts = [R()]
                dma("sp", nmw_t[:], nmw.partition_broadcast(128), writes=[r_wparts[0]])
                wv = w_in.rearrange("(kc p) f -> p kc f", p=128)
                for c0 in range(0, 3088, 512):
                    c1 = min(3088, c0 + 512)
                    r_wparts.append(R())
                    dma("pool", win[:, :, c0:c1], wv[:, :, c0:c1], writes=[r_wparts[-1]])
                jn = SB(st, "jn", [128, 2])
                P.op("pool", lambda: nc.gpsimd.memset(jn[:], 0.0), reads=r_wparts, writes=[r_win])
                xt = MB(st, "xt", [128, D], F32, 4)
                sq = SB(st, "sq", [128, D]); r_sq = R()
                stat = MB(st, "stat", [128, 2], F32, 4)
                ub = MB(st, "ub", [128, D], BF16, 4)
                uT = MB(st, "uT", [128, 8, 512], BF16, 2)
                pT = MB(st, "pT", [128, 8, 128], BF16, 2, psum=True)
                pA = MB(st, "pA", [128, 512], F32, 3, psum=True)
                pB = MB(st, "pB", [128, 512], F32, 2, psum=True)
                zs = MB(st, "zs", [128, 512], F32, 2)
                vs = MB(st, "vs", [128, 512], BF16, 2)
                ds = MB(st, "ds", [128, 16], F32, 2)
                fs = MB(st, "fs", [128, 512], BF16, 3)
                sqA = MB(st, "sqA", [128, 512], BF16, 5)
                pNA = MB(st, "pNA", [128, 512], F32, 1, psum=True)
                nmax = SB(st, "nmax", [128, 16]); nmt = SB(st, "nmt", [128, 1]); r_nm = R()
                P.op("pool", lambda: nc.gpsimd.memset(nmax[:], 0.0), writes=[r_nm])
                def prepA1(tb):
                    ubs = []
                    for j in range(4):
                        t0 = tb * 512 + j * 128
                        xtt, r_xt = xt.nxt()
                        dma("sp", xtt[:], x[s, t0:t0 + 128, :], writes=[r_xt])
                        stt, r_st = stat.nxt()
                        rms_rstd("a", xtt[:], stt[:, 0:1], stt[:, 1:2], sq[:], r_xt, r_st, D)
                        ubt, r_ub = ub.nxt()
                        P.op("dve", lambda ubt=ubt, xtt=xtt, stt=stt: nc.vector.scalar_tensor_tensor(
                            out=ubt[:], in0=xtt[:], scalar=stt[:, 1:2], in1=nmw_t[:], op0=ALU.mult, op1=ALU.mult),
                            reads=[r_xt, r_st, r_win], writes=[r_ub])
                        ubs.append((ubt, r_ub))
                    return ubs

                def prepA2(ubs):
                    uTt, r_uT = uT.nxt()
                    for j in range(4):
                        ubt, r_ub = ubs[j]
                        pTt, r_pT = pT.nxt()
                        for k in range(8):
                            P.op("pe", lambda k=k, pTt=pTt, ubt=ubt: nc.tensor.transpose(
                                out=pTt[:, k, :], in_=ubt[:, k * 128:(k + 1) * 128], identity=ident_b),
                                reads=[r_ub, r_c], writes=[r_pT])
                        evac(uTt[:, :, j * 128:(j + 1) * 128], pTt[:], [r_pT], [r_uT])
                    return uTt, r_uT

                pend_bm = []

                def emit_bm(sqb, r_sqb, cc):
                    pn, r_pn = pNA.nxt()
                    P.op("pe", lambda: nc.tensor.matmul(pn[:], lhsT=bones_c, rhs=sqb[:], start=True, stop=True),
                         reads=[r_sqb, r_c], writes=[r_pn])
                    P.op("dve", lambda: nc.vector.reduce_max(out=nmt[:, 0:1], in_=pn[:], axis=AX.X),
                         reads=[r_pn], writes=[r_nm])
                    P.op("dve", lambda: nc.vector.tensor_tensor(out=nmax[:, cc - 8:cc - 7], in0=nmax[:, cc - 8:cc - 7],
                                                                in1=nmt[:, 0:1], op=ALU.max), reads=[r_nm], writes=[r_nm])

                pendA = prepA2(prepA1(0))
                NTB = L // 512
                for tb in range(NTB):
                    uTt, r_uT = pendA
                    ubs_n = prepA1(tb + 1) if tb + 1 < NTB else None
                    for j in range(4):
                        t0 = tb * 512 + j * 128
                        for (c0, n, kind) in ((0, 512, "z"), (1536, 16, "dt"), (2576, 512, "v")):
                            pt, r_p = pA.nxt()
                            for k in range(8):
                                P.op("pe", lambda k=k, pt=pt, c0=c0, n=n, j=j, uTt=uTt: nc.tensor.matmul(
                                    pt[:, 0:n], lhsT=uTt[:, k, j * 128:(j + 1) * 128], rhs=win[:, k, c0:c0 + n],
                                    start=(k == 0), stop=(k == 7)), reads=[r_uT, r_win], writes=[r_p])
                            if kind == "z":
                                o, r_o = zs.nxt()
                                evac(o[:], pt[:], [r_p], [r_o])
                                dma("sp", z_d[t0:t0 + 128, :], o[:], reads=[r_o], writes=[r_z])
                            elif kind == "v":
                                o, r_o = vs.nxt()
                                evac(o[:], pt[:], [r_p], [r_o])
                                dma("sp", v_d[t0:t0 + 128, :], o[:], reads=[r_o], writes=[r_v])
                            else:
                                o, r_o = ds.nxt()
                                evac(o[:], pt[:, 0:16], [r_p], [r_o])
                                dma("sp", dt_d[t0:t0 + 128, :], o[:], reads=[r_o], writes=[r_dt])
                    for cc in range(16):
                        c0 = 512 + cc * 128 if cc < 8 else (1552 + (cc - 8) * 128)
                        pt, r_p = pB.nxt()
                        for k in range(8):
                            P.op("pe", lambda k=k, pt=pt, c0=c0, uTt=uTt: nc.tensor.matmul(
                                pt[:], lhsT=win[:, k, c0:c0 + 128], rhs=uTt[:, k, :],
                                start=(k == 0), stop=(k == 7)), reads=[r_uT, r_win], writes=[r_p])
                        if len(pend_bm) > 2:
                            emit_bm(*pend_bm.pop(0))
                        o, r_o = fs.nxt()
                        evac(o[:], pt[:], [r_p], [r_o])
                        if cc >= 8:
                            sqb, r_sqb = sqA.nxt()
                            P.op("pool", lambda sqb=sqb, o=o: nc.gpsimd.tensor_tensor(out=sqb[:], in0=o[:], in1=o[:], op=ALU.mult),
                                 reads=[r_o], writes=[r_sqb])
                            pend_bm.append((sqb, r_sqb, cc))
                        if cc < 8:
                            dma("sp", xbc_d[cc * 128:(cc + 1) * 128, 2 + tb * 512:2 + (tb + 1) * 512], o[:],
                                reads=[r_o], writes=[r_xbc])
                        elif cc < 12:
                            dma("sp", qT_d[(cc - 8) * 128:(cc - 7) * 128, tb * 512:(tb + 1) * 512], o[:],
                                reads=[r_o], writes=[r_q])
                        else:
                            dma("sp", kT_d[(cc - 12) * 128:(cc - 11) * 128, tb * 512:(tb + 1) * 512], o[:],
                                reads=[r_o], writes=[r_k])
                    if ubs_n is not None:
                        pendA = prepA2(ubs_n)
                while pend_bm:
                    emit_bm(*pend_bm.pop(0))
                dma("sp", nmax_d, nmax[:], reads=[r_nm])

            P.barrier()
            with ExitStack() as st:
                xs_tm = SB(st, "xs_tm", [128, NC, 512], BF16)
                B_tm = SB(st, "B_tm", [128, NC, 256], BF16)
                BT = SB(st, "BT", [128, 2, L], BF16)
                CT = SB(st, "CT", [128, 2, L], BF16)
                r_prep = R()
                cw = SB(st, "cw", [128, 8, 5]); cbias = SB(st, "cbias", [128, 8, 1]); r_cw = R()
                dma("sp", cw[:], convw.rearrange("p (cc k) -> p cc k", k=5), writes=[r_cw])
                dma("sp", cbias[:], convb.rearrange("p (cc k) -> p cc k", k=1), writes=[r_cw])
                dtr = SB(st, "dtr", [128, NC, 16]); r_dtr = R()
                dtt = SB(st, "dtt", [128, NC, 16])
                adt = SB(st, "adt", [128, NC, 16])
                cs = SB(st, "cs", [128, NC, 16])
                ecs = SB(st, "ecs", [128, NC, 16])
                dte = SB(st, "dte", [128, NC, 16])
                cd = SB(st, "cd", [128, NC, 16])
                tmp1 = SB(st, "tmp1", [128, NC, 16]); tmp2 = SB(st, "tmp2", [128, NC, 16])
                dtb_t = SB(st, "dtb_t", [128, 16]); a_t = SB(st, "a_t", [128, 16]); dsk_t = SB(st, "dsk_t", [128, 8])
                snw_t = SB(st, "snw_t", [128, 512])
                r_dtp = R()
                dma("sp", dtr[:], dt_d.rearrange("(c p) h -> p c h", p=128), reads=[r_dt], writes=[r_dtr])
                dma("sp", dtb_t[:], dtb.partition_broadcast(128), writes=[r_dtp])
                dma("sp", a_t[:], alog.partition_broadcast(128), writes=[r_dtp])
                dma("sp", dsk_t[:], dsk.partition_broadcast(128), writes=[r_dtp])
                dma("sp", snw_t[:], snw.partition_broadcast(128), writes=[r_dtp])
                bc16 = lambda t: t[:].unsqueeze(1).to_broadcast([128, NC, 16])
                P.op("act", lambda: nc.scalar.activation(out=a_t[:], in_=a_t[:], func=AF.Exp), reads=[r_dtp], writes=[r_dtp])
                P.op("dve", lambda: nc.vector.tensor_scalar(out=a_t[:], in0=a_t[:], scalar1=-1.0, scalar2=None, op0=ALU.mult),
                     reads=[r_dtp], writes=[r_dtp])
                P.op("dve", lambda: nc.vector.tensor_tensor(out=dtr[:], in0=dtr[:], in1=bc16(dtb_t), op=ALU.add),
                     reads=[r_dtr, r_dtp], writes=[r_dtr])
                P.op("dve", lambda: nc.vector.tensor_scalar(out=tmp1[:], in0=dtr[:], scalar1=30.0, scalar2=None, op0=ALU.min),
                     reads=[r_dtr], writes=[r_dtp])
                P.op("act", lambda: nc.scalar.activation(out=tmp1[:], in_=tmp1[:], func=AF.Exp),
                     reads=[r_dtp], writes=[r_dtp])
                P.op("dve", lambda: nc.vector.tensor_scalar(out=tmp1[:], in0=tmp1[:], scalar1=1.0, scalar2=None, op0=ALU.add),
                     reads=[r_dtp], writes=[r_dtp])
                P.op("act", lambda: nc.scalar.activation(out=tmp1[:], in_=tmp1[:], func=AF.Ln),
                     reads=[r_dtp], writes=[r_dtp])
                P.op("dve", lambda: nc.vector.tensor_tensor(out=dtt[:], in0=tmp1[:], in1=dtr[:], op=ALU.max),
                     reads=[r_dtp, r_dtr], writes=[r_dtp])
                P.op("dve", lambda: nc.vector.tensor_tensor(out=adt[:], in0=dtt[:], in1=bc16(a_t), op=ALU.mult),
                     reads=[r_dtp], writes=[r_dtp])
                stc = ExitStack()
                pC = MB(stc, "pC", [128, 512], F32, 2, psum=True)
                adt_f = adt[:].rearrange("p c h -> p (c h)")
                ncol = NC * 16
                for (lh, dst, lo, hi) in ((CF(CF_TF), cs, 0, 8), (CF(CF_TB), cs, 8, 16), (ones_f, tmp1, 0, 16)):
                    for c0 in range(0, ncol, 512):
                        c1 = min(ncol, c0 + 512)
                        pt, r_p = pC.nxt()
                        P.op("pe", lambda pt=pt, lh=lh, c0=c0, c1=c1: nc.tensor.matmul(
                            pt[:, 0:c1 - c0], lhsT=lh, rhs=adt_f[:, c0:c1], start=True, stop=True),
                            reads=[r_dtp, r_c], writes=[r_p])
                        ca, cb_ = c0 // 16, c1 // 16
                        evac(dst[:, ca:cb_, lo:hi], pt[:, 0:c1 - c0].rearrange("p (c h) -> p c h", h=16)[:, :, lo:hi],
                             [r_p], [r_dtp], eng="dve")
                P.op("act", lambda: nc.scalar.activation(out=ecs[:], in_=cs[:], func=AF.Exp), reads=[r_dtp], writes=[r_dtp])
                P.op("dve", lambda: nc.vector.tensor_tensor(out=tmp2[:], in0=tmp1[:], in1=cs[:], op=ALU.subtract),
                     reads=[r_dtp], writes=[r_dtp])
                P.op("act", lambda: nc.scalar.activation(out=dte[:], in_=tmp2[:], func=AF.Exp), reads=[r_dtp], writes=[r_dtp])
                P.op("act", lambda: nc.scalar.activation(out=cd[:], in_=tmp1[:], func=AF.Exp), reads=[r_dtp], writes=[r_dtp])

                with ExitStack() as st2:
                    raw = MB(st2, "raw", [128, L + 4], BF16, 2)
                    xsT = MB(st2, "xsT", [128, L], BF16, 2)
                    pTr = MB(st2, "pTr", [128, 4, 128], BF16, 2, psum=True)
                    pcv = MB(st2, "pcv", [128, 512], F32, 2, psum=True)
                    dgw = SB(st2, "dgw", [128, 8, 5, 128], BF16); r_dg = R()
                    for cc in range(8):
                        for k in range(5):
                            P.op("dve", lambda cc=cc, k=k: nc.vector.tensor_scalar(
                                out=dgw[:, cc, k, :], in0=ident_b, scalar1=cw[:, cc, k:k + 1], scalar2=None, op0=ALU.mult),
                                reads=[r_cw, r_c], writes=[r_dg])
                    for cc in range(8):
                        rw, r_rw = raw.nxt()
                        dma("sp", rw[:], xbc_d[cc * 128:(cc + 1) * 128, :], reads=[r_xbc], writes=[r_rw])
                        if cc < 4:
                            dst, r_d = xsT.nxt()
                            dstap = dst[:]
                        elif cc < 6:
                            dstap, r_d = BT[:, cc - 4, :], r_prep
                        else:
                            dstap, r_d = CT[:, cc - 6, :], r_prep
                        for tbk in range(L // 512):
                            o0 = tbk * 512
                            pc, r_pc = pcv.nxt()
                            for k in range(5):
                                P.op("pe", lambda pc=pc, rw=rw, cc=cc, k=k, o0=o0: nc.tensor.matmul(
                                    pc[:], lhsT=dgw[:, cc, k, :], rhs=rw[:, o0 + k:o0 + k + 512], start=(k == 0), stop=(k == 4)),
                                    reads=[r_rw, r_dg], writes=[r_pc])
                            P.op("act", lambda pc=pc, dstap=dstap, o0=o0, cc=cc: nc.scalar.activation(
                                out=dstap[:, o0:o0 + 512], in_=pc[:], func=AF.Silu, bias=cbias[:, cc, 0:1]),
                                reads=[r_pc, r_cw], writes=[r_d])
                        if cc < 6:
                            for c0 in range(0, NC, 4):
                                pt, r_p = pTr.nxt()
                                for i in range(4):
                                    P.op("pe", lambda pt=pt, i=i, c0=c0, dstap=dstap: nc.tensor.transpose(
                                        out=pt[:, i, :], in_=dstap[:, (c0 + i) * 128:(c0 + i + 1) * 128],
                                        identity=ident_b), reads=[r_d, r_c], writes=[r_p])
                                if cc < 4:
                                    evac(xs_tm[:, c0:c0 + 4, cc * 128:(cc + 1) * 128], pt[:], [r_p], [r_prep])
                                else:
                                    evac(B_tm[:, c0:c0 + 4, (cc - 4) * 128:(cc - 3) * 128], pt[:], [r_p], [r_prep])

                stc.close()
                P.barrier()
                with ExitStack() as st2:
                    Hb_all = SB(st2, "Hb_all", [128, NC, 2, 256], BF16); r_Hb = R()
                    Hf = [[SB(st2, "Hf%d%d" % (d_, g), [128, 256]) for g in range(2)] for d_ in range(2)]
                    r_H = [[R(), R()], [R(), R()]]
                    Hbf = MB(st2, "Hbf", [128, 256], BF16, 2)
                    Hbf_g = [None, None]
                    xdt = MB(st2, "xdt", [128, 512], BF16, 3)
                    xdte = MB(st2, "xdte", [128, 512], BF16, 3)
                    RH = MB(st2, "RH", [128, 4, 128], F32, 2)
                    Eb = MB(st2, "Eb", [128, 4, 128], BF16, 2)
                    MT = MB(st2, "MT", [128, 4, 128], BF16, 2)
                    cbm = MB(st2, "cbm", [128, 128], BF16, 4)
                    yacc = MB(st2, "yacc", [128, 512], F32, 2)
                    ytmp = MB(st2, "ytmp", [128, 256], F32, 2)
                    zt = MB(st2, "zt", [128, 512], F32, 3)
                    gsc = SB(st2, "gsc", [128, 512]); r_gsc = R()
                    gst = MB(st2, "gst", [128, 2], F32, 2)
                    yo = MB(st2, "yo", [128, 512], BF16, 2)
                    pS = MB(st2, "pS", [128, 256], F32, 2, psum=True)
                    pCB = MB(st2, "pCB", [128, 128], F32, 1, psum=True)
                    pSeg = MB(st2, "pSeg", [128, 512], F32, 2, psum=True)
                    pY = MB(st2, "pY", [128, 256], F32, 2, psum=True)
                    pYo = MB(st2, "pYo", [128, 256], F32, 1, psum=True)
                    for d_ in range(2):
                        for g in range(2):
                            P.op("pool", lambda d_=d_, g=g: nc.gpsimd.memset(Hf[d_][g][:], 0.0), writes=[r_H[d_][g]])

                    def xdt_ops(c, d_):
                        a, r_a = xdt.nxt()
                        b, r_b = xdte.nxt()
                        h0 = 8 * d_
                        P.op("pool", lambda: nc.gpsimd.tensor_tensor(
                            out=a[:].rearrange("p (h e) -> p h e", h=8),
                            in0=xs_tm[:, c, :].rearrange("p (h e) -> p h e", h=8),
                            in1=dtt[:, c, h0:h0 + 8].unsqueeze(2).to_broadcast([128, 8, 64]), op=ALU.mult),
                            reads=[r_prep, r_dtp], writes=[r_a])
                        P.op("pool", lambda: nc.gpsimd.tensor_tensor(
                            out=b[:].rearrange("p (h e) -> p h e", h=8),
                            in0=a[:].rearrange("p (h e) -> p h e", h=8),
                            in1=dte[:, c, h0:h0 + 8].unsqueeze(2).to_broadcast([128, 8, 64]), op=ALU.mult),
                            reads=[r_a, r_dtp], writes=[r_b])
                        return (a, r_a), (b, r_b)

                    def state_update(c, d_, g, xe, r_xe):
                        pt, r_p = pS.nxt()
                        P.op("pe", lambda: nc.tensor.matmul(pt[:], lhsT=B_tm[:, c, g * 128:(g + 1) * 128],
                                                            rhs=xe[:, g * 256:(g + 1) * 256], start=True, stop=True),
                             reads=[r_prep, r_xe], writes=[r_p])
                        h0 = 8 * d_ + 4 * g
                        H = Hf[d_][g]
                        P.op("dve", lambda: nc.vector.tensor_tensor(
                            out=H[:].rearrange("p (h e) -> p h e", h=4), in0=H[:].rearrange("p (h e) -> p h e", h=4),
                            in1=cd[:, c, h0:h0 + 4].unsqueeze(2).to_broadcast([128, 4, 64]), op=ALU.mult),
                            reads=[r_H[d_][g], r_dtp], writes=[r_H[d_][g]])
                        P.op("dve", lambda: nc.vector.tensor_tensor(out=H[:], in0=H[:], in1=pt[:], op=ALU.add),
                             reads=[r_H[d_][g], r_p], writes=[r_H[d_][g]])

                    for c in range(NC - 1, -1, -1):
                        for g in range(2):
                            P.op("act", lambda c=c, g=g: nc.scalar.copy(out=Hb_all[:, c, g, :], in_=Hf[1][g][:]),
                                 reads=[r_H[1][g]], writes=[r_Hb])
                        if c > 0:
                            (_, _), (xe, r_xe) = xdt_ops(c, 1)
                            for g in range(2):
                                state_update(c, 1, g, xe, r_xe)

                    zq = []

                    def load_z(c):
                        z, r_zt = zt.nxt()
                        dma("sp", z[:], z_d[c * 128:(c + 1) * 128, :], reads=[r_z], writes=[r_zt])
                        zq.append((z, r_zt))
                    load_z(0)
                    for c in range(NC):
                        if c + 1 < NC:
                            load_z(c + 1)
                        ya, r_ya = yacc.nxt()
                        P.op("dve", lambda ya=ya, c=c: nc.vector.tensor_tensor(
                            out=ya[:].rearrange("p (h e) -> p h e", h=8),
                            in0=xs_tm[:, c, :].rearrange("p (h e) -> p h e", h=8),
                            in1=dsk_t[:].unsqueeze(2).to_broadcast([128, 8, 64]), op=ALU.mult),
                            reads=[r_prep, r_dtp], writes=[r_ya])
                        xd = [None, None]
                        for d_ in range(2):
                            xd[d_] = xdt_ops(c, d_)
                        def run_combos(c, ya, r_ya, xd):
                            pre = {}

                            def preG(g):
                                pcb, r_pcb = pCB.nxt()
                                P.op("pe", lambda: nc.tensor.matmul(
                                    pcb[:], lhsT=BT[:, g, c * 128:(c + 1) * 128], rhs=CT[:, g, c * 128:(c + 1) * 128],
                                    start=True, stop=True), reads=[r_prep], writes=[r_pcb])
                                cms = []
                                for d_ in range(2):
                                    cm, r_cm = cbm.nxt()
                                    P.op("dve", lambda cm=cm, d_=d_: nc.vector.tensor_tensor(
                                        out=cm[:], in0=pcb[:], in1=CF(CF_MF + d_), op=ALU.mult),
                                        reads=[r_pcb, r_c], writes=[r_cm])
                                    cms.append((cm, r_cm))
                                hb, r_hb = Hbf.nxt()
                                P.op("act", lambda: nc.scalar.copy(out=hb[:], in_=Hf[0][g][:]),
                                     reads=[r_H[0][g]], writes=[r_hb])
                                pre[g] = (cms, hb, r_hb)

                            cst = {}

                            def stX(g, d_):
                                h0 = 8 * d_ + 4 * g
                                rh, r_rh = RH.nxt()
                                P.op("pool", lambda: nc.gpsimd.tensor_tensor(
                                    out=rh[:], in0=CF(CF_TF + d_).unsqueeze(1).to_broadcast([128, 4, 128]),
                                    in1=adt[:, c, h0:h0 + 4].unsqueeze(2).to_broadcast([128, 4, 128]), op=ALU.mult),
                                    reads=[r_c, r_dtp], writes=[r_rh])
                                cst[(g, d_)] = dict(rh=(rh, r_rh))

                            def stY(g, d_):
                                rh, r_rh = cst[(g, d_)]["rh"]
                                psg, r_psg = pSeg.nxt()
                                P.op("pe", lambda: nc.tensor.matmul(
                                    psg[:], lhsT=CF(CF_UF + d_), rhs=rh[:].rearrange("p h l -> p (h l)"),
                                    start=True, stop=True), reads=[r_rh, r_c], writes=[r_psg])
                                eb, r_eb = Eb.nxt()
                                P.op("act", lambda: nc.scalar.activation(
                                    out=eb[:].rearrange("p h l -> p (h l)"), in_=psg[:], func=AF.Exp),
                                    reads=[r_psg], writes=[r_eb])
                                mt, r_mt = MT.nxt()
                                cm, r_cm = pre[g][0][d_]
                                P.op("dve", lambda: nc.vector.tensor_tensor(
                                    out=mt[:], in0=eb[:], in1=cm[:].unsqueeze(1).to_broadcast([128, 4, 128]),
                                    op=ALU.mult), reads=[r_eb, r_cm], writes=[r_mt])
                                cst[(g, d_)]["mt"] = (mt, r_mt)

                            def stZ(g, d_):
                                h0 = 8 * d_ + 4 * g
                                mt, r_mt = cst[(g, d_)]["mt"]
                                (xa, r_xa), (xe, r_xe) = xd[d_]
                                _, hb, r_hb = pre[g]
                                py, r_py = pY.nxt()
                                for r_ in range(4):
                                    hh = 4 * g + r_
                                    P.op("pe", lambda r_=r_, hh=hh: nc.tensor.matmul(
                                        py[:, r_ * 64:(r_ + 1) * 64], lhsT=mt[:, r_, :], rhs=xa[:, hh * 64:(hh + 1) * 64],
                                        start=True, stop=True), reads=[r_mt, r_xa], writes=[r_py])
                                pyo, r_pyo = pYo.nxt()
                                if d_ == 0:
                                    rhs_h, r_rhs = hb[:], r_hb
                                else:
                                    rhs_h, r_rhs = Hb_all[:, c, g, :], r_Hb
                                P.op("pe", lambda: nc.tensor.matmul(
                                    pyo[:], lhsT=CT[:, g, c * 128:(c + 1) * 128], rhs=rhs_h, start=True, stop=True),
                                    reads=[r_prep, r_rhs], writes=[r_pyo])
                                yt, r_yt = ytmp.nxt()
                                P.op("dve", lambda: nc.vector.tensor_tensor(
                                    out=yt[:].rearrange("p (h e) -> p h e", h=4),
                                    in0=pyo[:].rearrange("p (h e) -> p h e", h=4),
                                    in1=ecs[:, c, h0:h0 + 4].unsqueeze(2).to_broadcast([128, 4, 64]), op=ALU.mult),
                                    reads=[r_pyo, r_dtp], writes=[r_yt])
                                yg = ya[:, g * 256:(g + 1) * 256]
                                P.op("dve", lambda: nc.vector.tensor_tensor(out=yg, in0=yg, in1=yt[:], op=ALU.add),
                                     reads=[r_yt, r_ya], writes=[r_ya])
                                P.op("dve", lambda: nc.vector.tensor_tensor(out=yg, in0=yg, in1=py[:], op=ALU.add),
                                     reads=[r_py, r_ya], writes=[r_ya])

                            preG(0)
                            preG(1)
                            K4 = [(0, 0), (0, 1), (1, 0), (1, 1)]
                            stX(*K4[0]); stX(*K4[1]); stY(*K4[0]); stX(*K4[2]); stY(*K4[1]); stZ(*K4[0])
                            stX(*K4[3]); stY(*K4[2]); stZ(*K4[1]); stY(*K4[3]); stZ(*K4[2]); stZ(*K4[3])
                            if c < NC - 1:
                                for g in range(2):
                                    state_update(c, 0, g, xd[0][1][0], xd[0][1][1])

                        run_combos(c, ya, r_ya, xd)
                        z, r_zt = zq.pop(0)
                        P.op("act", lambda z=z: nc.scalar.activation(out=z[:], in_=z[:], func=AF.Silu), reads=[r_zt], writes=[r_zt])
                        P.op("dve", lambda ya=ya, z=z: nc.vector.tensor_tensor(out=ya[:], in0=ya[:], in1=z[:], op=ALU.mult),
                             reads=[r_zt, r_ya], writes=[r_ya])
                        gs, r_gs = gst.nxt()
                        rms_rstd("g", ya[:], gs[:, 0:1], gs[:, 1:2], gsc[:], r_ya, r_gs, 512)
                        yob, r_yo = yo.nxt()
                        P.op("dve", lambda yob=yob, ya=ya, gs=gs: nc.vector.scalar_tensor_tensor(
                            out=yob[:], in0=ya[:], scalar=gs[:, 1:2], in1=snw_t[:], op0=ALU.mult, op1=ALU.mult),
                            reads=[r_ya, r_gs, r_dtp], writes=[r_yo])
                        dma("sp", mix_d[c * 128:(c + 1) * 128, 0:512], yob[:], reads=[r_yo], writes=[r_mix])

            P.barrier()
            with ExitStack() as st:
                QA = [MB(st, "QA%d" % c_, [67, L], BF16, 2) for c_ in range(2)]
                KAa = [MB(st, "KAa%d" % c_, [67, L], BF16, 2) for c_ in range(2)]
                KAb = [MB(st, "KAb%d" % c_, [67, L], BF16, 2) for c_ in range(2)]
                VA = MB(st, "VA", [128, NC, 129], BF16, 2)
                btab_t = MB(st, "btab_t", [128, NC * NQB], F32, 2)
                bias_t = [MB(st, "bias_t%d" % c_, [128, NC * NQB], F32, 2) for c_ in range(2)]
                lam_t = SB(st, "lam_t", [128, 4, 64]); r_lam = R()
                lam_s = SB(st, "lam_s", [128, 4])
                subw_t = SB(st, "subw_t", [128, 128])
                nrmb = MB(st, "nrmb", [128, 2, 16], F32, 2)
                nrm = SB(st, "nrm", [128, 8]); r_nrm = R()
                Mbc = MB(st, "Mbc", [128, 2], F32, 2)
                mdiag = SB(st, "mdiag", [128, 2])
                PT = [MB(st, "PT%d" % c_, [128, 512], BF16, 3) for c_ in range(2)]
                osq2 = MB(st, "osq2", [128, 128], F32, 2)
                Sfix = MB(st, "Sfix", [128, 512], F32, 2)
                pSc = [MB(st, "pSc%d" % c_, [128, 512], F32, 2, psum=True) for c_ in range(2)]
                pO = MB(st, "pO", [128, 2, 256], F32, 4, psum=True)
                osb = MB(st, "osb", [128, 2, 129], F32, 5)
                ot = MB(st, "ot", [128, 128], F32, 2)
                ost = MB(st, "ost", [128, 4], F32, 8)
                osq = SB(st, "osq", [128, 128]); r_osq = R()
                ob = MB(st, "ob", [128, 128], BF16, 5)
                zf_list = list(range(0, na, 2)) if s == 0 else []
                zf_per = -(-len(zf_list) // (4 * NQB)) if zf_list else 0
                dma("sp", lam_t[:], lam.rearrange("a d -> (a d)").partition_broadcast(128), writes=[r_lam])
                dma("sp", subw_t[:], sub_w.partition_broadcast(128), writes=[r_lam])
                P.op("dve", lambda: nc.vector.tensor_tensor(out=lam_t[:, 0:2, :], in0=lam_t[:, 0:2, :], in1=lam_t[:, 2:4, :], op=ALU.mult),
                     reads=[r_lam], writes=[r_lam])
                P.op("dve", lambda: nc.vector.reduce_sum(out=lam_s[:, 0:2], in_=lam_t[:, 0:2, :], axis=AX.X), reads=[r_lam], writes=[r_lam])
                P.op("act", lambda: nc.scalar.activation(out=lam_s[:, 0:2], in_=lam_s[:, 0:2], func=AF.Exp), reads=[r_lam], writes=[r_lam])
                P.op("dve", lambda: nc.vector.tensor_tensor(out=lam_s[:, 2:3], in0=lam_s[:, 1:2], in1=lam_s[:, 0:1], op=ALU.subtract),
                     reads=[r_lam], writes=[r_lam])
                P.op("dve", lambda: nc.vector.tensor_scalar(out=lam_s[:, 2:3], in0=lam_s[:, 2:3], scalar1=-LAMBDA_INIT, scalar2=None, op0=ALU.add),
                     reads=[r_lam], writes=[r_lam])
                P.op("dve", lambda: nc.vector.tensor_scalar(out=subw_t[:], in0=subw_t[:], scalar1=1.0 - LAMBDA_INIT, scalar2=None, op0=ALU.mult),
                     reads=[r_lam], writes=[r_lam])
                pN = pSc[0]
                hctx = {}

                def setup_loads(h):
                    qa, ka, kb = [], [], []
                    for c_ in range(2):
                        t_, r_ = QA[c_].nxt(); qa.append((t_, r_))
                        row0 = h * 128 + c_ * 64
                        dma("sp", t_[0:64, :], qT_d[row0:row0 + 64, :], reads=[r_q], writes=[r_])
                        dma("sp", t_[64:67, :], qrows_d[h], writes=[r_])
                        t_, r_ = KAa[c_].nxt(); ka.append((t_, r_))
                        dma("sp", t_[0:64, :], kT_d[row0:row0 + 64, :], reads=[r_k], writes=[r_])
                        dma("sp", t_[64:67, :], krA_d[h], writes=[r_])
                        t_, r_ = KAb[c_].nxt(); kb.append((t_, r_))
                        dma("sp", t_[0:64, :], kT_d[row0:row0 + 64, :], reads=[r_k], writes=[r_])
                        dma("sp", t_[64:67, :], krB_d[h], writes=[r_])
                    va, r_va = VA.nxt()
                    dma("sp", va[:, :, 0:128], v_d[:, h * 128:(h + 1) * 128].rearrange("(c p) e -> p c e", p=128),
                        reads=[r_v], writes=[r_va])
                    P.op("pool", lambda va=va: nc.gpsimd.memset(va[:, :, 128:129], 1.0), writes=[r_va])
                    bt, r_bt = btab_t.nxt()
                    dma("sp", bt[:], btab_d[h:h + 1, :].partition_broadcast(128), writes=[r_bt])
                    hpre[h] = dict(qa=qa, ka=ka, kb=kb, va=(va, r_va), bt=(bt, r_bt))

                def setup_final(h):
                    pr = hpre.pop(h)
                    qa, ka, kb, bt, r_bt = pr["qa"], pr["ka"], pr["kb"], pr["bt"][0], pr["bt"][1]
                    nr, r_nr = nrmb.nxt()
                    dma("sp", nr[:, 0, :], nmax_d[0:1, :].partition_broadcast(128), writes=[r_nr])
                    dma("sp", nr[:, 1, :], nmax_d[64:65, :].partition_broadcast(128), writes=[r_nr])
                    mb, r_mb = Mbc.nxt()
                    P.op("dve", lambda: nc.vector.tensor_tensor(out=mb[:], in0=nr[:, :, h], in1=nr[:, :, 4 + h], op=ALU.mult),
                         reads=[r_nr], writes=[r_mb])
                    P.op("pool", lambda: nc.gpsimd.tensor_tensor(out=mb[:], in0=mb[:], in1=poshalf[:, 0:1].to_broadcast([128, 2]), op=ALU.pow),
                         reads=[r_mb, r_c], writes=[r_mb])
                    P.op("dve", lambda: nc.vector.tensor_scalar(out=mb[:], in0=mb[:], scalar1=-0.125 * 1.02, scalar2=None, op0=ALU.mult),
                         reads=[r_mb], writes=[r_mb])
                    bi = []
                    for c_ in range(2):
                        b_, r_b = bias_t[c_].nxt()
                        P.op("dve", lambda b_=b_, c_=c_: nc.vector.tensor_scalar(
                            out=b_[:], in0=bt[:], scalar1=mb[:, c_:c_ + 1], scalar2=None, op0=ALU.add),
                            reads=[r_bt, r_mb], writes=[r_b])
                        bi.append((b_, r_b))
                    hctx[h] = dict(qa=qa, ka=ka, kb=kb, va=pr["va"], bi=bi, s8=8.0 * (2.0 ** (-8.0 * (h + 1) / 4)))

                hpre = {}
                def kt_order(qb):
                    dg = [kt for kt in range(NC) if 4 * qb <= kt < 4 * qb + 4]
                    return [kt for kt in range(NC) if kt not in dg] + dg
                steps = []
                for h in range(4):
                    for qb in range(NQB):
                        od = kt_order(qb)
                        for pos, kt in enumerate(od):
                            for c_ in range(2):
                                steps.append(dict(h=h, qb=qb, kt=kt, c=c_, first=(pos == 0), last=(pos == NC - 1)))
                qctx = {}

                def emit_qk(sp_):
                    h, qb, kt, c_ = sp_["h"], sp_["qb"], sp_["kt"], sp_["c"]
                    cx = hctx[h]
                    caseB = kt >= 4 * qb + 4
                    diag = (4 * qb <= kt < 4 * qb + 4)
                    kop, r_kop = (cx["kb"] if caseB else cx["ka"])[c_]
                    qop, r_qop = cx["qa"][c_]
                    ps_, r_ps = pSc[c_].nxt()
                    P.op("pe", lambda: nc.tensor.matmul(
                        ps_[:], lhsT=kop[:, kt * 128:(kt + 1) * 128], rhs=qop[:, qb * 512:(qb + 1) * 512],
                        start=True, stop=True), reads=[r_kop, r_qop], writes=[r_ps])
                    src_ap, r_src = ps_[:], r_ps
                    if diag:
                        sf, r_sf = Sfix.nxt()
                        dk = kt - 4 * qb
                        s8 = cx["s8"]
                        P.op("dve", lambda: nc.vector.scalar_tensor_tensor(
                            out=sf[:], in0=CF(CF_D2 + 4 * dk, 4), scalar=-s8, in1=ps_[:], op0=ALU.mult, op1=ALU.add),
                            reads=[r_ps, r_c], writes=[r_sf])
                        src_ap, r_src = sf[:], r_sf
                    sp_["src"] = (src_ap, r_src)

                def emit_exp(sp_):
                    h, qb, kt, c_ = sp_["h"], sp_["qb"], sp_["kt"], sp_["c"]
                    src_ap, r_src = sp_["src"]
                    pt_, r_pt = PT[c_].nxt()
                    b_, r_b = hctx[h]["bi"][c_]
                    col = kt * NQB + qb
                    P.op("act", lambda: nc.scalar.activation(
                        out=pt_[:], in_=src_ap, func=AF.Exp, bias=b_[:, col:col + 1], scale=0.125),
                        reads=[r_src, r_b], writes=[r_pt])
                    sp_["pt"] = (pt_, r_pt)

                def emit_pv(sp_):
                    h, qb, kt, c_ = sp_["h"], sp_["qb"], sp_["kt"], sp_["c"]
                    if sp_["first"] and c_ == 0:
                        qctx[(h, qb)] = [pO.nxt() for _ in range(4)]
                    po = qctx[(h, qb)]
                    pt_, r_pt = sp_["pt"]
                    va, r_va = hctx[h]["va"]
                    for sub in range(4):
                        P.op("pe", lambda sub=sub: nc.tensor.matmul(
                            po[sub][0][:, c_, 0:129], lhsT=pt_[:, sub * 128:(sub + 1) * 128], rhs=va[:, kt, :],
                            start=(sp_["first"] and c_ == 0), stop=(sp_["last"] and c_ == 1), skip_group_check=True),
                            reads=[r_pt, r_va], writes=[po[sub][1]])

                def emit_epilogue(h, qb):
                    po = qctx.pop((h, qb))
                    for _ in range(zf_per):
                        if zf_list:
                            a0 = zf_list.pop(0)
                            dma("sp", xs_v[:, a0:a0 + 2, :], zero_bf[:].rearrange("p (a d) -> p a d", a=2), reads=[r_c])
                    os_l = []
                    for sub in range(4):
                        pot, r_po = po[sub]
                        o_, r_o = osb.nxt()
                        evac(o_[:], pot[:, :, 0:129], [r_po], [r_o], eng="dve")
                        os_l.append((o_, r_o))
                    for sub in range(4):
                        o_, r_o = os_l[sub]
                        t0 = qb * 512 + sub * 128
                        os_, r_os = ost.nxt()
                        P.op("dve", lambda o_=o_, os_=os_: nc.vector.reciprocal(out=os_[:, 0:2], in_=o_[:, :, 128]),
                             reads=[r_o], writes=[r_os])
                        P.op("dve", lambda os_=os_: nc.vector.tensor_tensor(out=os_[:, 1:2], in0=os_[:, 1:2], in1=lam_s[:, 2:3], op=ALU.mult),
                             reads=[r_os, r_lam], writes=[r_os])
                        oo, r_oo = ot.nxt()
                        P.op("dve", lambda oo=oo, o_=o_, os_=os_: nc.vector.tensor_scalar(
                            out=oo[:], in0=o_[:, 0, 0:128], scalar1=os_[:, 0:1], scalar2=None, op0=ALU.mult),
                            reads=[r_o, r_os], writes=[r_oo])
                        P.op("dve", lambda oo=oo, o_=o_, os_=os_: nc.vector.scalar_tensor_tensor(
                            out=oo[:], in0=o_[:, 1, 0:128], scalar=os_[:, 1:2], in1=oo[:], op0=ALU.mult, op1=ALU.add),
                            reads=[r_o, r_os, r_oo], writes=[r_oo])
                        oq, r_oq = osq2.nxt()
                        P.op("dve", lambda oo=oo, oq=oq: nc.vector.tensor_tensor(out=oq[:], in0=oo[:], in1=oo[:], op=ALU.mult),
                             reads=[r_oo], writes=[r_oq])
                        P.op("dve", lambda oq=oq, os_=os_: nc.vector.reduce_sum(out=os_[:, 2:3], in_=oq[:], axis=AX.X),
                             reads=[r_oq], writes=[r_os])
                        P.op("dve", lambda os_=os_: nc.vector.tensor_scalar(out=os_[:, 3:4], in0=os_[:, 2:3], scalar1=1.0 / 128, scalar2=EPS,
                                                                         op0=ALU.mult, op1=ALU.add), reads=[r_os], writes=[r_os])
                        P.op("pool", lambda os_=os_: nc.gpsimd.tensor_tensor(out=os_[:, 3:4], in0=os_[:, 3:4], in1=neghalf[:, 0:1], op=ALU.pow),
                             reads=[r_os, r_c], writes=[r_os])
                        ob_, r_ob = ob.nxt()
                        P.op("dve", lambda ob_=ob_, oo=oo, os_=os_: nc.vector.scalar_tensor_tensor(
                            out=ob_[:], in0=oo[:], scalar=os_[:, 3:4], in1=subw_t[:], op0=ALU.mult, op1=ALU.mult),
                            reads=[r_oo, r_os, r_lam], writes=[r_ob])
                        dma("pool", mix_d[t0:t0 + 128, 512 + h * 128:512 + (h + 1) * 128], ob_[:], reads=[r_ob], writes=[r_mix])

                setup_loads(0)
                setup_final(0)
                AHEAD, LAG = 2, 2
                nst = len(steps)
                per_head = NQB * NC * 2
                pend_parts = []
                for i in range(min(AHEAD, nst)):
                    emit_qk(steps[i])
                for i in range(nst + LAG):
                    if i < nst:
                        sp_ = steps[i]
                        hh = sp_["h"]
                        rel_i = i - hh * per_head
                        if hh + 1 < 4:
                            if rel_i == 2:
                                setup_loads(hh + 1)
                            if rel_i == per_head // 2:
                                setup_final(hh + 1)
                        emit_exp(sp_)
                        if i + AHEAD < nst:
                            emit_qk(steps[i + AHEAD])
                    j = i - LAG
                    if j >= 0:
                        sj = steps[j]
                        emit_pv(sj)
                        if sj["last"] and sj["c"] == 1:
                            emit_epilogue(sj["h"], sj["qb"])

            P.barrier()
            with ExitStack() as st:
                wo = SB(st, "wo", [128, 8, D], BF16); r_wo = R()
                wov = w_out.rearrange("(kc p) f -> p kc f", p=128)
                r_woparts = []
                for c0 in range(0, D, 256):
                    r_woparts.append(R())
                    dma("pool", wo[:, :, c0:c0 + 256], wov[:, :, c0:c0 + 256], writes=[r_woparts[-1]])
                wr_t = SB(st, "wr_t", [128, 8, 36]); br_t = SB(st, "br_t", [128, 36])
                nfw_t = SB(st, "nfw_t", [128, D])
                r_woparts.append(R())
                dma("sp", nfw_t[:], nfw.partition_broadcast(128), writes=[r_woparts[-1]])
                r_woparts.append(R())
                dma("sp", wr_t[:], wr.rearrange("(kc p) f -> p kc f", p=128), writes=[r_woparts[-1]])
                r_woparts.append(R())
                dma("sp", br_t[:], br.partition_broadcast(128), writes=[r_woparts[-1]])
                jn2 = SB(st, "jn2", [128, 2])
                P.op("pool", lambda: nc.gpsimd.memset(jn2[:], 0.0), reads=r_woparts, writes=[r_wo])
                mx = MB(st, "mx", [128, D], BF16, 4)
                mxT = MB(st, "mxT", [128, 8, 128], BF16, 2)
                xt = MB(st, "xt2", [128, D], F32, 4)
                ht = MB(st, "ht", [128, D], F32, 2)
                sq = SB(st, "sq2", [128, D]); r_sq = R()
                stat = MB(st, "stat2", [128, 2], F32, 2)
                xn = MB(st, "xn", [128, D], F32, 3)
                xnb = MB(st, "xnb", [128, D], BF16, 2)
                xnT = MB(st, "xnT", [128, 8, 128], F32, 3)
                pT = MB(st, "pT2", [128, 8, 128], BF16, 1, psum=True)
                pH = MB(st, "pH", [128, 512], F32, 2, psum=True)
                pTf = MB(st, "pTf", [128, 4, 128], F32, 2, psum=True)
                pL = MB(st, "pL", [128, 64], F32, 2, psum=True)
                lg_all = SB(st, "lg_all", [128, NC, 36])
                r_lg = R()

                ldq = []

                def loadD(c):
                    t0 = c * 128
                    m_, r_m = mx.nxt()
                    dma("sp", m_[:], mix_d[t0:t0 + 128, :], reads=[r_mix], writes=[r_m])
                    x_, r_x = xt.nxt()
                    dma("sp", x_[:], x[s, t0:t0 + 128, :], writes=[r_x])
                    ldq.append((m_, r_m, x_, r_x))

                def stageA1(c):
                    ti = s * NC + c
                    if c + 2 < NC:
                        loadD(c + 2)
                    m_, r_m, x_, r_x = ldq.pop(0)
                    if dbg:
                        dma("pool", dbg_d["dbg_mix"][ti * 128:(ti + 1) * 128, :], m_[:], reads=[r_m])
                    pt, r_p = pT.nxt()
                    for k in range(8):
                        P.op("pe", lambda k=k, pt=pt, m_=m_: nc.tensor.transpose(
                            out=pt[:, k, :], in_=m_[:, k * 128:(k + 1) * 128], identity=ident_b), reads=[r_m, r_c], writes=[r_p])
                    mt_, r_mt = mxT.nxt()
                    evac(mt_[:], pt[:], [r_p], [r_mt])
                    return mt_, r_mt, x_, r_x

                def stageA2(c, mt_, r_mt, x_, r_x):
                    ti = s * NC + c
                    h_, r_ht = ht.nxt()
                    for half in range(2):
                        ph, r_ph = pH.nxt()
                        for k in range(8):
                            P.op("pe", lambda k=k, ph=ph, half=half: nc.tensor.matmul(
                                ph[:], lhsT=mt_[:, k, :], rhs=wo[:, k, half * 512:(half + 1) * 512], start=(k == 0), stop=(k == 7)),
                                reads=[r_mt, r_wo], writes=[r_ph])
                        P.op("dve", lambda h_=h_, ph=ph, half=half: nc.vector.tensor_tensor(
                            out=h_[:, half * 512:(half + 1) * 512], in0=x_[:, half * 512:(half + 1) * 512], in1=ph[:], op=ALU.add),
                            reads=[r_x, r_ph], writes=[r_ht])
                    dma("sp", h_d[ti * 128:(ti + 1) * 128, :], h_[:], reads=[r_ht], writes=[r_h])
                    if dbg:
                        dma("sp", dbg_d["dbg_h"][ti * 128:(ti + 1) * 128, :], h_[:], reads=[r_ht])
                    st_, r_st = stat.nxt()
                    rms_rstd("f", h_[:], st_[:, 0:1], st_[:, 1:2], sq[:], r_ht, r_st, D)
                    xn_, r_xn_ = xn.nxt()
                    P.op("dve", lambda: nc.vector.scalar_tensor_tensor(
                        out=xn_[:], in0=h_[:], scalar=st_[:, 1:2], in1=nfw_t[:], op0=ALU.mult, op1=ALU.mult),
                        reads=[r_ht, r_st, r_wo], writes=[r_xn_])
                    xb_, r_xb = xnb.nxt()
                    P.op("act", lambda: nc.scalar.copy(out=xb_[:], in_=xn_[:]), reads=[r_xn_], writes=[r_xb])
                    dma("sp", xn_d[ti * 128:(ti + 1) * 128, :], xb_[:], reads=[r_xb], writes=[r_xn])
                    return xn_, r_xn_

                def stageB1(c, xn_, r_xn_):
                    xT_, r_xT = xnT.nxt()
                    for k0 in range(0, 8, 4):
                        ptf, r_ptf = pTf.nxt()
                        for k in range(4):
                            P.op("pe", lambda k=k, k0=k0, ptf=ptf: nc.tensor.transpose(
                                out=ptf[:, k, :], in_=xn_[:, (k0 + k) * 128:(k0 + k + 1) * 128], identity=ident_f),
                                reads=[r_xn_, r_c], writes=[r_ptf])
                        evac(xT_[:, k0:k0 + 4, :], ptf[:], [r_ptf], [r_xT])
                    return xT_, r_xT

                def stageB2(c, xT_, r_xT):
                    pl, r_pl = pL.nxt()
                    for k in range(8):
                        P.op("pe", lambda k=k: nc.tensor.matmul(
                            pl[:, 0:36], lhsT=xT_[:, k, :], rhs=wr_t[:, k, :], start=(k == 0), stop=(k == 7)),
                            reads=[r_xT, r_wo], writes=[r_pl])
                    P.op("dve", lambda: nc.vector.tensor_tensor(out=lg_all[:, c, :], in0=pl[:, 0:36], in1=br_t[:], op=ALU.add),
                         reads=[r_pl, r_wo], writes=[r_lg])

                loadD(0)
                if NC > 1:
                    loadD(1)
                pend = stageA2(0, *stageA1(0))
                for c in range(NC):
                    a1 = stageA1(c + 1) if c + 1 < NC else None
                    b1 = stageB1(c, *pend)
                    nxt_ = stageA2(c + 1, *a1) if a1 is not None else None
                    stageB2(c, *b1)
                    pend = nxt_

                def route_batch(T0, lg_all, st, r_lg):
                    V = nc.vector
                    RW = [r_lg, r_route]

                    def dv(f):
                        P.op("dve", f, reads=RW, writes=RW)
                    q8 = SB(st, "q8", [128, 8, NC])
                    g4 = SB(st, "g4", [128, NC, 4])
                    me = SB(st, "me", [128, NC, NE])
                    lgG = lg_all[:, :, 0:4]
                    lgE = lg_all[:, :, 4:36]
                    b4 = lambda ap: ap.unsqueeze(2).to_broadcast([128, NC, 4])
                    b32 = lambda ap: ap.unsqueeze(2).to_broadcast([128, NC, NE])
                    o1 = oh1_all[:, T0:T0 + NC, :]
                    o2 = oh2_all[:, T0:T0 + NC, :]
                    dv(lambda: V.reduce_max(out=q8[:, 0, :], in_=lgG, axis=AX.X))
                    dv(lambda: V.tensor_tensor(out=g4[:], in0=lgG, in1=b4(q8[:, 0, :]), op=ALU.subtract))
                    P.op("act", lambda: nc.scalar.activation(out=g4[:], in_=g4[:], func=AF.Exp), reads=RW, writes=RW)
                    dv(lambda: V.reduce_sum(out=q8[:, 1, :], in_=g4[:], axis=AX.X))
                    dv(lambda: V.reciprocal(out=q8[:, 2, :], in_=q8[:, 1, :]))
                    dv(lambda: V.tensor_tensor(out=g4[:], in0=lgG, in1=b4(q8[:, 0, :]), op=ALU.is_equal))
                    dv(lambda: V.tensor_scalar(out=g4[:], in0=g4[:], scalar1=-1.0, scalar2=NEGBIG, op0=ALU.add, op1=ALU.mult))
                    for g in range(4):
                        dv(lambda g=g: V.tensor_tensor(out=me[:, :, g * 8:(g + 1) * 8], in0=lgE[:, :, g * 8:(g + 1) * 8],
                                                       in1=g4[:, :, g].unsqueeze(2).to_broadcast([128, NC, 8]), op=ALU.add))
                    dv(lambda: V.reduce_max(out=q8[:, 3, :], in_=me[:], axis=AX.X))
                    dv(lambda: V.tensor_tensor(out=o1, in0=me[:], in1=b32(q8[:, 3, :]), op=ALU.is_equal))
                    dv(lambda: V.scalar_tensor_tensor(out=me[:], in0=o1, scalar=-NEGBIG, in1=me[:], op0=ALU.mult, op1=ALU.add))
                    dv(lambda: V.reduce_max(out=q8[:, 4, :], in_=me[:], axis=AX.X))
                    dv(lambda: V.tensor_tensor(out=o2, in0=me[:], in1=b32(q8[:, 4, :]), op=ALU.is_equal))
                    dv(lambda: V.tensor_tensor(out=q8[:, 5, :], in0=q8[:, 4, :], in1=q8[:, 3, :], op=ALU.subtract))
                    P.op("act", lambda: nc.scalar.activation(out=q8[:, 5, :], in_=q8[:, 5, :], func=AF.Exp), reads=RW, writes=RW)
                    dv(lambda: V.tensor_scalar(out=q8[:, 6, :], in0=q8[:, 5, :], scalar1=1.0, scalar2=None, op0=ALU.add))
                    dv(lambda: V.reciprocal(out=q8[:, 6, :], in_=q8[:, 6, :]))
                    dv(lambda: V.tensor_tensor(out=g_all[:, T0:T0 + NC, 0], in0=q8[:, 6, :], in1=q8[:, 2, :], op=ALU.mult))
                    dv(lambda: V.tensor_tensor(out=g_all[:, T0:T0 + NC, 1], in0=g_all[:, T0:T0 + NC, 0], in1=q8[:, 5, :], op=ALU.mult))
                    aoh_all = SB(st, "aoh_all", [128, NC, NE], BF16)
                    dv(lambda: V.tensor_tensor(out=aoh_all[:], in0=o1, in1=o2, op=ALU.add))
                    for c in range(NC):
                        ti = T0 + c
                        pl2, r_pl2 = pL.nxt()
                        P.op("pe", lambda pl2=pl2, c=c: nc.tensor.matmul(pl2[:, 0:32], lhsT=tstrict_b, rhs=aoh_all[:, c, :], start=True, stop=True),
                             reads=RW + [r_c], writes=[r_pl2])
                        P.op("pe", lambda pl2=pl2, c=c: nc.tensor.matmul(pl2[:, 32:64], lhsT=ones_b, rhs=aoh_all[:, c, :], start=True, stop=True),
                             reads=RW + [r_c], writes=[r_pl2])
                        P.op("dve", lambda pl2=pl2, ti=ti: V.tensor_tensor(out=rank_all[:, ti, :], in0=pl2[:, 0:32], in1=carry[:], op=ALU.add),
                             reads=[r_pl2, r_route], writes=[r_route])
                        P.op("dve", lambda pl2=pl2: V.tensor_tensor(out=carry[:], in0=carry[:], in1=pl2[:, 32:64], op=ALU.add),
                             reads=[r_pl2, r_route], writes=[r_route])


                route_batch(s * NC, lg_all, st, r_lg)

        for s_ in range(NSEQ):
            seq_body(s_)

        P.barrier()
        with ExitStack() as st:
            V = nc.vector
            pe_ = SB(st, "pe_", [128, NE]); ps_a = SB(st, "ps_a", [128, NE]); ps_b = SB(st, "ps_b", [128, NE])
            tmpb = SB(st, "tmpb", [128, NT, NE])
            slotf = SB(st, "slotf", [128, NT, 2])
            bef = SB(st, "bef", [128, 4]); bdiag = SB(st, "bdiag", [128, 128])
            bebc = SB(st, "bebc", [128, 128])
            idxf = SB(st, "idxf", [128, NB, 4])
            pbc = MB(st, "pbc", [128, 128], F32, 1, psum=True)
            RWR = [r_route]

            def dv(f):
                P.op("dve", f, reads=RWR + [r_c], writes=RWR)
            dv(lambda: V.tensor_scalar(out=pe_[:], in0=carry[:], scalar1=0.0, scalar2=None, op0=ALU.is_gt))
            for m_ in range(1, (T + BS - 1) // BS):
                dv(lambda m_=m_: V.scalar_tensor_tensor(out=pe_[:], in0=carry[:], scalar=float(m_ * BS), in1=pe_[:],
                                                        op0=ALU.is_gt, op1=ALU.add))
            dv(lambda: V.tensor_scalar(out=pe_[:], in0=pe_[:], scalar1=float(BS), scalar2=None, op0=ALU.mult))
            src, dst = pe_, ps_a
            sh = 1
            while sh < NE:
                dv(lambda src=src, dst=dst, sh=sh: V.tensor_copy(out=dst[:, 0:sh], in_=src[:, 0:sh]))
                dv(lambda src=src, dst=dst, sh=sh: V.tensor_tensor(out=dst[:, sh:NE], in0=src[:, sh:NE], in1=src[:, 0:NE - sh], op=ALU.add))
                src, dst = dst, (ps_b if dst is ps_a else ps_a)
                if src is pe_:
                    pass
                sh *= 2
            p_end = src
            p_start = ps_b if p_end is ps_a else ps_a
            dv(lambda: V.tensor_tensor(out=p_start[:], in0=p_end[:], in1=pe_[:], op=ALU.subtract))
            dv(lambda: V.tensor_tensor(out=rank_all[:], in0=rank_all[:], in1=p_start[:].unsqueeze(1).to_broadcast([128, NT, NE]), op=ALU.add))
            for k_, oh in enumerate((oh1_all, oh2_all)):
                dv(lambda oh=oh: V.tensor_tensor(out=tmpb[:], in0=rank_all[:], in1=oh[:], op=ALU.mult))
                dv(lambda k_=k_: V.reduce_sum(out=slotf[:, :, k_], in_=tmpb[:], axis=AX.X))
            dv(lambda: V.tensor_copy(out=slot_i[:], in_=slotf[:]))
            m0 = CF_MISC * 128
            dv(lambda: V.tensor_scalar(out=pe_[:], in0=p_end[:], scalar1=cf[:, m0:m0 + 1], scalar2=None, op0=ALU.is_le))
            dv(lambda: V.reduce_sum(out=bef[:, 0:1], in_=pe_[:], axis=AX.X))
            dv(lambda: V.tensor_scalar(out=bdiag[:], in0=ident_f, scalar1=bef[:, 0:1], scalar2=None, op0=ALU.mult))
            pb, r_pb = pbc.nxt()
            P.op("pe", lambda: nc.tensor.matmul(pb[:], lhsT=ones_f, rhs=bdiag[:], start=True, stop=True), reads=RWR + [r_c], writes=[r_pb])
            P.op("dve", lambda: V.tensor_copy(out=bebc[:], in_=pb[:]), reads=[r_pb], writes=RWR)
            beadj = SB(st, "beadj", [128, 128])
            same = SB(st, "same", [128, 128])
            dv(lambda: V.tensor_copy(out=beadj[:], in_=bebc[:]))
            if NB > 2:
                dv(lambda: V.tensor_tensor(out=same[:, 2:NB], in0=bebc[:, 2:NB], in1=bebc[:, 0:NB - 2], op=ALU.is_equal))
                dv(lambda: V.scalar_tensor_tensor(out=beadj[:, 2:NB], in0=same[:, 2:NB], scalar=64.0, in1=bebc[:, 2:NB],
                                                  op0=ALU.mult, op1=ALU.add))
            for h2 in range(2):
                dv(lambda h2=h2: V.tensor_scalar(out=idxf[:, :, h2], in0=beadj[:, 0:NB], scalar1=256.0, scalar2=float(h2), op0=ALU.mult, op1=ALU.add))
                dv(lambda h2=h2: V.scalar_tensor_tensor(out=idxf[:, :, h2], in0=cf[:, m0 + 1:m0 + 2].to_broadcast([128, NB]), scalar=2.0,
                                                        in1=idxf[:, :, h2], op0=ALU.mult, op1=ALU.add))
            dv(lambda: V.tensor_copy(out=idx_g[:], in_=idxf[:, :, 0:2]))
            for fc in range(4):
                dv(lambda fc=fc: V.tensor_scalar(out=idxf[:, :, fc], in0=beadj[:, 0:NB], scalar1=512.0, scalar2=float(fc * 128), op0=ALU.mult, op1=ALU.add))
                dv(lambda fc=fc: V.tensor_tensor(out=idxf[:, :, fc], in0=idxf[:, :, fc], in1=cf[:, m0 + 1:m0 + 2].to_broadcast([128, NB]), op=ALU.add))
            dv(lambda: V.tensor_copy(out=idx_d[:], in_=idxf[:]))
            fence_t = SB(st, "fence_t", [128, 8])
            for _ in range(2):
                dv(lambda: V.memset(fence_t[:], 0.0))
            if dbg:
                dbgt = SB(st, "dbgt", [128, NT, 8])
                dv(lambda: V.memset(dbgt[:], 0.0))
                dv(lambda: V.tensor_copy(out=dbgt[:, :, 0:2], in_=slotf[:]))
                dv(lambda: V.tensor_copy(out=dbgt[:, :, 2:4], in_=g_all[:]))
                dv(lambda: V.tensor_copy(out=dbgt[:, :, 4:5], in_=bebc[:, 0:NT].unsqueeze(2)))
                dma("sp", dbg_d["dbg_route"], dbgt[:].rearrange("p a b -> p (a b)"), reads=RWR)
            xl = MB(st, "xl", [128, D], BF16, 3)
            bcs = {}

            def mk_bcs():
                bcs["s"] = nc.gpsimd.alloc_register("bc_slot")
                return nc.gpsimd.reg_mov(bcs["s"], NSLOT - 1)
            P.op("pool", mk_bcs)
            for ti in range(NT):
                t_, r_t = xl.nxt()
                dma("sp", t_[:], xn_d[ti * 128:(ti + 1) * 128, :], reads=[r_xn], writes=[r_t])
                for k_ in range(2):
                    P.op("pool", lambda t_=t_, ti=ti, k_=k_: nc.gpsimd.indirect_dma_start(
                        out=xs_d, out_offset=bass.IndirectOffsetOnAxis(ap=slot_i[:, ti, k_:k_ + 1], axis=0),
                        in_=t_[:], in_offset=None, bounds_check=bcs["s"], oob_is_err=False),
                        reads=[r_t, r_route, r_xs], writes=[r_xs], dma=True)

        P.barrier()
        with ExitStack() as st:
            Wg = MB(st, "Wg", [128, 2, 2048], BF16, 2)
            Wu = MB(st, "Wu", [128, 2, 2048], BF16, 2)
            Wd = MB(st, "Wd", [128, 4, 1024], BF16, 2)
            xb = MB(st, "xb", [128, D], BF16, 2 * NSUB)
            xbT = MB(st, "xbT", [128, 8, BS], BF16, 2)
            hd = MB(st, "hd", [128, 4, BS], BF16, 2)
            sg = MB(st, "sg", [128, BS], F32, 2)
            ysb = MB(st, "ysb", [128, D], BF16, 3)
            pT = MB(st, "pT3", [128, 8, 128], BF16, 2, psum=True)
            pG = MB(st, "pG", [128, BS], F32, 2, psum=True)
            pU = MB(st, "pU", [128, BS], F32, 2, psum=True)
            pYm = MB(st, "pYm", [128, 512], F32, 2, psum=True)
            bcr = {}

            def mk_bc():
                bcr["g"] = nc.gpsimd.alloc_register("bc_g")
                bcr["d"] = nc.gpsimd.alloc_register("bc_d")
                nc.gpsimd.reg_mov(bcr["g"], NE * 256 - 1)
                return nc.gpsimd.reg_mov(bcr["d"], NE * 512 - 1)
            P.op("pool", mk_bc)
            def do_T(xtiles):
                xT_, r_xT = xbT.nxt()
                for sub in range(NSUB):
                    x_, r_x = xtiles[sub]
                    pt, r_p = pT.nxt()
                    for j in range(8):
                        P.op("pe", lambda j=j, pt=pt, x_=x_: nc.tensor.transpose(
                            out=pt[:, j, :], in_=x_[:].rearrange("s (p j) -> s j p", j=8)[:, j, :], identity=ident_b),
                            reads=[r_x, r_c], writes=[r_p])
                    evac(xT_[:, :, sub * 128:(sub + 1) * 128], pt[:], [r_p], [r_xT])
                return xT_, r_xT

            for b in range(NB):
                g_, r_g = Wg.nxt()
                u_, r_u = Wu.nxt()
                d_, r_d = Wd.nxt()
                for h2 in range(2):
                    P.op("pool", lambda g_=g_, b=b, h2=h2: nc.gpsimd.indirect_dma_start(
                        out=g_[:, h2, :], out_offset=None, in_=wg,
                        in_offset=bass.IndirectOffsetOnAxis(ap=idx_g[:, b, h2:h2 + 1], axis=0),
                        bounds_check=bcr["g"], oob_is_err=False),
                        reads=[r_route], writes=[r_g], dma=True)
                    P.op("pool", lambda u_=u_, b=b, h2=h2: nc.gpsimd.indirect_dma_start(
                        out=u_[:, h2, :], out_offset=None, in_=wu,
                        in_offset=bass.IndirectOffsetOnAxis(ap=idx_g[:, b, h2:h2 + 1], axis=0),
                        bounds_check=bcr["g"], oob_is_err=False),
                        reads=[r_route], writes=[r_u], dma=True)
                for fc in range(4):
                    P.op("pool", lambda d_=d_, b=b, fc=fc: nc.gpsimd.indirect_dma_start(
                        out=d_[:, fc, :], out_offset=None, in_=wd,
                        in_offset=bass.IndirectOffsetOnAxis(ap=idx_d[:, b, fc:fc + 1], axis=0),
                        bounds_check=bcr["d"], oob_is_err=False),
                        reads=[r_route], writes=[r_d], dma=True)
                if b == 0:
                    xq = []
                    for sub in range(NSUB):
                        x_, r_x = xb.nxt()
                        dma("sp", x_[:], xs_d[sub * 128:(sub + 1) * 128, :], reads=[r_xs], writes=[r_x])
                        xq.append((x_, r_x))
                    xT_pend = do_T(xq)
                xT_, r_xT = xT_pend
                if b + 1 < NB:
                    xq = []
                    for sub in range(NSUB):
                        x_, r_x = xb.nxt()
                        r0 = (b + 1) * BS + sub * 128
                        dma("sp", x_[:], xs_d[r0:r0 + 128, :], reads=[r_xs], writes=[r_x])
                        xq.append((x_, r_x))
                h_, r_h_ = hd.nxt()
                gv = g_[:].rearrange("p a (j f) -> p (a j) f", f=512)
                uv = u_[:].rearrange("p a (j f) -> p (a j) f", f=512)
                for fc in range(4):
                    pg, r_pg = pG.nxt()
                    pu, r_pu = pU.nxt()
                    for j in range(8):
                        P.op("pe", lambda j=j, pg=pg, gv=gv, fc=fc, xT_=xT_: nc.tensor.matmul(
                            pg[:], lhsT=gv[:, j, fc * 128:(fc + 1) * 128], rhs=xT_[:, j, :], start=(j == 0), stop=(j == 7)),
                            reads=[r_g, r_xT], writes=[r_pg])
                    for j in range(8):
                        P.op("pe", lambda j=j, pu=pu, uv=uv, fc=fc, xT_=xT_: nc.tensor.matmul(
                            pu[:], lhsT=uv[:, j, fc * 128:(fc + 1) * 128], rhs=xT_[:, j, :], start=(j == 0), stop=(j == 7)),
                            reads=[r_u, r_xT], writes=[r_pu])
                    s_, r_s = sg.nxt()
                    P.op("act", lambda s_=s_, pg=pg: nc.scalar.activation(out=s_[:], in_=pg[:], func=AF.Silu), reads=[r_pg], writes=[r_s])
                    P.op("dve", lambda h_=h_, s_=s_, pu=pu, fc=fc: nc.vector.tensor_tensor(out=h_[:, fc, :], in0=s_[:], in1=pu[:], op=ALU.mult),
                         reads=[r_s, r_pu], writes=[r_h_])
                if b + 1 < NB:
                    xT_pend = do_T(xq)
                for sub in range(NSUB):
                    y_, r_y_ = ysb.nxt()
                    for half in range(2):
                        py, r_py = pYm.nxt()
                        for fc in range(4):
                            P.op("pe", lambda fc=fc, py=py, h_=h_, d_=d_, sub=sub, half=half: nc.tensor.matmul(
                                py[:], lhsT=h_[:, fc, sub * 128:(sub + 1) * 128], rhs=d_[:, fc, half * 512:(half + 1) * 512],
                                start=(fc == 0), stop=(fc == 3)), reads=[r_h_, r_d], writes=[r_py])
                        evac(y_[:, half * 512:(half + 1) * 512], py[:], [r_py], [r_y_])
                    r0 = b * BS + sub * 128
                    dma("sp", y_d[r0:r0 + 128, :], y_[:], reads=[r_y_], writes=[r_y])

        P.barrier()
        with ExitStack() as st:
            ht = MB(st, "ht3", [128, D], F32, 5)
            nlw_t = SB(st, "nlw_t", [128, D]); r_nl = R()
            dma("sp", nlw_t[:], nlw.partition_broadcast(128), writes=[r_nl])
            y1 = MB(st, "y1", [128, D], BF16, 4)
            y2 = MB(st, "y2", [128, D], BF16, 4)
            sq = SB(st, "sq3", [128, D])
            stat = MB(st, "stat3", [128, 2], F32, 4)
            ot = MB(st, "ot3", [128, D], F32, 3)
            outv = out.rearrange("s l d -> (s l) d")
            hq = []

            def load_h(ti):
                h_, r_ht = ht.nxt()
                dma("sp", h_[:], h_d[ti * 128:(ti + 1) * 128, :], reads=[r_h], writes=[r_ht])
                hq.append((h_, r_ht))
            PRE = 3
            for ti in range(min(PRE, NT)):
                load_h(ti)
            for ti in range(NT):
                if ti + PRE < NT:
                    load_h(ti + PRE)
                h_, r_ht = hq.pop(0)
                ys = []
                for k_, yb in enumerate((y1, y2)):
                    y_, r_y_ = yb.nxt()
                    P.op("pool", lambda y_=y_, ti=ti, k_=k_: nc.gpsimd.indirect_dma_start(
                        out=y_[:], out_offset=None, in_=y_d,
                        in_offset=bass.IndirectOffsetOnAxis(ap=slot_i[:, ti, k_:k_ + 1], axis=0),
                        bounds_check=bcs["s"], oob_is_err=False),
                        reads=[r_route, r_y], writes=[r_y_], dma=True)
                    ys.append((y_, r_y_))
                for k_ in range(2):
                    y_, r_y_ = ys[k_]
                    P.op("dve", lambda h_=h_, y_=y_, ti=ti, k_=k_: nc.vector.scalar_tensor_tensor(
                        out=h_[:], in0=y_[:], scalar=g_all[:, ti, k_:k_ + 1], in1=h_[:], op0=ALU.mult, op1=ALU.add),
                        reads=[r_y_, r_ht, r_route], writes=[r_ht])
                st_, r_st = stat.nxt()
                rms_rstd("z", h_[:], st_[:, 0:1], st_[:, 1:2], sq[:], r_ht, r_st, D)
                o_, r_o = ot.nxt()
                P.op("dve", lambda o_=o_, h_=h_, st_=st_: nc.vector.scalar_tensor_tensor(
                    out=o_[:], in0=h_[:], scalar=st_[:, 1:2], in1=nlw_t[:], op0=ALU.mult, op1=ALU.mult),
                    reads=[r_ht, r_st, r_nl], writes=[r_o])
                dma("sp", outv[ti * 128:(ti + 1) * 128, :], o_[:], reads=[r_o])
        P.emit(top)
    return nc


def host_inputs(inp, L, BS, n_cores, nseq):
    consts, _ = make_consts(L, BS)
    f = lambda a: np.ascontiguousarray(np.asarray(a, dtype=np.float32))
    common = dict(
        w_in=f(inp["w_in"][0]), w_out=f(inp["w_out"][0]),
        wr=f(np.concatenate([inp["w_router_group"][0], inp["w_router_exp"][0]], axis=1)),
        br=f(np.concatenate([inp["b_router_group"][0], inp["b_router_exp"][0]])[None, :]),
        wg=f(inp["w_exp_gate"][0]).reshape(NE * 128 * 2, 2048),
        wu=f(inp["w_exp_up"][0]).reshape(NE * 128 * 2, 2048),
        wd=f(inp["w_exp_down"][0]).reshape(NE * 512, 1024),
        nmw=f(inp["norm_mix_w"]), nfw=f(inp["norm_ffn_w"]), nlw=f(inp["norm_final_w"])[None, :],
        convw=f(np.asarray(inp["conv_w"][0]).T.reshape(8, 128, 5).transpose(1, 0, 2).reshape(128, 40)),
        convb=f(np.asarray(inp["conv_b"][0]).reshape(8, 128).T),
        dtb=f(np.concatenate([inp["dt_bias_fwd"][0], inp["dt_bias_bwd"][0]])[None, :]),
        alog=f(np.concatenate([inp["a_log_fwd"][0], inp["a_log_bwd"][0]])[None, :]),
        dsk=f(inp["ssd_d"]), snw=f(inp["ssd_norm_w"]),
        lam=f(np.stack([inp["lambda_q1"][0], inp["lambda_q2"][0], inp["lambda_k1"][0], inp["lambda_k2"][0]])),
        sub_w=f(inp["subln_w"]),
        **consts,
    )
    xs = f(inp["x"])
    maps = []
    for c in range(n_cores):
        m = dict(common)
        m["x"] = np.ascontiguousarray(xs[c * nseq:(c + 1) * nseq])
        maps.append(m)
    return maps


def kernel(**inputs):
    n_cores, nseq, L, BS = 8, 2, 4096, 512
    nc = build(nseq, L, BS)
    maps = host_inputs(inputs, L, BS, n_cores, nseq)
    res = run_bass_kernel_spmd(nc, maps, core_ids=list(range(n_cores)))
    return np.concatenate([np.asarray(r["out"], dtype=np.float32) for r in res.results], axis=0)
```
